# Optimizing a Trainium2 kernel written in Bass

```python
import math
import jax, jax.numpy as jnp
from jax import lax
import numpy as np

D_MODEL = 1024
BATCH = 8
SEQ = 4096
DEPTH = 2

N_MIXERS = 2
N_A_LAYERS = (DEPTH + 1) // 2
N_B_LAYERS = DEPTH // 2
RMS_EPS = 1e-6
ROPE_THETA = 500000.0

H_A = 8
DH_A = D_MODEL // H_A
ROT_A = DH_A // 4
H_IDX = 8
D_IDX = 64
ROT_IDX = D_IDX // 4
TOPK_MAX = 256
Q_BLOCK = 128
A_COLS = [H_A * DH_A, DH_A, DH_A, H_IDX * D_IDX, D_IDX, H_IDX]
A_IN = sum(A_COLS)

H_R = 4
DK_R = D_MODEL // H_R
DV_R = 2 * DK_R
RET_THETA = 10000.0
CHUNK = 128
B_COLS = [H_R * DK_R, H_R * DK_R, H_R * DV_R, H_R * DV_R]
B_IN = sum(B_COLS)

D_FF = 4 * D_MODEL

kernel_name = "dsa_retention_interleaved_hybrid"


def _offsets(cols):
    return [int(v) for v in np.cumsum(cols)[:-1]]


def rmsnorm(x, g):
    xf = x.astype(jnp.float32)
    y = xf * lax.rsqrt(jnp.mean(xf * xf, axis=-1, keepdims=True) + RMS_EPS)
    return (y * g.astype(jnp.float32)).astype(x.dtype)


def rotary(x, pos, rot_dim, theta):
    half = rot_dim // 2
    inv = theta ** (-jnp.arange(half, dtype=jnp.float32) / half)
    ang = pos.astype(jnp.float32)[..., None] * inv
    cos = jnp.cos(ang)[:, :, None, :]
    sin = jnp.sin(ang)[:, :, None, :]
    x1 = x[..., :half].astype(jnp.float32)
    x2 = x[..., half:rot_dim].astype(jnp.float32)
    rot = jnp.concatenate([x1 * cos - x2 * sin, x2 * cos + x1 * sin], axis=-1).astype(x.dtype)
    return jnp.concatenate([rot, x[..., rot_dim:]], axis=-1)


def dsa_mixer(h, pos, w_in, w_out):
    B, S, _ = h.shape
    topk = min(TOPK_MAX, S // 4)
    nb = S // Q_BLOCK
    q, k, v, qi, ki, wi = jnp.split(h @ w_in, _offsets(A_COLS), axis=-1)
    q = rotary(q.reshape(B, S, H_A, DH_A), pos, ROT_A, ROPE_THETA)
    k = rotary(k[:, :, None, :], pos, ROT_A, ROPE_THETA)[:, :, 0]
    qi = rotary(qi.reshape(B, S, H_IDX, D_IDX), pos, ROT_IDX, ROPE_THETA)
    ki = rotary(ki[:, :, None, :], pos, ROT_IDX, ROPE_THETA)[:, :, 0]
    wi = wi * (H_IDX ** -0.5 * D_IDX ** -0.5)
    kv = jnp.concatenate([k, v], axis=-1)
    key_idx = jnp.arange(S)
    scale = 1.0 / math.sqrt(DH_A)

    def to_blocks(a):
        return a.reshape((B, nb, Q_BLOCK) + a.shape[2:]).swapaxes(0, 1)

    t_blk = jnp.arange(S).reshape(nb, Q_BLOCK)

    def block(args):
        qb, qib, wb, tb = args
        sc = jnp.einsum('bqhd,bsd->bqhs', qib, ki).astype(jnp.float32)
        idx_score = jnp.einsum('bqhs,bqh->bqs', jax.nn.relu(sc), wb.astype(jnp.float32))
        idx_score = jnp.where(key_idx[None, None, :] <= tb[None, :, None], idx_score, -jnp.inf)
        _, sel = lax.top_k(idx_score, topk)
        kvg = jax.vmap(lambda a, i: a[i])(kv, sel)
        kg, vg = kvg[..., :DH_A], kvg[..., DH_A:]
        valid = sel <= tb[None, :, None]
        logits = jnp.einsum('bqhd,bqkd->bqhk', qb, kg).astype(jnp.float32) * scale
        logits = jnp.where(valid[:, :, None, :], logits, -jnp.inf)
        p = jax.nn.softmax(logits, axis=-1)
        return jnp.einsum('bqhk,bqkd->bqhd', p.astype(vg.dtype), vg)

    o = lax.map(block, (to_blocks(q), to_blocks(qi), to_blocks(wi), t_blk))
    o = o.swapaxes(0, 1).reshape(B, S, H_A * DH_A)
    return o @ w_out


def retention_mixer(h, pos, w_in, gn_g, w_out):
    B, S, _ = h.shape
    n = S // CHUNK
    q, k, v, g = jnp.split(h @ w_in, _offsets(B_COLS), axis=-1)
    q = rotary(q.reshape(B, S, H_R, DK_R), pos, DK_R, RET_THETA)
    k = rotary(k.reshape(B, S, H_R, DK_R), pos, DK_R, RET_THETA) * (DK_R ** -0.5)
    v = v.reshape(B, S, H_R, DV_R)

    def chunks(a):
        return a.reshape(B, n, CHUNK, H_R, a.shape[-1]).transpose(1, 0, 3, 2, 4).astype(jnp.float32)

    log_gamma = jnp.log1p(-jnp.exp2(-5.0 - jnp.arange(H_R, dtype=jnp.float32)))
    j = jnp.arange(CHUNK, dtype=jnp.float32)
    diff = j[:, None] - j[None, :]
    decay = jnp.where(diff >= 0, jnp.exp(jnp.maximum(diff, 0.0) * log_gamma[:, None, None]), 0.0)
    xi = jnp.exp((j + 1.0) * log_gamma[:, None])[..., None]
    zeta = jnp.exp((CHUNK - 1.0 - j) * log_gamma[:, None])[..., None]
    gamma_c = jnp.exp(CHUNK * log_gamma)[:, None, None]

    def step(state, inp):
        qc, kc, vc = inp
        inner = jnp.einsum('bhnd,bhmd->bhnm', qc, kc) * decay
        out = jnp.einsum('bhnm,bhme->bhne', inner, vc) + jnp.einsum('bhnd,bhde->bhne', qc * xi, state)
        state = gamma_c * state + jnp.einsum('bhmd,bhme->bhde', kc * zeta, vc)
        return state, out

    state0 = jnp.zeros((B, H_R, DK_R, DV_R), jnp.float32)
    _, ys = lax.scan(step, state0, (chunks(q), chunks(k), chunks(v)))
    y = ys.transpose(1, 0, 3, 2, 4).reshape(B, S, H_R, DV_R)
    mu = jnp.mean(y, axis=-1, keepdims=True)
    var = jnp.mean(jnp.square(y - mu), axis=-1, keepdims=True)
    y = ((y - mu) * lax.rsqrt(var + RMS_EPS)).reshape(B, S, H_R * DV_R) * gn_g.astype(jnp.float32)
    y = (jax.nn.silu(g.astype(jnp.float32)) * y).astype(h.dtype)
    return y @ w_out


def sqrelu_mlp(h, w_up, w_down):
    return jnp.square(jax.nn.relu(h @ w_up)) @ w_down


def setup_inputs(seed: int = 0) -> dict:
    key = jax.random.key(seed)
    ks = jax.random.split(key, 12)

    def w(k, shape, fan_in):
        return jax.random.normal(k, shape, jnp.float32) * (fan_in ** -0.5)

    def gain(k, shape):
        return 1.0 + 0.02 * jax.random.normal(k, shape, jnp.float32)

    x = jax.random.normal(ks[0], (BATCH, SEQ, D_MODEL), jnp.float32)
    positions = jnp.broadcast_to(jnp.arange(SEQ, dtype=jnp.int32)[None, :], (BATCH, SEQ))
    return {
        "x": x,
        "positions": positions,
        "norm_mix_g": gain(ks[1], (DEPTH, D_MODEL)),
        "norm_mlp_g": gain(ks[2], (DEPTH, D_MODEL)),
        "w_in_a": w(ks[3], (N_A_LAYERS, D_MODEL, A_IN), D_MODEL),
        "w_out_a": w(ks[4], (N_A_LAYERS, H_A * DH_A, D_MODEL), H_A * DH_A),
        "w_in_b": w(ks[5], (N_B_LAYERS, D_MODEL, B_IN), D_MODEL),
        "ret_norm_g": gain(ks[6], (N_B_LAYERS, H_R * DV_R)),
        "w_out_b": w(ks[7], (N_B_LAYERS, H_R * DV_R, D_MODEL), H_R * DV_R),
        "w_mlp_up": w(ks[8], (DEPTH, D_MODEL, D_FF), D_MODEL),
        "w_mlp_down": w(ks[9], (DEPTH, D_FF, D_MODEL), D_FF),
        "final_norm_g": gain(ks[10], (D_MODEL,)),
    }


def reference(x, positions, norm_mix_g, norm_mlp_g, w_in_a, w_out_a, w_in_b, ret_norm_g,
              w_out_b, w_mlp_up, w_mlp_down, final_norm_g):
    h = x
    for i in range(DEPTH):
        j = i // N_MIXERS
        hn = rmsnorm(h, norm_mix_g[i])
        if i % N_MIXERS == 0:
            h = h + dsa_mixer(hn, positions, w_in_a[j], w_out_a[j])
        else:
            h = h + retention_mixer(hn, positions, w_in_b[j], ret_norm_g[j], w_out_b[j])
        h = h + sqrelu_mlp(rmsnorm(h, norm_mlp_g[i]), w_mlp_up[i], w_mlp_down[i])
    return rmsnorm(h, final_norm_g)
```

```python
import math
import numpy as np
import concourse.bass as bass
import concourse.mybir as mybir
from concourse.bass_utils import run_bass_kernel_spmd

F32 = mybir.dt.float32
BF16 = mybir.dt.bfloat16
I32 = mybir.dt.int32
AF = mybir.ActivationFunctionType
ALU = mybir.AluOpType
AX = mybir.AxisListType

D = 1024
NCORES = 8
RMS_EPS = 1e-6
DFF = 4096
H_A, DH_A, ROT_A = 8, 128, 32
H_IDX, D_IDX, ROT_IDX = 8, 64, 16
A_IN = 1864
ROPE_THETA = 500000.0
H_R, DK_R, DV_R = 4, 256, 512
B_IN = 6144
RET_THETA = 10000.0
NEG = -30000.0
BIG = 3.0e38


class Buf:
    __slots__ = ("name", "w", "rs")

    def __init__(self, name):
        self.name = name
        self.w = None
        self.rs = []


class Prog:
    ENG = ("pe", "act", "dve", "pool", "sp")
    NDSEM = 8

    def __init__(self):
        self.st = {e: [] for e in self.ENG}
        self.seen = {e: {} for e in self.ENG}
        self.dq = {e: 0 for e in ("sp", "act", "pool")}
        self.dval = {}
        self.out_events = []

    def _deps(self, eng, reads, writes):
        deps = []
        for b in reads:
            if b.w is not None:
                deps.append(b.w)
        for b in writes:
            if b.w is not None:
                ev = b.w
                if not (ev[0] == "e" and ev[1] == eng):
                    deps.append(ev)
            for ev in b.rs:
                if not (ev[0] == "e" and ev[1] == eng):
                    deps.append(ev)
        return deps

    def _filter(self, eng, deps):
        waits = {}
        for ev in deps:
            if ev[0] == "e":
                if ev[1] == eng and eng == "pe":
                    continue
                key = ("e", ev[1])
            else:
                key = ("d", ev[1])
            if self.seen[eng].get(key, -1) >= ev[2]:
                continue
            if waits.get(key, -1) < ev[2]:
                waits[key] = ev[2]
        for key, v in waits.items():
            self.seen[eng][key] = v
            if key[0] == "e":
                self.st[key[1]][v]["mark"] = True
        return list(waits.items())

    def _commit(self, ev, reads, writes):
        for b in reads:
            b.rs.append(ev)
        for b in writes:
            b.w = ev
            b.rs = []

    def op(self, eng, fn, reads=(), writes=()):
        deps = self._deps(eng, reads, writes)
        waits = self._filter(eng, deps)
        idx = len(self.st[eng])
        self.st[eng].append({"fn": fn, "waits": waits, "mark": False, "dma": None})
        ev = ("e", eng, idx)
        self._commit(ev, reads, writes)
        return ev

    def dma(self, q, out, in_, reads=(), writes=(), is_out=False, **kw):
        k = self.dq[q] % self.NDSEM
        self.dq[q] += 1
        key = (q, k)
        prev = self.dval.get(key, 0)
        deps = self._deps(None, reads, writes)
        if prev > 0:
            deps.append(("d", key, prev))
        waits = self._filter(q, deps)
        val = prev + 16
        self.dval[key] = val
        idx = len(self.st[q])
        self.st[q].append({"fn": lambda e, o=out, i=in_: e.dma_start(out=o, in_=i, **kw),
                           "waits": waits, "mark": False, "dma": (key, 16)})
        ev = ("d", key, val)
        self._commit(ev, reads, writes)
        if is_out:
            self.out_events.append(ev)
        return ev

    def barrier(self):
        lasts = []
        for e in self.ENG:
            for i in range(len(self.st[e]) - 1, -1, -1):
                r = self.st[e][i]
                if r["fn"] is not None and r["dma"] is None:
                    lasts.append(("e", e, i))
                    break
        for key, v in self.dval.items():
            lasts.append(("d", key, v))
        for e in self.ENG:
            deps = [ev for ev in lasts if not (ev[0] == "e" and ev[1] == e)]
            waits = self._filter(e, deps)
            if waits:
                self.st[e].append({"fn": None, "waits": waits, "mark": False, "dma": None})

    def finish(self):
        waits = self._filter("sp", list(self.out_events))
        self.st["sp"].append({"fn": None, "waits": waits, "mark": False, "dma": None})

    def emit(self, nc, es):
        esem = {e: es.enter_context(nc.semaphore("s_" + e)) for e in self.ENG}
        dsem = {}
        for q in ("sp", "act", "pool"):
            for k in range(self.NDSEM):
                dsem[(q, k)] = es.enter_context(nc.semaphore("d_%s%d" % (q, k)))
        cum = {}
        for e in self.ENG:
            c = 0
            arr = []
            for r in self.st[e]:
                if r["mark"]:
                    c += 1
                arr.append(c)
            cum[e] = arr
        block = es.enter_context(nc.Block())

        def run(eng_name):
            def body(engine):
                for r in self.st[eng_name]:
                    for key, v in r["waits"]:
                        if key[0] == "e":
                            engine.wait_ge(esem[key[1]], cum[key[1]][v])
                        else:
                            engine.wait_ge(dsem[key[1]], v)
                    if r["fn"] is None:
                        continue
                    ins = r["fn"](engine)
                    if r["dma"] is not None:
                        ins.then_inc(dsem[r["dma"][0]], 16)
                        assert not r["mark"]
                    elif r["mark"]:
                        ins.then_inc(esem[eng_name], 1)
            return body

        block.tensor(run("pe"))
        block.scalar(run("act"))
        block.vector(run("dve"))
        block.gpsimd(run("pool"))
        block.sync(run("sp"))


class Arena:
    def __init__(self, nc, es, nbytes):
        self.t = es.enter_context(nc.sbuf_tensor("arena", [128, nbytes // 4], F32))
        self.nbytes = nbytes
        self.off = 0
        self.marks = []

    def alloc(self, shape, dtype):
        esz = 2 if dtype == BF16 else 4
        n = int(np.prod(shape))
        nb = (n * esz + 63) // 64 * 64
        assert self.off + nb <= self.nbytes, ("SBUF arena overflow", self.off, nb, self.nbytes)
        a = self.off // 4
        ap = self.t[:, a:a + nb // 4]
        if dtype != F32:
            ap = ap.bitcast(dtype)
        ap = ap[:, 0:n]
        self.off += nb
        if len(shape) == 2:
            ap = ap.rearrange("p (a b) -> p a b", a=shape[0])
        elif len(shape) == 3:
            ap = ap.rearrange("p (a b c) -> p a b c", a=shape[0], b=shape[1])
        return ap

    def push(self):
        self.marks.append(self.off)

    def pop(self):
        self.off = self.marks.pop()


class Ctx:
    pass


def load_weight_bf16(c, w_dram, dst, dst_buf, K, C, col0=0, ncols=None, splits=2):
    ncols = C if ncols is None else ncols
    kc = K // 128
    src = w_dram.rearrange("(k p) c -> p k c", p=128)
    p = c.p
    CH = 1024
    if ncols >= CH:
        pieces = [(k, 1, c0, min(CH, ncols - c0)) for k in range(kc) for c0 in range(0, ncols, CH)]
    else:
        kk = max(1, CH // ncols)
        pieces = [(k, min(kk, kc - k), 0, ncols) for k in range(0, kc, kk)]
    engs = ("dve", "act", "dve", "act", "dve", "act", "pool")
    for (k, nk, c0, w) in pieces:
        i = c.stage_i
        c.stage_i += 1
        st = c.stage[i % len(c.stage)]
        sb = c.stage_b[i % len(c.stage)]
        stv = st[:, 0:nk * w].rearrange("p (a b) -> p a b", a=nk)
        p.dma("sp" if i % 2 == 0 else "act", stv, src[:, k:k + nk, col0 + c0:col0 + c0 + w], writes=[sb])
        eng = engs[i % len(engs)]
        if eng == "act":
            p.op("act", lambda e, stv=stv, k=k, nk=nk, c0=c0, w=w: e.copy(out=dst[:, k:k + nk, c0:c0 + w], in_=stv),
                 reads=[sb], writes=[dst_buf])
        else:
            p.op(eng, lambda e, stv=stv, k=k, nk=nk, c0=c0, w=w: e.tensor_copy(out=dst[:, k:k + nk, c0:c0 + w], in_=stv),
                 reads=[sb], writes=[dst_buf])


def rstd_ops(c, stat, stat_buf, scale, n=1):
    p = c.p
    p.op("dve", lambda e: e.tensor_scalar(out=stat[:, n:2 * n], in0=stat[:, 0:n], scalar1=scale, scalar2=RMS_EPS,
                                          op0=ALU.mult, op1=ALU.add),
         reads=[stat_buf], writes=[stat_buf])
    p.op("act", lambda e: e.activation(out=stat[:, n:2 * n], in_=stat[:, n:2 * n], func=AF.Ln),
         reads=[stat_buf], writes=[stat_buf])
    p.op("act", lambda e: e.activation(out=stat[:, 2 * n:3 * n], in_=stat[:, n:2 * n], func=AF.Exp, scale=-0.5),
         reads=[stat_buf], writes=[stat_buf])


def rmsnorm_tile(c, xt, xt_buf, g_bc, g_buf, hn, hn_buf, sq_junk, junk_buf, stat, stat_buf):
    p = c.p
    p.op("act", lambda e: e.activation(out=sq_junk, in_=xt, func=AF.Square, accum_out=stat[:, 0:1]),
         reads=[xt_buf], writes=[junk_buf, stat_buf])
    rstd_ops(c, stat, stat_buf, 1.0 / D)
    p.op("dve", lambda e: e.scalar_tensor_tensor(out=hn, in0=xt, scalar=stat[:, 2:3], in1=g_bc,
                                                 op0=ALU.mult, op1=ALU.mult),
         reads=[xt_buf, stat_buf, g_buf], writes=[hn_buf])


def transpose_to(c, src, src_buf, nchunks, ps, ps_buf, dst, dst_buf, evac="act"):
    p = c.p
    for k in range(nchunks):
        p.op("pe", lambda e, k=k: e.transpose(out=ps[:, k, :], in_=src[:, k * 128:(k + 1) * 128], identity=c.ident),
             reads=[src_buf, c.ident_b], writes=[ps_buf])
    if evac == "act":
        p.op("act", lambda e: e.copy(out=dst, in_=ps[:, 0:nchunks, :]), reads=[ps_buf], writes=[dst_buf])
    else:
        p.op(evac, lambda e: e.tensor_copy(out=dst, in_=ps[:, 0:nchunks, :]), reads=[ps_buf], writes=[dst_buf])


def mlp_phase(c, li, h_in, h_out, final_g=None, out_dram=None):
    p, ar, nc = c.p, c.ar, c.nc
    S = c.S
    TB = 256
    NJ = TB // 128
    nblk = S // TB
    ar.push()
    wup = ar.alloc([8, DFF], BF16)
    wdn = ar.alloc([32, D], BF16)
    wup_b, wdn_b = Buf("wup"), Buf("wdn")
    gbc = ar.alloc([D], F32)
    gbc_b = Buf("gbc")
    p.dma("sp", gbc, c.norm_mlp_g[li:li + 1, :].partition_broadcast(128), writes=[gbc_b])
    load_weight_bf16(c, c.w_mlp_up[li], wup, wup_b, D, DFF, splits=4)
    load_weight_bf16(c, c.w_mlp_down[li], wdn, wdn_b, DFF, D, splits=4)
    if final_g is not None:
        fg = ar.alloc([D], F32)
        fg_b = Buf("fg")
        p.dma("sp", fg, final_g.partition_broadcast(128), writes=[fg_b])
    NX = 4
    xt = [ar.alloc([D], F32) for _ in range(NX)]
    xt_b = [Buf("xt%d" % i) for i in range(NX)]
    hn = [ar.alloc([D], BF16) for _ in range(2)]
    hn_b = [Buf("hn%d" % i) for i in range(2)]
    junk = ar.alloc([D], BF16)
    junk_b = Buf("junk")
    stat = [ar.alloc([4], F32) for _ in range(4)]
    stat_b = [Buf("stat%d" % i) for i in range(4)]
    hnT = [ar.alloc([8, TB], BF16) for _ in range(2)]
    hnT_b = [Buf("hnT%d" % i) for i in range(2)]
    actT = ar.alloc([32, TB], BF16)
    actT_b = [Buf("actT%d" % i) for i in range(32)]
    rl = [ar.alloc([TB], F32) for _ in range(2)]
    rl_b = [Buf("rl%d" % i) for i in range(2)]
    pst = [c.psum[i].bitcast(BF16) for i in (0, 1)]
    pst = [t.rearrange("p (a b) -> p a b", b=128) for t in pst]
    pst_b = [c.psum_b[0], c.psum_b[1]]
    xi = [0]

    def stage1(b):
        tiles = []
        for j in range(NJ):
            t = b * NJ + j
            s = xi[0] % NX
            xi[0] += 1
            p.dma("sp", xt[s], h_in[t * 128:(t + 1) * 128, :], writes=[xt_b[s]])
            hs = t % 2
            ss = t % 4
            rmsnorm_tile(c, xt[s], xt_b[s], gbc, gbc_b, hn[hs], hn_b[hs], junk, junk_b, stat[ss], stat_b[ss])
            transpose_to(c, hn[hs], hn_b[hs], 8, pst[t % 2], pst_b[t % 2],
                         hnT[b % 2][:, :, j * 128:(j + 1) * 128], hnT_b[b % 2])
            tiles.append(s)
        return tiles

    def stage2(b, tiles):
        hT = hnT[b % 2]
        for f in range(32):
            ps = c.psum[2 + f % 2]
            ps_b = c.psum_b[2 + f % 2]
            for k in range(8):
                p.op("pe", lambda e, ps=ps, k=k, f=f: e.matmul(ps[:, 0:TB], lhsT=wup[:, k, f * 128:(f + 1) * 128],
                                                               rhs=hT[:, k, :], start=(k == 0), stop=(k == 7)),
                     reads=[wup_b, hnT_b[b % 2]], writes=[ps_b])
            r = rl[f % 2]
            p.op("act", lambda e, ps=ps, r=r: e.activation(out=r, in_=ps[:, 0:TB], func=AF.Relu),
                 reads=[ps_b], writes=[rl_b[f % 2]])
            p.op("dve", lambda e, r=r, f=f: e.tensor_tensor(out=actT[:, f, :], in0=r, in1=r, op=ALU.mult),
                 reads=[rl_b[f % 2]], writes=[actT_b[f]])
        for j in range(NJ):
            t = b * NJ + j
            s = tiles[j]
            for half in range(2):
                ps = c.psum[4 + (2 * j + half) % 4]
                ps_b = c.psum_b[4 + (2 * j + half) % 4]
                for f in range(32):
                    p.op("pe", lambda e, ps=ps, f=f, j=j, half=half: e.matmul(
                        ps[:, 0:512], lhsT=actT[:, f, j * 128:(j + 1) * 128],
                        rhs=wdn[:, f, half * 512:(half + 1) * 512], start=(f == 0), stop=(f == 31)),
                         reads=[wdn_b, actT_b[f]], writes=[ps_b])
                p.op("dve", lambda e, ps=ps, s=s, half=half: e.tensor_tensor(
                    out=xt[s][:, half * 512:(half + 1) * 512], in0=ps[:, 0:512],
                    in1=xt[s][:, half * 512:(half + 1) * 512], op=ALU.add),
                     reads=[ps_b, xt_b[s]], writes=[xt_b[s]])
            if final_g is None:
                p.dma("sp", h_out[t * 128:(t + 1) * 128, :], xt[s], reads=[xt_b[s]])
            else:
                ss = t % 4
                p.op("act", lambda e, s=s, ss=ss: e.activation(out=junk, in_=xt[s], func=AF.Square,
                                                               accum_out=stat[ss][:, 0:1]),
                     reads=[xt_b[s]], writes=[junk_b, stat_b[ss]])
                rstd_ops(c, stat[ss], stat_b[ss], 1.0 / D)
                p.op("dve", lambda e, s=s, ss=ss: e.scalar_tensor_tensor(out=xt[s], in0=xt[s], scalar=stat[ss][:, 2:3],
                                                                         in1=fg, op0=ALU.mult, op1=ALU.mult),
                     reads=[xt_b[s], stat_b[ss], fg_b], writes=[xt_b[s]])
                p.dma("sp", out_dram[t * 128:(t + 1) * 128, :], xt[s], reads=[xt_b[s]], is_out=True)

    prev = None
    for b in range(nblk + 1):
        cur = stage1(b) if b < nblk else None
        if prev is not None:
            stage2(b - 1, prev)
        prev = cur
    p.barrier()
    ar.pop()


def sincos_from_turns(c, t, t_b, n, barrier=True):
    p, ar = c.p, c.ar
    if barrier:
        ar.push()
        ni = ar.alloc([n], I32)
        nf = ar.alloc([n], F32)
        tb = Buf("sc_tmp")
    else:
        if not hasattr(c, "sc_ni"):
            c.sc_ni = ar.alloc([n], I32)
            c.sc_nf = ar.alloc([n], F32)
            c.sc_tb = Buf("sc_tmpR")
        ni, nf, tb = c.sc_ni, c.sc_nf, c.sc_tb
    p.op("dve", lambda e: e.tensor_copy(out=ni, in_=t), reads=[t_b], writes=[tb])
    p.op("dve", lambda e: e.tensor_copy(out=nf, in_=ni), reads=[tb], writes=[tb])
    p.op("dve", lambda e: e.tensor_tensor(out=t, in0=t, in1=nf, op=ALU.subtract), reads=[t_b, tb], writes=[t_b])
    p.op("dve", lambda e: e.tensor_scalar(out=nf, in0=t, scalar1=0.5, scalar2=None, op0=ALU.is_gt), reads=[t_b], writes=[tb])
    p.op("dve", lambda e: e.tensor_tensor(out=t, in0=t, in1=nf, op=ALU.subtract), reads=[t_b, tb], writes=[t_b])
    p.op("act", lambda e: e.activation(out=t, in_=t, func=AF.Sin, scale=2 * math.pi * (1 - 1e-6)), reads=[t_b], writes=[t_b])
    if barrier:
        p.barrier()
        ar.pop()


def dsa_phase(c, h_in, h_out):
    p, ar = c.p, c.ar
    S = c.S
    NT = S // 128
    topk = min(256, S // 4)
    NIT = 14
    PS = c.ps_all

    def bank(i, n=1):
        return PS[:, i * 512:(i + n) * 512]

    def bankbf(i):
        return PS[:, i * 512:(i + 1) * 512].bitcast(BF16).rearrange("p (a b) -> p a b", b=128)
    pb = c.psum_b
    qT_d = c.qT_d
    qiT_d = c.qiT_d

    ar.push()
    kT = ar.alloc([S], BF16)
    V = ar.alloc([NT, 128], BF16)
    kiT2 = ar.alloc([S], BF16)
    wi = ar.alloc([NT, 8], F32)
    wout = ar.alloc([8, D], BF16)
    tab = ar.alloc([NT, 48], F32)
    posi = ar.alloc([NT], I32)
    posf = ar.alloc([NT], F32)
    gbc = ar.alloc([D], F32)
    kT_b = [Buf("kT%d" % t) for t in range(NT)]
    V_b = [Buf("V%d" % t) for t in range(NT)]
    ki_b = [Buf("ki%d" % t) for t in range(NT)]
    wi_b = [Buf("wi%d" % t) for t in range(NT)]
    qTd_b = [Buf("qTd%d" % t) for t in range(NT)]
    qiTd_b = [Buf("qiTd%d" % t) for t in range(NT)]
    wout_b, tab_b, pos_b, gbc_b = Buf("wout"), Buf("tab"), Buf("pos"), Buf("gbcA")
    K = c.K

    p.dma("sp", gbc, c.norm_mix_g[0:1, :].partition_broadcast(128), writes=[gbc_b])
    p.dma("sp", posi, c.pos, writes=[pos_b])
    p.op("dve", lambda e: e.tensor_copy(out=posf, in_=posi), reads=[pos_b], writes=[pos_b])
    p.op("dve", lambda e: e.tensor_tensor(out=tab, in0=posf.unsqueeze(2).broadcast_to([128, NT, 48]),
                                          in1=K["invrow"].unsqueeze(1).broadcast_to([128, NT, 48]), op=ALU.mult),
         reads=[pos_b, c.K_b], writes=[tab_b])
    p.op("dve", lambda e: e.tensor_tensor(out=tab, in0=tab, in1=K["offs"].unsqueeze(1).broadcast_to([128, NT, 48]),
                                          op=ALU.add), reads=[tab_b, c.K_b], writes=[tab_b])
    tabf = tab.rearrange("p a b -> p (a b)")
    if c.dbg is not None:
        p.dma("sp", c.dbgt[:, NT * 48:NT * 49], posf, reads=[pos_b], is_out=True)
    sincos_from_turns(c, tabf, tab_b, NT * 48)
    if c.dbg is not None:
        p.dma("sp", c.dbgt[:, 0:NT * 48], tabf, reads=[tab_b], is_out=True)

    ar.push()
    win = ar.alloc([8, A_IN], BF16)
    win_b = Buf("win")
    load_weight_bf16(c, c.w_in_a, win, win_b, D, A_IN)
    load_weight_bf16(c, c.w_out_a, wout, wout_b, D, D)
    xt = [ar.alloc([D], F32) for _ in range(2)]
    xt_b = [Buf("xtA%d" % i) for i in range(2)]
    hn = [ar.alloc([D], BF16) for _ in range(2)]
    hn_b = [Buf("hnA%d" % i) for i in range(2)]
    junk = ar.alloc([D], BF16)
    junk_b = Buf("junkA")
    stat = [ar.alloc([4], F32) for _ in range(2)]
    stat_b = [Buf("statA%d" % i) for i in range(2)]
    hnT = [ar.alloc([8, 128], BF16) for _ in range(2)]
    hnT_b = [Buf("hnTA%d" % i) for i in range(2)]
    proj = [ar.alloc([A_IN], F32) for _ in range(2)]
    proj_b = [[Buf("proj%d_%d" % (a, i)) for i in range(4)] for a in range(2)]
    qkb = ar.alloc([9, 128], BF16)
    qkb_b = Buf("qkb")
    ixb = ar.alloc([10, 64], BF16)
    ixb_b = Buf("ixb")
    tA = [ar.alloc([9, 16], F32) for _ in range(4)]
    tA_b = [Buf("tA%d" % i) for i in range(4)]
    tI = [ar.alloc([9, 8], F32) for _ in range(4)]
    tI_b = [Buf("tI%d" % i) for i in range(4)]
    qTt = [ar.alloc([8, 128], BF16) for _ in range(2)]
    qTt_b = [Buf("qTt%d" % i) for i in range(2)]
    qiTt = [ar.alloc([4, 128], BF16) for _ in range(2)]
    qiTt_b = [Buf("qiTt%d" % i) for i in range(2)]
    chunks = [(0, 512), (512, 512), (1024, 512), (1536, A_IN - 1536)]

    def a1_front(t):
        s = t % 2
        p.dma("sp", xt[s], h_in[t * 128:(t + 1) * 128, :], writes=[xt_b[s]])
        rmsnorm_tile(c, xt[s], xt_b[s], gbc, gbc_b, hn[s], hn_b[s], junk, junk_b, stat[s], stat_b[s])
        transpose_to(c, hn[s], hn_b[s], 8, bankbf(0), pb[0], hnT[s], hnT_b[s])
        for ci, (c0, w) in enumerate(chunks):
            ps = bank(1 + ci)
            for k in range(8):
                p.op("pe", lambda e, ps=ps, k=k, c0=c0, w=w: e.matmul(ps[:, 0:w], lhsT=hnT[s][:, k, :], rhs=win[:, k, c0:c0 + w],
                                                                      start=(k == 0), stop=(k == 7)),
                     reads=[hnT_b[s], win_b], writes=[pb[1 + ci]])
            p.op("act", lambda e, ps=ps, c0=c0, w=w: e.copy(out=proj[s][:, c0:c0 + w], in_=ps[:, 0:w]),
                 reads=[pb[1 + ci]], writes=[proj_b[s][ci]])

    def a1_back(t):
        s = t % 2
        pj = proj[s]

        def pbufs(lo, hi):
            return [proj_b[s][i] for i, (c0, w) in enumerate(chunks) if c0 < hi and c0 + w > lo]
        pa = pj[:, 0:1152].rearrange("p (h d) -> p h d", h=9)
        cosA = tab[:, t, 16:32].unsqueeze(1).broadcast_to([128, 9, 16])
        sinA = tab[:, t, 0:16].unsqueeze(1).broadcast_to([128, 9, 16])
        rA = pbufs(0, 1152)
        p.op("dve", lambda e: e.tensor_tensor(out=tA[0], in0=pa[:, :, 0:16], in1=cosA, op=ALU.mult), reads=rA + [tab_b], writes=[tA_b[0]])
        p.op("dve", lambda e: e.tensor_tensor(out=tA[1], in0=pa[:, :, 16:32], in1=sinA, op=ALU.mult), reads=rA + [tab_b], writes=[tA_b[1]])
        p.op("dve", lambda e: e.tensor_tensor(out=tA[2], in0=pa[:, :, 16:32], in1=cosA, op=ALU.mult), reads=rA + [tab_b], writes=[tA_b[2]])
        p.op("dve", lambda e: e.tensor_tensor(out=tA[3], in0=pa[:, :, 0:16], in1=sinA, op=ALU.mult), reads=rA + [tab_b], writes=[tA_b[3]])
        p.op("dve", lambda e: e.tensor_tensor(out=qkb[:, :, 0:16], in0=tA[0], in1=tA[1], op=ALU.subtract), reads=[tA_b[0], tA_b[1]], writes=[qkb_b])
        p.op("dve", lambda e: e.tensor_tensor(out=qkb[:, :, 16:32], in0=tA[2], in1=tA[3], op=ALU.add), reads=[tA_b[2], tA_b[3]], writes=[qkb_b])
        p.op("dve", lambda e: e.tensor_copy(out=qkb[:, :, 32:128], in_=pa[:, :, 32:128]), reads=rA, writes=[qkb_b])
        pi_ = pj[:, 1280:1856].rearrange("p (h d) -> p h d", h=9)
        cosI = tab[:, t, 40:48].unsqueeze(1).broadcast_to([128, 9, 8])
        sinI = tab[:, t, 32:40].unsqueeze(1).broadcast_to([128, 9, 8])
        rI = pbufs(1280, 1856)
        p.op("pool", lambda e: e.tensor_tensor(out=tI[0], in0=pi_[:, :, 0:8], in1=cosI, op=ALU.mult), reads=rI + [tab_b], writes=[tI_b[0]])
        p.op("pool", lambda e: e.tensor_tensor(out=tI[1], in0=pi_[:, :, 8:16], in1=sinI, op=ALU.mult), reads=rI + [tab_b], writes=[tI_b[1]])
        p.op("pool", lambda e: e.tensor_tensor(out=tI[2], in0=pi_[:, :, 8:16], in1=cosI, op=ALU.mult), reads=rI + [tab_b], writes=[tI_b[2]])
        p.op("pool", lambda e: e.tensor_tensor(out=tI[3], in0=pi_[:, :, 0:8], in1=sinI, op=ALU.mult), reads=rI + [tab_b], writes=[tI_b[3]])
        p.op("pool", lambda e: e.tensor_tensor(out=ixb[:, 0:9, 0:8], in0=tI[0], in1=tI[1], op=ALU.subtract), reads=[tI_b[0], tI_b[1]], writes=[ixb_b])
        p.op("pool", lambda e: e.tensor_tensor(out=ixb[:, 0:9, 8:16], in0=tI[2], in1=tI[3], op=ALU.add), reads=[tI_b[2], tI_b[3]], writes=[ixb_b])
        p.op("act", lambda e: e.copy(out=ixb[:, 0:9, 16:64], in_=pi_[:, :, 16:64]), reads=rI, writes=[ixb_b])
        p.op("act", lambda e: e.copy(out=ixb[:, 9, :], in_=ixb[:, 8, :]), reads=[ixb_b], writes=[ixb_b])
        p.op("act", lambda e: e.copy(out=V[:, t, :], in_=pj[:, 1152:1280]), reads=pbufs(1152, 1280), writes=[V_b[t]])
        p.op("act", lambda e: e.mul(out=wi[:, t, :], in_=pj[:, 1856:1864], mul=float(512 ** -0.5)), reads=pbufs(1856, 1864), writes=[wi_b[t]])
        qkf = qkb.rearrange("p h d -> p (h d)")
        ixf = ixb.rearrange("p h d -> p (h d)")
        for h in range(8):
            p.op("pe", lambda e, h=h: e.transpose(out=bankbf(5)[:, h, :], in_=qkf[:, h * 128:(h + 1) * 128], identity=c.ident),
                 reads=[qkb_b, c.ident_b], writes=[pb[5]])
        p.op("act", lambda e: e.copy(out=qTt[s], in_=bankbf(5)), reads=[pb[5]], writes=[qTt_b[s]])
        p.dma("sp", qT_d[t], qTt[s], reads=[qTt_b[s]], writes=[qTd_b[t]])
        p.op("pe", lambda e: e.transpose(out=bankbf(6)[:, 0, :], in_=qkf[:, 1024:1152], identity=c.ident),
             reads=[qkb_b, c.ident_b], writes=[pb[6]])
        for g in range(5):
            p.op("pe", lambda e, g=g: e.transpose(out=bankbf(6)[:, 1 + g, :], in_=ixf[:, g * 128:(g + 1) * 128], identity=c.ident),
                 reads=[ixb_b, c.ident_b], writes=[pb[6]])
        p.op("act", lambda e: e.copy(out=kT[:, t * 128:(t + 1) * 128], in_=bankbf(6)[:, 0, :]), reads=[pb[6]], writes=[kT_b[t]])
        p.op("act", lambda e: e.copy(out=qiTt[s], in_=bankbf(6)[:, 1:5, :]), reads=[pb[6]], writes=[qiTt_b[s]])
        p.op("act", lambda e: e.copy(out=kiT2[:, t * 128:(t + 1) * 128], in_=bankbf(6)[:, 5, :]), reads=[pb[6]], writes=[ki_b[t]])
        p.dma("sp", qiT_d[t], qiTt[s], reads=[qiTt_b[s]], writes=[qiTd_b[t]])

    a1_front(0)
    for t in range(NT):
        if t + 1 < NT:
            a1_front(t + 1)
        a1_back(t)
    p.barrier()
    ar.pop()

    mb_d = c.mb_d
    mbd_b = [Buf("mbd%d" % t) for t in range(NT)]
    ar.push()
    rh = [ar.alloc([8, 512], BF16) for _ in range(2)]
    rh_b = [[Buf("rh%d_%d" % (i, h)) for h in range(8)] for i in range(2)]
    diag = [ar.alloc([8, 128], BF16) for _ in range(2)]
    diag_b = [Buf("diag%d" % i) for i in range(2)]
    score = [ar.alloc([S], F32) for _ in range(3)]
    score_b = [Buf("score%d" % i) for i in range(3)]
    cjunk = ar.alloc([S], BF16)
    cjunk_b = Buf("cjunk")
    mbx = [ar.alloc([S], BF16) for _ in range(2)]
    mbx_b = [Buf("mbx%d" % i) for i in range(2)]
    qiTs = [ar.alloc([4, 128], BF16) for _ in range(2)]
    qiTs_b = [Buf("qiTs%d" % i) for i in range(2)]
    bs = [ar.alloc([8 + NIT], F32) for _ in range(3)]
    bs_b = [Buf("bs%d" % i) for i in range(3)]
    bs2 = [ar.alloc([4], F32) for _ in range(3)]
    bs2_b = [Buf("bs2_%d" % i) for i in range(3)]
    bs2a_b = [Buf("bs2a_%d" % i) for i in range(3)]
    ajunk = ar.alloc([S], BF16)
    ajunk_b = Buf("ajunk")
    cjunks = [cjunk, ar.alloc([S], BF16)]
    cjunks_b = [cjunk_b, Buf("cjunk1")]
    ajunks = [ajunk, ar.alloc([S], BF16)]
    ajunks_b = [ajunk_b, Buf("ajunk1")]

    def x_loads(i):
        p.dma("sp", qiTs[i % 2], qiT_d[i], reads=[qiTd_b[i]], writes=[qiTs_b[i % 2]])

    def bis_steps(i, n):
        s = i % 3
        sj = i % 2
        L = (i + 1) * 128
        b_, bb = bs[s], bs_b[s]
        LA = (L // 256) * 128
        sc = score[s]
        thr = float(topk) - 0.5 - 0.5 * LA
        steps = []
        steps.append(lambda: p.op("dve", lambda e: e.tensor_tensor(out=b_[:, 2:3], in0=b_[:, 0:1], in1=b_[:, 8 + n:9 + n], op=ALU.add),
                                  reads=[bb], writes=[bb]))

        def counts():
            if LA > 0:
                p.op("act", lambda e: e.activation(out=ajunks[sj][:, 0:LA], in_=sc[:, 0:LA], func=AF.Sign, bias=b_[:, 2:3], scale=-1.0,
                                                   accum_out=bs2[s][:, 1:2]), reads=[bb, score_b[s]], writes=[bs2a_b[s], ajunks_b[sj]])
            p.op("dve", lambda e: e.tensor_scalar(out=cjunks[sj][:, LA:L], in0=sc[:, LA:L], scalar1=b_[:, 2:3], scalar2=None,
                                                  op0=ALU.is_ge, op1=ALU.add, accum_out=b_[:, 3:4]),
                 reads=[bb, score_b[s]], writes=[bb, cjunks_b[sj]])
        steps.append(counts)
        if LA > 0:
            steps.append(lambda: p.op("dve", lambda e: e.scalar_tensor_tensor(out=b_[:, 3:4], in0=bs2[s][:, 1:2], scalar=-0.5, in1=b_[:, 3:4],
                                                                              op0=ALU.mult, op1=ALU.add), reads=[bb, bs2a_b[s]], writes=[bb]))
        steps.append(lambda: p.op("dve", lambda e: e.scalar_tensor_tensor(out=b_[:, 4:5], in0=b_[:, 3:4], scalar=thr,
                                                                          in1=b_[:, 8 + n:9 + n], op0=ALU.is_ge, op1=ALU.mult),
                                  reads=[bb], writes=[bb]))
        steps.append(lambda: p.op("dve", lambda e: e.tensor_tensor(out=b_[:, 0:1], in0=b_[:, 0:1], in1=b_[:, 4:5], op=ALU.add),
                                  reads=[bb], writes=[bb]))
        return steps

    def x_final(i):
        s = i % 3
        sm = i % 2
        L = (i + 1) * 128
        b_, bb = bs[s], bs_b[s]
        m_ = mbx[sm]
        sc = score[s][:, 0:L]
        p.op("dve", lambda e: e.tensor_scalar(out=m_[:, 0:L], in0=sc, scalar1=b_[:, 0:1], scalar2=NEG,
                                              op0=ALU.is_lt, op1=ALU.mult),
             reads=[score_b[s], bb], writes=[mbx_b[sm]])
        p.dma("sp", mb_d[i, :, 0:L], m_[:, 0:L], reads=[mbx_b[sm]], writes=[mbd_b[i]])

    def x_chunk(i, ch, nch):
        s = i % 3
        sq = i % 2
        L = (i + 1) * 128
        b_, bb = bs[s], bs_b[s]
        sco, scb = score[s], score_b[s]
        k0 = ch * 512
        w = min(512, L - k0)
        rr = rh[ch % 2]
        rrb = rh_b[ch % 2]
        kb = [ki_b[t] for t in range(k0 // 128, (k0 + w) // 128)]
        for h in range(8):
            hp, j = h % 2, h // 2
            bi = h % 2
            ps = bank(bi)
            p.op("pe", lambda e, ps=ps, hp=hp, j=j: e.matmul(
                ps[:, 0:w], lhsT=qiTs[sq][hp * 64:(hp + 1) * 64, j, :], rhs=kiT2[hp * 64:(hp + 1) * 64, k0:k0 + w],
                start=True, stop=True), reads=[qiTs_b[sq]] + kb, writes=[pb[bi]])
            p.op("act", lambda e, ps=ps, h=h: e.activation(out=rr[:, h, 0:w], in_=ps[:, 0:w], func=AF.Relu),
                 reads=[pb[bi]], writes=[rrb[h]])
        b2 = 2 + ch % 2
        ps2 = bank(b2)
        for h in range(8):
            p.op("pe", lambda e, h=h: e.matmul(ps2[:, 0:w], lhsT=diag[sq][:, h, :], rhs=rr[:, h, 0:w],
                                               start=(h == 0), stop=(h == 7)),
                 reads=[diag_b[sq], rrb[h]], writes=[pb[b2]])
        last = (ch == nch - 1)
        wc = w - 128 if last else w
        if wc > 0:
            p.op("dve", lambda e: e.tensor_copy(out=sco[:, k0:k0 + wc], in_=ps2[:, 0:wc]), reads=[pb[b2]], writes=[scb])
        if last:
            p.op("dve", lambda e: e.tensor_reduce(out=b_[:, 5:6], in_=ps2[:, w - 128:w], axis=AX.X, op=ALU.min),
                 reads=[pb[b2]], writes=[bb])
            p.op("dve", lambda e: e.tensor_tensor(out=sco[:, k0 + w - 128:k0 + w], in0=ps2[:, w - 128:w],
                                                  in1=K["causal"], op=ALU.add),
                 reads=[pb[b2], c.K_b], writes=[scb])

    def x_setup(i):
        s = i % 3
        L = (i + 1) * 128
        b_, bb = bs[s], bs_b[s]
        sco, scb = score[s], score_b[s]
        sc = sco[:, 0:L]
        p.op("dve", lambda e: e.tensor_reduce(out=b_[:, 1:2], in_=sc, axis=AX.X, op=ALU.max), reads=[scb], writes=[bb])
        if L > 128:
            p.op("dve", lambda e: e.tensor_reduce(out=b_[:, 0:1], in_=sco[:, 0:L - 128], axis=AX.X, op=ALU.min),
                 reads=[scb], writes=[bb])
            p.op("dve", lambda e: e.tensor_tensor(out=b_[:, 0:1], in0=b_[:, 0:1], in1=b_[:, 5:6], op=ALU.min), reads=[bb], writes=[bb])
        else:
            p.op("dve", lambda e: e.tensor_copy(out=b_[:, 0:1], in_=b_[:, 5:6]), reads=[bb], writes=[bb])
        p.op("dve", lambda e: e.tensor_tensor(out=b_[:, 1:2], in0=b_[:, 1:2], in1=b_[:, 0:1], op=ALU.subtract), reads=[bb], writes=[bb])
        p.op("dve", lambda e: e.tensor_tensor(out=b_[:, 8:8 + NIT], in0=b_[:, 1:2].broadcast_to([128, NIT]),
                                              in1=K["pow2"][:, 0:NIT], op=ALU.mult), reads=[bb, c.K_b], writes=[bb])

    x_loads(0)
    H1 = NIT // 2
    for i in range(NT + 2):
        if i + 1 < NT:
            x_loads(i + 1)
        its = []
        ia = list(range(0, H1)) if 1 <= i <= NT else []
        ib = list(range(H1, NIT)) if 2 <= i <= NT + 1 else []
        while ia or ib:
            sa = bis_steps(i - 1, ia.pop(0)) if ia else []
            sb = bis_steps(i - 2, ib.pop(0)) if ib else []
            while sa or sb:
                if sa:
                    its.append(sa.pop(0))
                if sb:
                    its.append(sb.pop(0))
        if i < NT:
            sq = i % 2
            p.op("pool", lambda e, sq=sq, i=i: e.tensor_tensor(out=diag[sq], in0=c.ident.unsqueeze(1).broadcast_to([128, 8, 128]),
                                                               in1=wi[:, i, :].unsqueeze(2).broadcast_to([128, 8, 128]), op=ALU.mult),
                 reads=[c.ident_b, wi_b[i]], writes=[diag_b[sq]])
            nch = ((i + 1) * 128 + 511) // 512
            per = (len(its) + nch - 1) // nch if its else 0
            for ch in range(nch):
                x_chunk(i, ch, nch)
                for _ in range(per):
                    if its:
                        its.pop(0)()
        while its:
            its.pop(0)()
        if 2 <= i <= NT + 1:
            x_final(i - 2)
        if i < NT:
            x_setup(i)
    p.barrier()
    ar.pop()

    ar.push()
    mb = [ar.alloc([S], BF16) for _ in range(2)]
    mb_b = [Buf("mb%d" % i) for i in range(2)]
    pT = [ar.alloc([4, 128], BF16) for _ in range(4)]
    pT_b = [Buf("pT%d" % i) for i in range(4)]
    recip = [ar.alloc([512], F32) for _ in range(2)]
    recip_b = [Buf("recip%d" % i) for i in range(2)]
    oT = [ar.alloc([8, 128], BF16) for _ in range(2)]
    oT_b = [[Buf("oT%d_%d" % (i, hh)) for hh in range(2)] for i in range(2)]
    qTs = [ar.alloc([8, 128], BF16) for _ in range(2)]
    qTs_b = [Buf("qTs%d" % i) for i in range(2)]
    xo = [ar.alloc([D], F32) for _ in range(3)]
    xo_b = [Buf("xo%d" % i) for i in range(3)]
    scale = float(DH_A ** -0.5)
    pti = [0]

    def y_loads(i):
        s = i % 2
        L = (i + 1) * 128
        p.dma("sp", qTs[s], qT_d[i], reads=[qTd_b[i]], writes=[qTs_b[s]])
        p.dma("sp", mb[s][:, 0:L], mb_d[i, :, 0:L], reads=[mbd_b[i]], writes=[mb_b[s]])
        p.dma("sp", xo[i % 3], h_in[i * 128:(i + 1) * 128, :], writes=[xo_b[i % 3]])

    def Yhalf(i, hh):
        s = i % 2
        m_ = mb[s]
        o_ = oT[s]
        bo, bl = (5, 6) if hh == 0 else (0, 1)
        pts = {}

        def qk(kt):
            bi = 3 + kt % 2
            ab = bank(bi)
            p.op("pe", lambda e: e.matmul(ab, lhsT=kT[:, kt * 128:(kt + 1) * 128],
                                          rhs=qTs[s][:, hh * 4:(hh + 1) * 4, :], start=True, stop=False),
                 reads=[kT_b[kt], qTs_b[s]], writes=[pb[bi]])
            p.op("pe", lambda e: e.matmul(ab, lhsT=m_[:, kt * 128:(kt + 1) * 128], rhs=K["I4"], start=False, stop=True),
                 reads=[mb_b[s], c.K_b], writes=[pb[bi]])
            pt = pT[pti[0] % 4]
            ptb = pT_b[pti[0] % 4]
            pti[0] += 1
            pts[kt] = (pt, ptb)
            p.op("act", lambda e: e.activation(out=pt.rearrange("p a b -> p (a b)"), in_=ab, func=AF.Exp, scale=scale),
                 reads=[pb[bi]], writes=[ptb])

        def pv(kt):
            pt, ptb = pts.pop(kt)
            p.op("pe", lambda e: e.matmul(bank(bo), lhsT=V[:, kt, :], rhs=pt.rearrange("p a b -> p (a b)"),
                                          start=(kt == 0), stop=(kt == i)),
                 reads=[V_b[kt], ptb], writes=[pb[bo]])
            p.op("pe", lambda e: e.matmul(bank(bl), lhsT=K["ones"], rhs=pt.rearrange("p a b -> p (a b)"),
                                          start=(kt == 0), stop=(kt == i)),
                 reads=[c.K_b, ptb], writes=[pb[bl]])

        qk(0)
        for kt in range(i + 1):
            if kt + 1 <= i:
                qk(kt + 1)
            pv(kt)
        rc, rcb = recip[hh], recip_b[hh]
        p.op("act", lambda e: e.activation(out=rc, in_=bank(bl), func=AF.Ln), reads=[pb[bl]], writes=[rcb])
        p.op("act", lambda e: e.activation(out=rc, in_=rc, func=AF.Exp, scale=-1.0), reads=[rcb], writes=[rcb])
        p.op("dve", lambda e: e.tensor_tensor(out=o_[:, hh * 4:(hh + 1) * 4, :].rearrange("p a b -> p (a b)"), in0=bank(bo),
                                              in1=rc, op=ALU.mult),
             reads=[pb[bo], rcb], writes=[oT_b[s][hh]])

    def Yout(i):
        s = i % 2
        o_ = oT[s]
        for half in range(2):
            bi = 2 if half == 0 else 7
            ps = bank(bi)
            for h in range(8):
                p.op("pe", lambda e, h=h, ps=ps, half=half: e.matmul(ps, lhsT=o_[:, h, :], rhs=wout[:, h, half * 512:(half + 1) * 512],
                                                                     start=(h == 0), stop=(h == 7)),
                     reads=[oT_b[s][h // 4], wout_b], writes=[pb[bi]])
            p.op("dve", lambda e, half=half, ps=ps: e.tensor_tensor(out=xo[i % 3][:, half * 512:(half + 1) * 512], in0=ps,
                                                             in1=xo[i % 3][:, half * 512:(half + 1) * 512], op=ALU.add),
                 reads=[pb[bi], xo_b[i % 3]], writes=[xo_b[i % 3]])
        p.dma("sp", h_out[i * 128:(i + 1) * 128, :], xo[i % 3], reads=[xo_b[i % 3]], writes=[c.hres_b[i]])

    y_loads(0)
    for i in range(NT):
        if i + 1 < NT:
            y_loads(i + 1)
        Yhalf(i, 0)
        Yhalf(i, 1)
        if i >= 1:
            Yout(i - 1)
    Yout(NT - 1)
    p.barrier()
    ar.pop()
    ar.pop()


def ret_phase(c, h_in, h_out):
    p, ar = c.p, c.ar
    S = c.S
    NT = S // 128
    PS = c.ps_all
    pb = c.psum_b
    K = c.K
    kf = K["f"]

    def bank(i, n=1):
        return PS[:, i * 512:(i + n) * 512]

    def bankbf(i):
        return PS[:, i * 512:(i + 1) * 512].bitcast(BF16).rearrange("p (a b) -> p a b", b=128)
    ygT_d = c.ygT_d
    rp_d = c.rp_d
    ygd_b = [Buf("ygd%d" % t) for t in range(NT)]
    rpd_b = [Buf("rpd%d" % t) for t in range(NT)]
    RC = 368
    decT = kf[:, RC:RC + 512].rearrange("p (h n) -> p h n", h=4)
    xi = kf[:, RC + 512:RC + 516]
    zeta = kf[:, RC + 516:RC + 520]
    invr = kf[:, RC + 520:RC + 648]
    lg = [math.log1p(-2.0 ** (-5.0 - h)) for h in range(4)]
    gam_c = [float(math.exp(128.0 * x)) for x in lg]
    OQ, OK_, OKZ, OV, OSG = 0, 1024, 2048, 3072, 5120

    ar.push()
    win = ar.alloc([8, B_IN], BF16)
    win_b = Buf("winB")
    gbc = ar.alloc([D], F32)
    gbc_b = Buf("gbcR")
    posi = ar.alloc([NT], I32)
    posf = ar.alloc([NT], F32)
    pos_b = Buf("posR")
    p.dma("sp", gbc, c.norm_mix_g[1:2, :].partition_broadcast(128), writes=[gbc_b])
    p.dma("sp", posi, c.pos, writes=[pos_b])
    p.op("dve", lambda e: e.tensor_copy(out=posf, in_=posi), reads=[pos_b], writes=[pos_b])
    load_weight_bf16(c, c.w_in_b, win, win_b, D, B_IN)
    xt = [ar.alloc([D], F32) for _ in range(2)]
    xt_b = [Buf("xtR%d" % i) for i in range(2)]
    hn = [ar.alloc([D], BF16) for _ in range(2)]
    hn_b = [Buf("hnR%d" % i) for i in range(2)]
    junk = ar.alloc([D], BF16)
    junk_b = Buf("junkR")
    stat = [ar.alloc([4], F32) for _ in range(2)]
    stat_b = [Buf("statR%d" % i) for i in range(2)]
    hnT = [ar.alloc([8, 128], BF16) for _ in range(2)]
    hnT_b = [Buf("hnTR%d" % i) for i in range(2)]
    tb = [ar.alloc([4, 128], F32) for _ in range(2)]
    tb_b = [Buf("tbR%d" % i) for i in range(2)]
    tts = [[ar.alloc([2, 128], F32) for _ in range(4)] for _ in range(2)]
    tts_b = [[Buf("ttR%d_%d" % (a, i)) for i in range(4)] for a in range(2)]
    rot = [ar.alloc([2, 256], BF16) for _ in range(2)]
    rot_b = [Buf("rotR%d" % i) for i in range(2)]
    pk = [ar.alloc([7168], BF16) for _ in range(2)]
    pk_b = [Buf("pkR%d" % i) for i in range(2)]
    cnt = [0]

    def stageA(t):
        s = t % 2
        p.dma("sp", xt[s], h_in[t * 128:(t + 1) * 128, :], writes=[xt_b[s]])
        rmsnorm_tile(c, xt[s], xt_b[s], gbc, gbc_b, hn[s], hn_b[s], junk, junk_b, stat[s], stat_b[s])
        transpose_to(c, hn[s], hn_b[s], 8, bankbf(0), pb[0], hnT[s], hnT_b[s])
        tb_, tbb = tb[s], tb_b[s]
        p.op("dve", lambda e: e.tensor_scalar(out=tb_[:, 0, :], in0=invr, scalar1=posf[:, t:t + 1], scalar2=None, op0=ALU.mult),
             reads=[pos_b, c.K_b], writes=[tbb])
        p.op("dve", lambda e: e.tensor_scalar(out=tb_[:, 1, :], in0=tb_[:, 0, :], scalar1=0.25, scalar2=None, op0=ALU.add),
             reads=[tbb], writes=[tbb])
        sincos_from_turns(c, tb_[:, 0:2, :].rearrange("p a b -> p (a b)"), tbb, 256, barrier=False)
        p.op("dve", lambda e: e.tensor_scalar(out=tb_[:, 2:4, :], in0=tb_[:, 0:2, :], scalar1=float(DK_R ** -0.5), scalar2=None, op0=ALU.mult),
             reads=[tbb], writes=[tbb])

    def inproj(t, c0, bi):
        s = t % 2
        ps = bank(bi)
        for k in range(8):
            p.op("pe", lambda e, k=k: e.matmul(ps, lhsT=hnT[s][:, k, :], rhs=win[:, k, c0:c0 + 512], start=(k == 0), stop=(k == 7)),
                 reads=[hnT_b[s], win_b], writes=[pb[bi]])
        return ps

    def stageB1(t):
        s = t % 2
        pk_ = pk[s]
        for qk in range(2):
            sinT = tb[s][:, 2 * qk, :].unsqueeze(1).broadcast_to([128, 2, 128])
            cosT = tb[s][:, 2 * qk + 1, :].unsqueeze(1).broadcast_to([128, 2, 128])
            for ci in range(2):
                n = cnt[0]
                cnt[0] += 1
                bi = 1 + n % 3
                ps = inproj(t, qk * 1024 + ci * 512, bi)
                px = ps.rearrange("p (h d) -> p h d", h=2)
                tt, tt_b = tts[n % 2], tts_b[n % 2]
                r_, rb = rot[n % 2], rot_b[n % 2]

                def mul(o, ob, a, b_):
                    p.op("dve", lambda e: e.tensor_tensor(out=o, in0=a, in1=b_, op=ALU.mult), reads=[pb[bi], tb_b[s]], writes=[ob])
                mul(tt[0], tt_b[0], px[:, :, 0:128], cosT)
                mul(tt[1], tt_b[1], px[:, :, 128:256], sinT)
                mul(tt[2], tt_b[2], px[:, :, 128:256], cosT)
                mul(tt[3], tt_b[3], px[:, :, 0:128], sinT)
                p.op("dve", lambda e, r_=r_, tt=tt: e.tensor_tensor(out=r_[:, :, 0:128], in0=tt[0], in1=tt[1], op=ALU.subtract),
                     reads=[tt_b[0], tt_b[1]], writes=[rb])
                p.op("dve", lambda e, r_=r_, tt=tt: e.tensor_tensor(out=r_[:, :, 128:256], in0=tt[2], in1=tt[3], op=ALU.add),
                     reads=[tt_b[2], tt_b[3]], writes=[rb])
                if qk == 1:
                    for hh in range(2):
                        h = 2 * ci + hh
                        p.op("act", lambda e, r_=r_, hh=hh, h=h: e.activation(out=pk_[:, OKZ + h * 256:OKZ + (h + 1) * 256], in_=r_[:, hh, :],
                                                                              func=AF.Identity, scale=zeta[:, h:h + 1]),
                             reads=[rb, c.K_b], writes=[pk_b[s]])
                rf = r_.rearrange("p h d -> p (h d)")
                tbi = 4 + n % 2
                for j in range(4):
                    p.op("pe", lambda e, rf=rf, j=j, tbi=tbi: e.transpose(out=bankbf(tbi)[:, j, :], in_=rf[:, j * 128:(j + 1) * 128], identity=c.ident),
                         reads=[rb, c.ident_b], writes=[pb[tbi]])
                o0 = (OQ if qk == 0 else OK_) + ci * 512
                p.op("act", lambda e, o0=o0, tbi=tbi: e.copy(out=pk_[:, o0:o0 + 512].rearrange("p (a b) -> p a b", a=4), in_=bankbf(tbi)[:, 0:4, :]),
                     reads=[pb[tbi]], writes=[pk_b[s]])

    def stageB2(t):
        s = t % 2
        pk_ = pk[s]
        for ci in range(8):
            n = cnt[0]
            cnt[0] += 1
            bi = 1 + n % 3
            ps = inproj(t, 2048 + ci * 512, bi)
            if ci < 4:
                p.op("act", lambda e, ps=ps, ci=ci: e.copy(out=pk_[:, OV + ci * 512:OV + (ci + 1) * 512], in_=ps), reads=[pb[bi]], writes=[pk_b[s]])
            else:
                p.op("act", lambda e, ps=ps, ci=ci: e.activation(out=pk_[:, OSG + (ci - 4) * 512:OSG + (ci - 3) * 512], in_=ps, func=AF.Silu),
                     reads=[pb[bi]], writes=[pk_b[s]])
        p.dma("sp", rp_d[t], pk_, reads=[pk_b[s]], writes=[rpd_b[t]])

    stageA(0)
    for t in range(NT):
        stageB1(t)
        if t + 1 < NT:
            stageA(t + 1)
        stageB2(t)
    p.barrier()
    ar.pop()
    if hasattr(c, "sc_ni"):
        del c.sc_ni

    ar.push()
    state = ar.alloc([4, 2, 512], F32)
    state_bf = ar.alloc([4, 2, 512], BF16)
    st_b = [[Buf("st%d_%d" % (h, dc)) for dc in range(2)] for h in range(4)]
    stbf_b = [[Buf("stbf%d_%d" % (h, dc)) for dc in range(2)] for h in range(4)]
    gng = ar.alloc([2048], F32)
    gng_b = Buf("gng")
    p.dma("sp", gng, c.ret_norm_g[0:1, :].partition_broadcast(128), writes=[gng_b])
    p.op("dve", lambda e: e.memset(state.rearrange("p a b c -> p (a b c)"), 0.0), writes=[b for r in st_b for b in r])
    p.op("dve", lambda e: e.memset(state_bf.rearrange("p a b c -> p (a b c)"), 0.0), writes=[b for r in stbf_b for b in r])
    rp = [ar.alloc([7168], BF16) for _ in range(2)]
    rp_b = [Buf("rp%d" % i) for i in range(2)]
    idt = [ar.alloc([128], BF16) for _ in range(4)]
    idt_b = [Buf("idtR%d" % i) for i in range(4)]
    y = [ar.alloc([512], F32) for _ in range(4)]
    y_b = [Buf("yR%d" % i) for i in range(4)]
    junk2 = ar.alloc([512], BF16)
    junk2_b = Buf("junk2R")
    G = [ar.alloc([512], F32) for _ in range(2)]
    G_b = [Buf("GR%d" % i) for i in range(2)]
    A = [ar.alloc([512], F32) for _ in range(2)]
    A_b = [Buf("AR%d" % i) for i in range(2)]
    yg = ar.alloc([4, 512], BF16)
    yg_b = [Buf("ygR%d" % i) for i in range(4)]
    ygT = [ar.alloc([16, 128], BF16) for _ in range(2)]
    ygT_b = [Buf("ygTR%d" % i) for i in range(2)]
    gs = [ar.alloc([32], F32) for _ in range(2)]
    gs_b = [Buf("gsR%d" % i) for i in range(2)]

    def r1_load(t):
        p.dma("sp", rp[t % 2], rp_d[t], reads=[rpd_b[t]], writes=[rp_b[t % 2]])

    def r1_tile(t):
        s = t % 2
        if t + 1 < NT:
            r1_load(t + 1)
        r_ = rp[s]
        rb = rp_b[s]
        qT = r_[:, OQ:OQ + 1024].rearrange("p (a b) -> p a b", a=8)
        kT = r_[:, OK_:OK_ + 1024].rearrange("p (a b) -> p a b", a=8)
        kz = r_[:, OKZ:OKZ + 1024].rearrange("p (a b) -> p a b", a=4)
        v = r_[:, OV:OV + 2048].rearrange("p (a b) -> p a b", a=4)
        sg = r_[:, OSG:OSG + 2048].rearrange("p (a b) -> p a b", a=4)
        g_, gb = gs[s], gs_b[s]
        for h in range(4):
            pin = bank(6)[:, h * 128:(h + 1) * 128]
            for dc in range(2):
                p.op("pe", lambda e, pin=pin, h=h, dc=dc: e.matmul(pin, lhsT=kT[:, 2 * h + dc, :], rhs=qT[:, 2 * h + dc, :],
                                                                   start=(dc == 0), stop=(dc == 1)),
                     reads=[rb], writes=[pb[6]])
        for h in range(4):
            pin = bank(6)[:, h * 128:(h + 1) * 128]
            p.op("dve", lambda e, pin=pin, h=h: e.tensor_tensor(out=idt[h], in0=pin, in1=decT[:, h, :], op=ALU.mult),
                 reads=[pb[6], c.K_b], writes=[idt_b[h]])
        for h in range(4):
            bo = 7 if h % 2 == 0 else 5
            po = bank(bo)
            p.op("pe", lambda e, po=po, h=h: e.matmul(po, lhsT=idt[h], rhs=v[:, h, :], start=True, stop=False),
                 reads=[idt_b[h], rb], writes=[pb[bo]])
            for dc in range(2):
                p.op("pe", lambda e, po=po, h=h, dc=dc: e.matmul(po, lhsT=qT[:, 2 * h + dc, :], rhs=state_bf[:, h, dc, :],
                                                                 start=False, stop=(dc == 1)),
                     reads=[rb, stbf_b[h][dc]], writes=[pb[bo]])
            p.op("act", lambda e, po=po, h=h: e.activation(out=y[h], in_=po, func=AF.Identity, scale=xi[:, h:h + 1],
                                                           accum_out=g_[:, h:h + 1]),
                 reads=[pb[bo], c.K_b], writes=[y_b[h], gb])
            p.op("act", lambda e, h=h: e.activation(out=junk2, in_=y[h], func=AF.Square, accum_out=g_[:, 4 + h:5 + h]),
                 reads=[y_b[h]], writes=[junk2_b, gb])
        for h in range(4):
            for dc in range(2):
                bu = 4 if dc == 0 else 3
                pu = bank(bu)
                p.op("pe", lambda e, pu=pu, h=h, dc=dc: e.matmul(pu, lhsT=kz[:, h, dc * 128:(dc + 1) * 128], rhs=v[:, h, :], start=True, stop=True),
                     reads=[rb], writes=[pb[bu]])
                p.op("dve", lambda e, pu=pu, h=h, dc=dc: e.scalar_tensor_tensor(out=state[:, h, dc, :], in0=state[:, h, dc, :], scalar=gam_c[h],
                                                                                in1=pu, op0=ALU.mult, op1=ALU.add),
                     reads=[st_b[h][dc], pb[bu]], writes=[st_b[h][dc]])
                p.op("act", lambda e, h=h, dc=dc: e.copy(out=state_bf[:, h, dc, :], in_=state[:, h, dc, :]),
                     reads=[st_b[h][dc]], writes=[stbf_b[h][dc]])
        p.op("dve", lambda e: e.tensor_scalar(out=g_[:, 8:12], in0=g_[:, 0:4], scalar1=1.0 / 512, scalar2=None, op0=ALU.mult), reads=[gb], writes=[gb])
        p.op("dve", lambda e: e.tensor_tensor(out=g_[:, 24:28], in0=g_[:, 8:12], in1=g_[:, 8:12], op=ALU.mult), reads=[gb], writes=[gb])
        p.op("dve", lambda e: e.scalar_tensor_tensor(out=g_[:, 12:16], in0=g_[:, 4:8], scalar=1.0 / 512, in1=g_[:, 24:28],
                                                     op0=ALU.mult, op1=ALU.subtract), reads=[gb], writes=[gb])
        p.op("dve", lambda e: e.tensor_scalar(out=g_[:, 12:16], in0=g_[:, 12:16], scalar1=RMS_EPS, scalar2=None, op0=ALU.add), reads=[gb], writes=[gb])
        p.op("act", lambda e: e.activation(out=g_[:, 12:16], in_=g_[:, 12:16], func=AF.Ln), reads=[gb], writes=[gb])
        p.op("act", lambda e: e.activation(out=g_[:, 16:20], in_=g_[:, 12:16], func=AF.Exp, scale=-0.5), reads=[gb], writes=[gb])
        p.op("dve", lambda e: e.scalar_tensor_tensor(out=g_[:, 20:24], in0=g_[:, 8:12], scalar=-1.0, in1=g_[:, 16:20],
                                                     op0=ALU.mult, op1=ALU.mult), reads=[gb], writes=[gb])
        for h in range(4):
            Gh, Ghb = G[h % 2], G_b[h % 2]
            Ah, Ahb = A[h % 2], A_b[h % 2]
            p.op("dve", lambda e, h=h, Gh=Gh: e.tensor_tensor(out=Gh, in0=gng[:, h * 512:(h + 1) * 512], in1=sg[:, h, :], op=ALU.mult),
                 reads=[gng_b, rb], writes=[Ghb])
            p.op("dve", lambda e, h=h, Ah=Ah: e.tensor_scalar(out=Ah, in0=y[h], scalar1=g_[:, 16 + h:17 + h], scalar2=g_[:, 20 + h:21 + h],
                                                              op0=ALU.mult, op1=ALU.add),
                 reads=[y_b[h], gb], writes=[Ahb])
            p.op("dve", lambda e, h=h, Ah=Ah, Gh=Gh: e.tensor_tensor(out=yg[:, h, :], in0=Ah, in1=Gh, op=ALU.mult), reads=[Ahb, Ghb], writes=[yg_b[h]])
        ygf = yg.rearrange("p h d -> p (h d)")
        yT = ygT[s]
        for half in range(2):
            for j in range(8):
                jj = half * 8 + j
                p.op("pe", lambda e, jj=jj, j=j, half=half: e.transpose(out=bankbf(half)[:, j, :], in_=ygf[:, jj * 128:(jj + 1) * 128], identity=c.ident),
                     reads=[yg_b[jj // 4], c.ident_b], writes=[pb[half]])
            p.op("act", lambda e, yT=yT, half=half: e.copy(out=yT[:, half * 8:(half + 1) * 8, :], in_=bankbf(half)),
                 reads=[pb[half]], writes=[ygT_b[s]])
        p.dma("sp", ygT_d[t], yT, reads=[ygT_b[s]], writes=[ygd_b[t]])

    r1_load(0)
    for t in range(NT):
        r1_tile(t)
    p.barrier()
    ar.pop()

    ar.push()
    wout = ar.alloc([16, D], BF16)
    wout_b = Buf("woutR")
    load_weight_bf16(c, c.w_out_b, wout, wout_b, 2048, D)
    yl = [ar.alloc([16, 128], BF16) for _ in range(2)]
    yl_b = [Buf("ylR%d" % i) for i in range(2)]
    xo = [ar.alloc([D], F32) for _ in range(2)]
    xo_b = [Buf("xoR%d" % i) for i in range(2)]

    def r2_loads(t):
        p.dma("sp", yl[t % 2], ygT_d[t], reads=[ygd_b[t]], writes=[yl_b[t % 2]])
        p.dma("sp", xo[t % 2], h_in[t * 128:(t + 1) * 128, :], writes=[xo_b[t % 2]])
    for t in range(NT):
        s = t % 2
        if t == 0:
            r2_loads(0)
        if t + 1 < NT:
            r2_loads(t + 1)
        for half in range(2):
            ps = bank(2 * s + half)
            for j in range(16):
                p.op("pe", lambda e, ps=ps, j=j, half=half, s=s: e.matmul(ps, lhsT=yl[s][:, j, :], rhs=wout[:, j, half * 512:(half + 1) * 512],
                                                                          start=(j == 0), stop=(j == 15)),
                     reads=[yl_b[s], wout_b], writes=[pb[2 * s + half]])
            p.op("dve", lambda e, ps=ps, half=half, s=s: e.tensor_tensor(out=xo[s][:, half * 512:(half + 1) * 512], in0=ps,
                                                                         in1=xo[s][:, half * 512:(half + 1) * 512], op=ALU.add),
                 reads=[pb[2 * s + half], xo_b[s]], writes=[xo_b[s]])
        p.dma("sp", h_out[t * 128:(t + 1) * 128, :], xo[s], reads=[xo_b[s]], writes=[c.hres_b[t]])
    p.barrier()
    ar.pop()


def dump_phase(c):
    p, ar = c.p, c.ar
    ar.push()
    xt = [ar.alloc([D], F32) for _ in range(2)]
    xb = [Buf("dump%d" % i) for i in range(2)]
    for t in range(c.S // 128):
        p.dma("sp", xt[t % 2], c.hres[t * 128:(t + 1) * 128, :], writes=[xb[t % 2]])
        p.dma("sp", c.out[t * 128:(t + 1) * 128, :], xt[t % 2], reads=[xb[t % 2]], is_out=True)
    p.barrier()
    ar.pop()


NCONST = 368 + 648


def make_consts():
    k = np.zeros((128, NCONST), np.float32)
    k[:, 0:128] = np.eye(128, dtype=np.float32)
    q = np.arange(128)[:, None]
    kk = np.arange(128)[None, :]
    k[:, 128:256] = np.where(kk <= q, 0.0, -1.0e30).astype(np.float32)
    inv_a = (np.float32(ROPE_THETA) ** (-np.arange(16, dtype=np.float32) / np.float32(16))).astype(np.float32)
    inv_i = (np.float32(ROPE_THETA) ** (-np.arange(8, dtype=np.float32) / np.float32(8))).astype(np.float32)
    k[:, 256:304] = (np.concatenate([inv_a, inv_a, inv_i, inv_i]).astype(np.float64) / (2 * math.pi)).astype(np.float32)[None, :]
    k[:, 304:352] = np.concatenate([np.full(16, 0.0), np.full(16, 0.25), np.full(8, 0.0),
                                    np.full(8, 0.25)]).astype(np.float32)[None, :]
    k[:, 352:368] = (2.0 ** -(np.arange(16) + 1.0)).astype(np.float32)[None, :]
    RC = 368
    m = np.arange(128, dtype=np.float64)
    for h in range(4):
        lgm = math.log1p(-2.0 ** (-5.0 - h))
        dec = np.where(m[None, :] >= m[:, None], np.exp(-(m[:, None] + 1.0) * lgm), 0.0)
        k[:, RC + h * 128:RC + (h + 1) * 128] = dec.astype(np.float32)
        k[:, RC + 512 + h] = np.exp((m + 1.0) * lgm).astype(np.float32)
        k[:, RC + 516 + h] = np.exp((127.0 - m) * lgm).astype(np.float32)
    inv_r = (np.float32(RET_THETA) ** (-np.arange(128, dtype=np.float32) / np.float32(128))).astype(np.float64)
    k[:, RC + 520:RC + 648] = (inv_r / (2 * math.pi)).astype(np.float32)[None, :]
    return k


def build(S, phases=("mlp0",), debug=False):
    nc = bass.Bass("TRN2", target_bir_lowering=False)
    from contextlib import ExitStack
    es = ExitStack()
    c = Ctx()
    c.nc, c.S = nc, S
    c.p = Prog()

    def din(name, shape, dt=F32):
        return nc.dram_tensor(name, shape, dt, kind="ExternalInput").ap()

    c.x = din("x", [S, D])
    c.pos = din("positions", [128, S // 128], I32)
    c.norm_mix_g = din("norm_mix_g", [2, D])
    c.norm_mlp_g = din("norm_mlp_g", [2, D])
    c.w_in_a = din("w_in_a", [D, A_IN])
    c.w_out_a = din("w_out_a", [D, D])
    c.w_in_b = din("w_in_b", [D, B_IN])
    c.ret_norm_g = din("ret_norm_g", [1, 2048])
    c.w_out_b = din("w_out_b", [2048, D])
    c.w_mlp_up = din("w_mlp_up", [2, D, DFF])
    c.w_mlp_down = din("w_mlp_down", [2, DFF, D])
    c.final_norm_g = din("final_norm_g", [1, D])
    c.out = nc.dram_tensor("out", [S, D], F32, kind="ExternalOutput").ap()
    c.hres = nc.dram_tensor("hres", [S, D], F32, kind="Internal").ap()
    c.dbg = nc.dram_tensor("dbg", [S, 32], F32, kind="ExternalOutput").ap() if debug else None
    c.dbgs = nc.dram_tensor("dbgs", [S // 128, 128, S], F32, kind="ExternalOutput").ap() if debug else None
    c.dbgt = nc.dram_tensor("dbgt", [128, (S // 128) * 49], F32, kind="ExternalOutput").ap() if debug else None

    NT = S // 128
    c.qT_d = nc.dram_tensor("qT_d", [NT, 128, 1024], BF16, kind="Internal").ap().rearrange("t p (h q) -> t p h q", h=8)
    c.qiT_d = nc.dram_tensor("qiT_d", [NT, 128, 512], BF16, kind="Internal").ap().rearrange("t p (h q) -> t p h q", h=4)
    c.hres_b = [Buf("hres%d" % t) for t in range(NT)]
    c.mb_d = nc.dram_tensor("mb_d", [NT, 128, S], BF16, kind="Internal").ap()
    c.rp_d = nc.dram_tensor("rp_d", [NT, 128, 7168], BF16, kind="Internal").ap()
    c.ygT_d = nc.dram_tensor("ygT_d", [NT, 128, 2048], BF16, kind="Internal").ap().rearrange("t p (h q) -> t p h q", h=16)
    c.consts_in = din("consts", [128, NCONST])

    c.ar = Arena(nc, es, 206 * 1024)
    c.ps_all = es.enter_context(nc.psum_tensor("ps_all", [128, 4096], F32))[:, :]
    c.psum = [c.ps_all[:, i * 512:(i + 1) * 512] for i in range(8)]
    c.psum_b = [Buf("ps%d" % i) for i in range(8)]
    c.stage = [c.ar.alloc([1024], F32) for _ in range(2)]
    c.stage_b = [Buf("stage%d" % i) for i in range(2)]
    c.stage_i = 0
    kf = c.ar.alloc([NCONST], F32)
    c.K_b = Buf("K")
    c.p.dma("sp", kf, c.consts_in, writes=[c.K_b])
    c.ident = c.ar.alloc([128], BF16)
    c.ident_b = Buf("ident")
    c.p.op("dve", lambda e: e.tensor_copy(out=c.ident, in_=kf[:, 0:128]), reads=[c.K_b], writes=[c.ident_b])
    I4 = c.ar.alloc([4, 128], BF16)
    c.p.op("dve", lambda e: e.tensor_copy(out=I4, in_=kf[:, 0:128].unsqueeze(1).broadcast_to([128, 4, 128])),
           reads=[c.K_b], writes=[c.ident_b])
    ones = c.ar.alloc([128], BF16)
    c.p.op("dve", lambda e: e.memset(ones, 1.0), writes=[c.ident_b])
    c.K = {"causal": kf[:, 128:256], "invrow": kf[:, 256:304], "offs": kf[:, 304:352], "pow2": kf[:, 352:368],
           "I4": I4.rearrange("p a b -> p (a b)"), "ones": ones, "f": kf}
    c.K_b = c.ident_b

    for ph in phases:
        if ph == "mlp0":
            mlp_phase(c, 0, c.x, c.hres)
        elif ph == "mlp0f":
            mlp_phase(c, 0, c.x, None, final_g=c.final_norm_g, out_dram=c.out)
        elif ph == "dsa":
            dsa_phase(c, c.x, c.hres)
        elif ph == "mlp0h":
            mlp_phase(c, 0, c.hres, c.hres)
        elif ph == "reth":
            ret_phase(c, c.hres, c.hres)
        elif ph == "mlp1f":
            mlp_phase(c, 1, c.hres, None, final_g=c.final_norm_g, out_dram=c.out)
        elif ph == "ret":
            ret_phase(c, c.x, c.hres)
        elif ph == "dump":
            dump_phase(c)
    c.p.finish()
    c.p.emit(nc, es)
    es.close()
    return nc


FULL_PHASES = ("dsa", "mlp0h", "reth", "mlp1f")


def kernel(x, positions, norm_mix_g, norm_mlp_g, w_in_a, w_out_a, w_in_b, ret_norm_g, w_out_b,
           w_mlp_up, w_mlp_down, final_norm_g):
    f = lambda a: np.ascontiguousarray(np.asarray(a, dtype=np.float32))
    x = f(x)
    positions = np.asarray(positions).astype(np.int32)
    B, S, _ = x.shape
    nc = build(S, phases=FULL_PHASES)
    common = dict(
        norm_mix_g=f(norm_mix_g), norm_mlp_g=f(norm_mlp_g),
        w_in_a=f(np.asarray(w_in_a)[0]), w_out_a=f(np.asarray(w_out_a)[0]),
        w_in_b=f(np.asarray(w_in_b)[0]), ret_norm_g=f(np.asarray(ret_norm_g)).reshape(1, 2048),
        w_out_b=f(np.asarray(w_out_b)[0]), w_mlp_up=f(w_mlp_up), w_mlp_down=f(w_mlp_down),
        final_norm_g=f(final_norm_g).reshape(1, D), consts=make_consts())
    in_maps = []
    for b in range(B):
        m = dict(common)
        m["x"] = np.ascontiguousarray(x[b])
        m["positions"] = np.ascontiguousarray(positions[b].reshape(S // 128, 128).T)
        in_maps.append(m)
    res = run_bass_kernel_spmd(nc, in_maps, core_ids=list(range(B)))
    return np.stack([np.asarray(r["out"], dtype=np.float32) for r in res.results], axis=0)
```

```python
import math
import numpy as np
import concourse.bass as bass
import concourse.mybir as mybir
from concourse.bass_utils import run_bass_kernel_spmd

F32 = mybir.dt.float32
BF16 = mybir.dt.bfloat16
I32 = mybir.dt.int32
AF = mybir.ActivationFunctionType
ALU = mybir.AluOpType
AX = mybir.AxisListType

D = 1024
NCORES = 8
RMS_EPS = 1e-6
DFF = 4096
H_A, DH_A, ROT_A = 8, 128, 32
H_IDX, D_IDX, ROT_IDX = 8, 64, 16
A_IN = 1864
ROPE_THETA = 500000.0
H_R, DK_R, DV_R = 4, 256, 512
B_IN = 6144
RET_THETA = 10000.0
NEG = -30000.0
BIG = 3.0e38


class Buf:
    __slots__ = ("name", "w", "rs")

    def __init__(self, name):
        self.name = name
        self.w = None
        self.rs = []


class Prog:
    ENG = ("pe", "act", "dve", "pool", "sp")
    NDSEM = 8

    def __init__(self):
        self.st = {e: [] for e in self.ENG}
        self.seen = {e: {} for e in self.ENG}
        self.dq = {e: 0 for e in ("sp", "act", "pool")}
        self.dval = {}
        self.out_events = []

    def _deps(self, eng, reads, writes):
        deps = []
        for b in reads:
            if b.w is not None:
                deps.append(b.w)
        for b in writes:
            if b.w is not None:
                ev = b.w
                if not (ev[0] == "e" and ev[1] == eng):
                    deps.append(ev)
            for ev in b.rs:
                if not (ev[0] == "e" and ev[1] == eng):
                    deps.append(ev)
        return deps

    def _filter(self, eng, deps):
        waits = {}
        for ev in deps:
            if ev[0] == "e":
                if ev[1] == eng and eng == "pe":
                    continue
                key = ("e", ev[1])
            else:
                key = ("d", ev[1])
            if self.seen[eng].get(key, -1) >= ev[2]:
                continue
            if waits.get(key, -1) < ev[2]:
                waits[key] = ev[2]
        for key, v in waits.items():
            self.seen[eng][key] = v
            if key[0] == "e":
                self.st[key[1]][v]["mark"] = True
        return list(waits.items())

    def _commit(self, ev, reads, writes):
        for b in reads:
            b.rs.append(ev)
        for b in writes:
            b.w = ev
            b.rs = []

    def op(self, eng, fn, reads=(), writes=()):
        deps = self._deps(eng, reads, writes)
        waits = self._filter(eng, deps)
        idx = len(self.st[eng])
        self.st[eng].append({"fn": fn, "waits": waits, "mark": False, "dma": None})
        ev = ("e", eng, idx)
        self._commit(ev, reads, writes)
        return ev

    def dma(self, q, out, in_, reads=(), writes=(), is_out=False, **kw):
        k = self.dq[q] % self.NDSEM
        self.dq[q] += 1
        key = (q, k)
        prev = self.dval.get(key, 0)
        deps = self._deps(None, reads, writes)
        if prev > 0:
            deps.append(("d", key, prev))
        waits = self._filter(q, deps)
        val = prev + 16
        self.dval[key] = val
        idx = len(self.st[q])
        self.st[q].append({"fn": lambda e, o=out, i=in_: e.dma_start(out=o, in_=i, **kw),
                           "waits": waits, "mark": False, "dma": (key, 16)})
        ev = ("d", key, val)
        self._commit(ev, reads, writes)
        if is_out:
            self.out_events.append(ev)
        return ev

    def barrier(self):
        lasts = []
        for e in self.ENG:
            for i in range(len(self.st[e]) - 1, -1, -1):
                r = self.st[e][i]
                if r["fn"] is not None and r["dma"] is None:
                    lasts.append(("e", e, i))
                    break
        for key, v in self.dval.items():
            lasts.append(("d", key, v))
        for e in self.ENG:
            deps = [ev for ev in lasts if not (ev[0] == "e" and ev[1] == e)]
            waits = self._filter(e, deps)
            if waits:
                self.st[e].append({"fn": None, "waits": waits, "mark": False, "dma": None})

    def finish(self):
        waits = self._filter("sp", list(self.out_events))
        self.st["sp"].append({"fn": None, "waits": waits, "mark": False, "dma": None})

    def emit(self, nc, es):
        esem = {e: es.enter_context(nc.semaphore("s_" + e)) for e in self.ENG}
        dsem = {}
        for q in ("sp", "act", "pool"):
            for k in range(self.NDSEM):
                dsem[(q, k)] = es.enter_context(nc.semaphore("d_%s%d" % (q, k)))
        cum = {}
        for e in self.ENG:
            c = 0
            arr = []
            for r in self.st[e]:
                if r["mark"]:
                    c += 1
                arr.append(c)
            cum[e] = arr
        block = es.enter_context(nc.Block())

        def run(eng_name):
            def body(engine):
                for r in self.st[eng_name]:
                    for key, v in r["waits"]:
                        if key[0] == "e":
                            engine.wait_ge(esem[key[1]], cum[key[1]][v])
                        else:
                            engine.wait_ge(dsem[key[1]], v)
                    if r["fn"] is None:
                        continue
                    ins = r["fn"](engine)
                    if r["dma"] is not None:
                        ins.then_inc(dsem[r["dma"][0]], 16)
                        assert not r["mark"]
                    elif r["mark"]:
                        ins.then_inc(esem[eng_name], 1)
            return body

        block.tensor(run("pe"))
        block.scalar(run("act"))
        block.vector(run("dve"))
        block.gpsimd(run("pool"))
        block.sync(run("sp"))


class Arena:
    def __init__(self, nc, es, nbytes):
        self.t = es.enter_context(nc.sbuf_tensor("arena", [128, nbytes // 4], F32))
        self.nbytes = nbytes
        self.off = 0
        self.marks = []
        self.guard = nbytes

    def alloc_at(self, off, shape, dtype):
        esz = 2 if dtype == BF16 else 4
        n = int(np.prod(shape))
        nb = n * esz
        assert off % 64 == 0 and off + nb <= self.nbytes and self.off <= off, ("alloc_at", off, nb, self.off)
        self.guard = min(self.guard, off)
        ap = self.t[:, off // 4:(off + nb) // 4]
        if dtype != F32:
            ap = ap.bitcast(dtype)
        if len(shape) == 2:
            ap = ap.rearrange("p (a b) -> p a b", a=shape[0])
        return ap

    def release_guard(self):
        self.guard = self.nbytes

    def alloc(self, shape, dtype):
        esz = 2 if dtype == BF16 else 4
        n = int(np.prod(shape))
        nb = (n * esz + 63) // 64 * 64
        assert self.off + nb <= min(self.nbytes, self.guard), ("SBUF arena overflow", self.off, nb, self.nbytes, self.guard)
        a = self.off // 4
        ap = self.t[:, a:a + nb // 4]
        if dtype != F32:
            ap = ap.bitcast(dtype)
        ap = ap[:, 0:n]
        self.off += nb
        if len(shape) == 2:
            ap = ap.rearrange("p (a b) -> p a b", a=shape[0])
        elif len(shape) == 3:
            ap = ap.rearrange("p (a b c) -> p a b c", a=shape[0], b=shape[1])
        return ap

    def push(self):
        self.marks.append(self.off)

    def pop(self):
        self.off = self.marks.pop()


class Ctx:
    pass


def load_weight_bf16(c, w_dram, dst, dst_buf, K, C, col0=0, ncols=None, splits=2):
    ncols = C if ncols is None else ncols
    kc = K // 128
    src = w_dram.rearrange("(k p) c -> p k c", p=128)
    p = c.p
    CH = 1024
    if ncols >= CH:
        pieces = [(k, 1, c0, min(CH, ncols - c0)) for k in range(kc) for c0 in range(0, ncols, CH)]
    else:
        kk = max(1, CH // ncols)
        pieces = [(k, min(kk, kc - k), 0, ncols) for k in range(0, kc, kk)]
    engs = ("dve", "act", "dve", "act", "dve", "act", "pool")
    for (k, nk, c0, w) in pieces:
        i = c.stage_i
        c.stage_i += 1
        st = c.stage[i % len(c.stage)]
        sb = c.stage_b[i % len(c.stage)]
        stv = st[:, 0:nk * w].rearrange("p (a b) -> p a b", a=nk)
        p.dma("sp" if i % 2 == 0 else "act", stv, src[:, k:k + nk, col0 + c0:col0 + c0 + w], writes=[sb])
        eng = engs[i % len(engs)]
        if eng == "act":
            p.op("act", lambda e, stv=stv, k=k, nk=nk, c0=c0, w=w: e.copy(out=dst[:, k:k + nk, c0:c0 + w], in_=stv),
                 reads=[sb], writes=[dst_buf])
        else:
            p.op(eng, lambda e, stv=stv, k=k, nk=nk, c0=c0, w=w: e.tensor_copy(out=dst[:, k:k + nk, c0:c0 + w], in_=stv),
                 reads=[sb], writes=[dst_buf])


def prefetch_pieces(c, w_dram, dst, dst_buf, K, ncols, stage, stage_b, queue, eng):
    kc = K // 128
    src = w_dram.rearrange("(k p) c -> p k c", p=128)
    p = c.p
    CH = 1024
    pieces = [(k, c0, min(CH, ncols - c0)) for c0 in range(0, ncols, CH) for k in range(kc)]
    out = []
    for n, (k, c0, w) in enumerate(pieces):
        def piece(n=n, k=k, c0=c0, w=w):
            st, sb = stage[n % len(stage)], stage_b[n % len(stage)]
            p.dma(queue, st[:, 0:w], src[:, k, c0:c0 + w], writes=[sb])
            if eng == "act":
                p.op("act", lambda e: e.copy(out=dst[:, k, c0:c0 + w], in_=st[:, 0:w]), reads=[sb], writes=[dst_buf])
            else:
                p.op(eng, lambda e: e.tensor_copy(out=dst[:, k, c0:c0 + w], in_=st[:, 0:w]), reads=[sb], writes=[dst_buf])
        out.append(piece)
    return out


def rstd_ops(c, stat, stat_buf, scale, n=1):
    p = c.p
    p.op("dve", lambda e: e.tensor_scalar(out=stat[:, n:2 * n], in0=stat[:, 0:n], scalar1=scale, scalar2=RMS_EPS,
                                          op0=ALU.mult, op1=ALU.add),
         reads=[stat_buf], writes=[stat_buf])
    p.op("act", lambda e: e.activation(out=stat[:, n:2 * n], in_=stat[:, n:2 * n], func=AF.Ln),
         reads=[stat_buf], writes=[stat_buf])
    p.op("act", lambda e: e.activation(out=stat[:, 2 * n:3 * n], in_=stat[:, n:2 * n], func=AF.Exp, scale=-0.5),
         reads=[stat_buf], writes=[stat_buf])


def rmsnorm_tile(c, xt, xt_buf, g_bc, g_buf, hn, hn_buf, sq_junk, junk_buf, stat, stat_buf):
    p = c.p
    p.op("act", lambda e: e.activation(out=sq_junk, in_=xt, func=AF.Square, accum_out=stat[:, 0:1]),
         reads=[xt_buf], writes=[junk_buf, stat_buf])
    rstd_ops(c, stat, stat_buf, 1.0 / D)
    p.op("dve", lambda e: e.scalar_tensor_tensor(out=hn, in0=xt, scalar=stat[:, 2:3], in1=g_bc,
                                                 op0=ALU.mult, op1=ALU.mult),
         reads=[xt_buf, stat_buf, g_buf], writes=[hn_buf])


def transpose_to(c, src, src_buf, nchunks, ps, ps_buf, dst, dst_buf, evac="act"):
    p = c.p
    for k in range(nchunks):
        p.op("pe", lambda e, k=k: e.transpose(out=ps[:, k, :], in_=src[:, k * 128:(k + 1) * 128], identity=c.ident),
             reads=[src_buf, c.ident_b], writes=[ps_buf])
    if evac == "act":
        p.op("act", lambda e: e.copy(out=dst, in_=ps[:, 0:nchunks, :]), reads=[ps_buf], writes=[dst_buf])
    else:
        p.op(evac, lambda e: e.tensor_copy(out=dst, in_=ps[:, 0:nchunks, :]), reads=[ps_buf], writes=[dst_buf])


def mlp_phase(c, li, h_in, h_out, final_g=None, out_dram=None, pre_up=None, pre_dn=None):
    p, ar, nc = c.p, c.ar, c.nc
    S = c.S
    TB = 256
    NJ = TB // 128
    nblk = S // TB
    ar.push()
    if pre_up is not None:
        wup, wup_b = pre_up
    else:
        wup, wup_b = ar.alloc([8, DFF], BF16), Buf("wup")
    if pre_dn is not None:
        wdn, wdn_b = pre_dn
    else:
        wdn, wdn_b = ar.alloc([32, D], BF16), Buf("wdn")
    gbc = ar.alloc([D], F32)
    gbc_b = Buf("gbc")
    p.dma("sp", gbc, c.norm_mlp_g[li:li + 1, :].partition_broadcast(128), writes=[gbc_b])
    if pre_up is None:
        load_weight_bf16(c, c.w_mlp_up[li], wup, wup_b, D, DFF, splits=4)
    if pre_dn is None:
        load_weight_bf16(c, c.w_mlp_down[li], wdn, wdn_b, DFF, D, splits=4)
    if final_g is not None:
        fg = ar.alloc([D], F32)
        fg_b = Buf("fg")
        p.dma("sp", fg, final_g.partition_broadcast(128), writes=[fg_b])
    NX = 4
    xt = [ar.alloc([D], F32) for _ in range(NX)]
    xt_b = [Buf("xt%d" % i) for i in range(NX)]
    hn = [ar.alloc([D], BF16) for _ in range(2)]
    hn_b = [Buf("hn%d" % i) for i in range(2)]
    junk = ar.alloc([D], BF16)
    junk_b = Buf("junk")
    stat = [ar.alloc([4], F32) for _ in range(4)]
    stat_b = [Buf("stat%d" % i) for i in range(4)]
    hnT = [ar.alloc([8, TB], BF16) for _ in range(2)]
    hnT_b = [Buf("hnT%d" % i) for i in range(2)]
    actT = ar.alloc([32, TB], BF16)
    actT_b = [Buf("actT%d" % i) for i in range(32)]
    rl = [ar.alloc([TB], F32) for _ in range(2)]
    rl_b = [Buf("rl%d" % i) for i in range(2)]
    pst = [c.psum[i].bitcast(BF16) for i in (0, 1)]
    pst = [t.rearrange("p (a b) -> p a b", b=128) for t in pst]
    pst_b = [c.psum_b[0], c.psum_b[1]]
    xi = [0]

    def stage1(b):
        tiles = []
        for j in range(NJ):
            t = b * NJ + j
            s = xi[0] % NX
            xi[0] += 1
            p.dma("sp", xt[s], h_in[t * 128:(t + 1) * 128, :], writes=[xt_b[s]])
            hs = t % 2
            ss = t % 4
            rmsnorm_tile(c, xt[s], xt_b[s], gbc, gbc_b, hn[hs], hn_b[hs], junk, junk_b, stat[ss], stat_b[ss])
            transpose_to(c, hn[hs], hn_b[hs], 8, pst[t % 2], pst_b[t % 2],
                         hnT[b % 2][:, :, j * 128:(j + 1) * 128], hnT_b[b % 2])
            tiles.append(s)
        return tiles

    def stage2(b, tiles):
        hT = hnT[b % 2]
        for f in range(32):
            ps = c.psum[2 + f % 2]
            ps_b = c.psum_b[2 + f % 2]
            for k in range(8):
                p.op("pe", lambda e, ps=ps, k=k, f=f: e.matmul(ps[:, 0:TB], lhsT=wup[:, k, f * 128:(f + 1) * 128],
                                                               rhs=hT[:, k, :], start=(k == 0), stop=(k == 7)),
                     reads=[wup_b, hnT_b[b % 2]], writes=[ps_b])
            r = rl[f % 2]
            p.op("act", lambda e, ps=ps, r=r: e.activation(out=r, in_=ps[:, 0:TB], func=AF.Relu),
                 reads=[ps_b], writes=[rl_b[f % 2]])
            p.op("dve", lambda e, r=r, f=f: e.tensor_tensor(out=actT[:, f, :], in0=r, in1=r, op=ALU.mult),
                 reads=[rl_b[f % 2]], writes=[actT_b[f]])
        for j in range(NJ):
            t = b * NJ + j
            s = tiles[j]
            for half in range(2):
                ps = c.psum[4 + (2 * j + half) % 4]
                ps_b = c.psum_b[4 + (2 * j + half) % 4]
                for f in range(32):
                    p.op("pe", lambda e, ps=ps, f=f, j=j, half=half: e.matmul(
                        ps[:, 0:512], lhsT=actT[:, f, j * 128:(j + 1) * 128],
                        rhs=wdn[:, f, half * 512:(half + 1) * 512], start=(f == 0), stop=(f == 31)),
                         reads=[wdn_b, actT_b[f]], writes=[ps_b])
                p.op("dve", lambda e, ps=ps, s=s, half=half: e.tensor_tensor(
                    out=xt[s][:, half * 512:(half + 1) * 512], in0=ps[:, 0:512],
                    in1=xt[s][:, half * 512:(half + 1) * 512], op=ALU.add),
                     reads=[ps_b, xt_b[s]], writes=[xt_b[s]])
            if final_g is None:
                p.dma("sp", h_out[t * 128:(t + 1) * 128, :], xt[s], reads=[xt_b[s]])
            else:
                ss = t % 4
                p.op("act", lambda e, s=s, ss=ss: e.activation(out=junk, in_=xt[s], func=AF.Square,
                                                               accum_out=stat[ss][:, 0:1]),
                     reads=[xt_b[s]], writes=[junk_b, stat_b[ss]])
                rstd_ops(c, stat[ss], stat_b[ss], 1.0 / D)
                p.op("dve", lambda e, s=s, ss=ss: e.scalar_tensor_tensor(out=xt[s], in0=xt[s], scalar=stat[ss][:, 2:3],
                                                                         in1=fg, op0=ALU.mult, op1=ALU.mult),
                     reads=[xt_b[s], stat_b[ss], fg_b], writes=[xt_b[s]])
                p.dma("sp", out_dram[t * 128:(t + 1) * 128, :], xt[s], reads=[xt_b[s]], is_out=True)

    prev = None
    for b in range(nblk + 1):
        cur = stage1(b) if b < nblk else None
        if prev is not None:
            stage2(b - 1, prev)
        prev = cur
    p.barrier()
    ar.pop()
    ar.release_guard()


def sincos_from_turns(c, t, t_b, n, barrier=True):
    p, ar = c.p, c.ar
    if barrier:
        ar.push()
        ni = ar.alloc([n], I32)
        nf = ar.alloc([n], F32)
        tb = Buf("sc_tmp")
    else:
        if not hasattr(c, "sc_ni"):
            c.sc_ni = ar.alloc([n], I32)
            c.sc_nf = ar.alloc([n], F32)
            c.sc_tb = Buf("sc_tmpR")
        ni, nf, tb = c.sc_ni, c.sc_nf, c.sc_tb
    p.op("dve", lambda e: e.tensor_copy(out=ni, in_=t), reads=[t_b], writes=[tb])
    p.op("dve", lambda e: e.tensor_copy(out=nf, in_=ni), reads=[tb], writes=[tb])
    p.op("dve", lambda e: e.tensor_tensor(out=t, in0=t, in1=nf, op=ALU.subtract), reads=[t_b, tb], writes=[t_b])
    p.op("dve", lambda e: e.tensor_scalar(out=nf, in0=t, scalar1=0.5, scalar2=None, op0=ALU.is_gt), reads=[t_b], writes=[tb])
    p.op("dve", lambda e: e.tensor_tensor(out=t, in0=t, in1=nf, op=ALU.subtract), reads=[t_b, tb], writes=[t_b])
    p.op("act", lambda e: e.activation(out=t, in_=t, func=AF.Sin, scale=2 * math.pi * (1 - 1e-6)), reads=[t_b], writes=[t_b])
    if barrier:
        p.barrier()
        ar.pop()


def dsa_phase(c, h_in, h_out):
    p, ar = c.p, c.ar
    S = c.S
    NT = S // 128
    topk = min(256, S // 4)
    NIT = 14
    PS = c.ps_all

    def bank(i, n=1):
        return PS[:, i * 512:(i + n) * 512]

    def bankbf(i):
        return PS[:, i * 512:(i + 1) * 512].bitcast(BF16).rearrange("p (a b) -> p a b", b=128)
    pb = c.psum_b
    qT_d = c.qT_d
    qiT_d = c.qiT_d

    ar.push()
    kT = ar.alloc([S], BF16)
    V = ar.alloc([NT, 128], BF16)
    kiT2 = ar.alloc([S], BF16)
    wi = ar.alloc([NT, 8], F32)
    wout = ar.alloc([8, D], BF16)
    tab = ar.alloc([NT, 48], F32)
    posi = ar.alloc([NT], I32)
    posf = ar.alloc([NT], F32)
    gbc = ar.alloc([D], F32)
    kT_b = [Buf("kT%d" % t) for t in range(NT)]
    V_b = [Buf("V%d" % t) for t in range(NT)]
    ki_b = [Buf("ki%d" % t) for t in range(NT)]
    wi_b = [Buf("wi%d" % t) for t in range(NT)]
    qTd_b = [Buf("qTd%d" % t) for t in range(NT)]
    qiTd_b = [Buf("qiTd%d" % t) for t in range(NT)]
    wout_b, tab_b, pos_b, gbc_b = Buf("wout"), Buf("tab"), Buf("pos"), Buf("gbcA")
    K = c.K

    p.dma("sp", gbc, c.norm_mix_g[0:1, :].partition_broadcast(128), writes=[gbc_b])
    p.dma("sp", posi, c.pos, writes=[pos_b])
    p.op("dve", lambda e: e.tensor_copy(out=posf, in_=posi), reads=[pos_b], writes=[pos_b])
    p.op("dve", lambda e: e.tensor_tensor(out=tab, in0=posf.unsqueeze(2).broadcast_to([128, NT, 48]),
                                          in1=K["invrow"].unsqueeze(1).broadcast_to([128, NT, 48]), op=ALU.mult),
         reads=[pos_b, c.K_b], writes=[tab_b])
    p.op("dve", lambda e: e.tensor_tensor(out=tab, in0=tab, in1=K["offs"].unsqueeze(1).broadcast_to([128, NT, 48]),
                                          op=ALU.add), reads=[tab_b, c.K_b], writes=[tab_b])
    tabf = tab.rearrange("p a b -> p (a b)")
    if c.dbg is not None:
        p.dma("sp", c.dbgt[:, NT * 48:NT * 49], posf, reads=[pos_b], is_out=True)
    sincos_from_turns(c, tabf, tab_b, NT * 48)
    if c.dbg is not None:
        p.dma("sp", c.dbgt[:, 0:NT * 48], tabf, reads=[tab_b], is_out=True)

    ar.push()
    win = ar.alloc([8, A_IN], BF16)
    win_b = Buf("win")
    load_weight_bf16(c, c.w_in_a, win, win_b, D, A_IN)
    load_weight_bf16(c, c.w_out_a, wout, wout_b, D, D)
    xt = [ar.alloc([D], F32) for _ in range(2)]
    xt_b = [Buf("xtA%d" % i) for i in range(2)]
    hn = [ar.alloc([D], BF16) for _ in range(2)]
    hn_b = [Buf("hnA%d" % i) for i in range(2)]
    junk = ar.alloc([D], BF16)
    junk_b = Buf("junkA")
    stat = [ar.alloc([4], F32) for _ in range(2)]
    stat_b = [Buf("statA%d" % i) for i in range(2)]
    hnT = [ar.alloc([8, 128], BF16) for _ in range(2)]
    hnT_b = [Buf("hnTA%d" % i) for i in range(2)]
    proj = [ar.alloc([A_IN], F32) for _ in range(2)]
    proj_b = [[Buf("proj%d_%d" % (a, i)) for i in range(4)] for a in range(2)]
    qkb = ar.alloc([9, 128], BF16)
    qkb_b = Buf("qkb")
    ixb = ar.alloc([10, 64], BF16)
    ixb_b = Buf("ixb")
    tA = [ar.alloc([9, 16], F32) for _ in range(4)]
    tA_b = [Buf("tA%d" % i) for i in range(4)]
    tI = [ar.alloc([9, 8], F32) for _ in range(4)]
    tI_b = [Buf("tI%d" % i) for i in range(4)]
    qTt = [ar.alloc([8, 128], BF16) for _ in range(2)]
    qTt_b = [Buf("qTt%d" % i) for i in range(2)]
    qiTt = [ar.alloc([4, 128], BF16) for _ in range(2)]
    qiTt_b = [Buf("qiTt%d" % i) for i in range(2)]
    chunks = [(0, 512), (512, 512), (1024, 512), (1536, A_IN - 1536)]

    def a1_front(t):
        s = t % 2
        p.dma("sp", xt[s], h_in[t * 128:(t + 1) * 128, :], writes=[xt_b[s]])
        rmsnorm_tile(c, xt[s], xt_b[s], gbc, gbc_b, hn[s], hn_b[s], junk, junk_b, stat[s], stat_b[s])
        transpose_to(c, hn[s], hn_b[s], 8, bankbf(0), pb[0], hnT[s], hnT_b[s])
        for ci, (c0, w) in enumerate(chunks):
            ps = bank(1 + ci)
            for k in range(8):
                p.op("pe", lambda e, ps=ps, k=k, c0=c0, w=w: e.matmul(ps[:, 0:w], lhsT=hnT[s][:, k, :], rhs=win[:, k, c0:c0 + w],
                                                                      start=(k == 0), stop=(k == 7)),
                     reads=[hnT_b[s], win_b], writes=[pb[1 + ci]])
            p.op("act", lambda e, ps=ps, c0=c0, w=w: e.copy(out=proj[s][:, c0:c0 + w], in_=ps[:, 0:w]),
                 reads=[pb[1 + ci]], writes=[proj_b[s][ci]])

    def a1_back(t):
        s = t % 2
        pj = proj[s]

        def pbufs(lo, hi):
            return [proj_b[s][i] for i, (c0, w) in enumerate(chunks) if c0 < hi and c0 + w > lo]
        pa = pj[:, 0:1152].rearrange("p (h d) -> p h d", h=9)
        cosA = tab[:, t, 16:32].unsqueeze(1).broadcast_to([128, 9, 16])
        sinA = tab[:, t, 0:16].unsqueeze(1).broadcast_to([128, 9, 16])
        rA = pbufs(0, 1152)
        p.op("dve", lambda e: e.tensor_tensor(out=tA[0], in0=pa[:, :, 0:16], in1=cosA, op=ALU.mult), reads=rA + [tab_b], writes=[tA_b[0]])
        p.op("dve", lambda e: e.tensor_tensor(out=tA[1], in0=pa[:, :, 16:32], in1=sinA, op=ALU.mult), reads=rA + [tab_b], writes=[tA_b[1]])
        p.op("dve", lambda e: e.tensor_tensor(out=tA[2], in0=pa[:, :, 16:32], in1=cosA, op=ALU.mult), reads=rA + [tab_b], writes=[tA_b[2]])
        p.op("dve", lambda e: e.tensor_tensor(out=tA[3], in0=pa[:, :, 0:16], in1=sinA, op=ALU.mult), reads=rA + [tab_b], writes=[tA_b[3]])
        p.op("dve", lambda e: e.tensor_tensor(out=qkb[:, :, 0:16], in0=tA[0], in1=tA[1], op=ALU.subtract), reads=[tA_b[0], tA_b[1]], writes=[qkb_b])
        p.op("dve", lambda e: e.tensor_tensor(out=qkb[:, :, 16:32], in0=tA[2], in1=tA[3], op=ALU.add), reads=[tA_b[2], tA_b[3]], writes=[qkb_b])
        p.op("dve", lambda e: e.tensor_copy(out=qkb[:, :, 32:128], in_=pa[:, :, 32:128]), reads=rA, writes=[qkb_b])
        pi_ = pj[:, 1280:1856].rearrange("p (h d) -> p h d", h=9)
        cosI = tab[:, t, 40:48].unsqueeze(1).broadcast_to([128, 9, 8])
        sinI = tab[:, t, 32:40].unsqueeze(1).broadcast_to([128, 9, 8])
        rI = pbufs(1280, 1856)
        p.op("pool", lambda e: e.tensor_tensor(out=tI[0], in0=pi_[:, :, 0:8], in1=cosI, op=ALU.mult), reads=rI + [tab_b], writes=[tI_b[0]])
        p.op("pool", lambda e: e.tensor_tensor(out=tI[1], in0=pi_[:, :, 8:16], in1=sinI, op=ALU.mult), reads=rI + [tab_b], writes=[tI_b[1]])
        p.op("pool", lambda e: e.tensor_tensor(out=tI[2], in0=pi_[:, :, 8:16], in1=cosI, op=ALU.mult), reads=rI + [tab_b], writes=[tI_b[2]])
        p.op("pool", lambda e: e.tensor_tensor(out=tI[3], in0=pi_[:, :, 0:8], in1=sinI, op=ALU.mult), reads=rI + [tab_b], writes=[tI_b[3]])
        p.op("pool", lambda e: e.tensor_tensor(out=ixb[:, 0:9, 0:8], in0=tI[0], in1=tI[1], op=ALU.subtract), reads=[tI_b[0], tI_b[1]], writes=[ixb_b])
        p.op("pool", lambda e: e.tensor_tensor(out=ixb[:, 0:9, 8:16], in0=tI[2], in1=tI[3], op=ALU.add), reads=[tI_b[2], tI_b[3]], writes=[ixb_b])
        p.op("act", lambda e: e.copy(out=ixb[:, 0:9, 16:64], in_=pi_[:, :, 16:64]), reads=rI, writes=[ixb_b])
        p.op("act", lambda e: e.copy(out=ixb[:, 9, :], in_=ixb[:, 8, :]), reads=[ixb_b], writes=[ixb_b])
        p.op("act", lambda e: e.copy(out=V[:, t, :], in_=pj[:, 1152:1280]), reads=pbufs(1152, 1280), writes=[V_b[t]])
        p.op("act", lambda e: e.mul(out=wi[:, t, :], in_=pj[:, 1856:1864], mul=float(512 ** -0.5)), reads=pbufs(1856, 1864), writes=[wi_b[t]])
        qkf = qkb.rearrange("p h d -> p (h d)")
        ixf = ixb.rearrange("p h d -> p (h d)")
        for h in range(8):
            p.op("pe", lambda e, h=h: e.transpose(out=bankbf(5)[:, h, :], in_=qkf[:, h * 128:(h + 1) * 128], identity=c.ident),
                 reads=[qkb_b, c.ident_b], writes=[pb[5]])
        p.op("act", lambda e: e.copy(out=qTt[s], in_=bankbf(5)), reads=[pb[5]], writes=[qTt_b[s]])
        p.dma("sp", qT_d[t], qTt[s], reads=[qTt_b[s]], writes=[qTd_b[t]])
        p.op("pe", lambda e: e.transpose(out=bankbf(6)[:, 0, :], in_=qkf[:, 1024:1152], identity=c.ident),
             reads=[qkb_b, c.ident_b], writes=[pb[6]])
        for g in range(5):
            p.op("pe", lambda e, g=g: e.transpose(out=bankbf(6)[:, 1 + g, :], in_=ixf[:, g * 128:(g + 1) * 128], identity=c.ident),
                 reads=[ixb_b, c.ident_b], writes=[pb[6]])
        p.op("act", lambda e: e.copy(out=kT[:, t * 128:(t + 1) * 128], in_=bankbf(6)[:, 0, :]), reads=[pb[6]], writes=[kT_b[t]])
        p.op("act", lambda e: e.copy(out=qiTt[s], in_=bankbf(6)[:, 1:5, :]), reads=[pb[6]], writes=[qiTt_b[s]])
        p.op("act", lambda e: e.copy(out=kiT2[:, t * 128:(t + 1) * 128], in_=bankbf(6)[:, 5, :]), reads=[pb[6]], writes=[ki_b[t]])
        p.dma("sp", qiT_d[t], qiTt[s], reads=[qiTt_b[s]], writes=[qiTd_b[t]])

    a1_front(0)
    for t in range(NT):
        if t + 1 < NT:
            a1_front(t + 1)
        a1_back(t)
    p.barrier()
    ar.pop()

    mb_d = c.mb_d
    mbd_b = [Buf("mbd%d" % t) for t in range(NT)]
    ar.push()
    rh = [ar.alloc([8, 512], BF16) for _ in range(2)]
    rh_b = [[Buf("rh%d_%d" % (i, h)) for h in range(8)] for i in range(2)]
    diag = [ar.alloc([8, 128], BF16) for _ in range(2)]
    diag_b = [Buf("diag%d" % i) for i in range(2)]
    score = [ar.alloc([S], F32) for _ in range(3)]
    score_b = [Buf("score%d" % i) for i in range(3)]
    cjunk = ar.alloc([S], BF16)
    cjunk_b = Buf("cjunk")
    mbx = [ar.alloc([S], BF16) for _ in range(2)]
    mbx_b = [Buf("mbx%d" % i) for i in range(2)]
    qiTs = [ar.alloc([4, 128], BF16) for _ in range(2)]
    qiTs_b = [Buf("qiTs%d" % i) for i in range(2)]
    bs = [ar.alloc([8 + NIT], F32) for _ in range(3)]
    bs_b = [Buf("bs%d" % i) for i in range(3)]
    bs2 = [ar.alloc([4], F32) for _ in range(3)]
    bs2_b = [Buf("bs2_%d" % i) for i in range(3)]
    bs2a_b = [Buf("bs2a_%d" % i) for i in range(3)]
    ajunk = ar.alloc([S], BF16)
    ajunk_b = Buf("ajunk")
    cjunks = [cjunk, ar.alloc([S], BF16)]
    cjunks_b = [cjunk_b, Buf("cjunk1")]
    ajunks = [ajunk, ar.alloc([S], BF16)]
    ajunks_b = [ajunk_b, Buf("ajunk1")]

    def x_loads(i):
        p.dma("sp", qiTs[i % 2], qiT_d[i], reads=[qiTd_b[i]], writes=[qiTs_b[i % 2]])

    def bis_steps(i, n):
        s = i % 3
        sj = i % 2
        L = (i + 1) * 128
        b_, bb = bs[s], bs_b[s]
        LA = (L // 256) * 128
        sc = score[s]
        thr = float(topk) - 0.5 - 0.5 * LA
        steps = []
        steps.append(lambda: p.op("dve", lambda e: e.tensor_tensor(out=b_[:, 2:3], in0=b_[:, 0:1], in1=b_[:, 8 + n:9 + n], op=ALU.add),
                                  reads=[bb], writes=[bb]))

        def counts():
            if LA > 0:
                p.op("act", lambda e: e.activation(out=ajunks[sj][:, 0:LA], in_=sc[:, 0:LA], func=AF.Sign, bias=b_[:, 2:3], scale=-1.0,
                                                   accum_out=bs2[s][:, 1:2]), reads=[bb, score_b[s]], writes=[bs2a_b[s], ajunks_b[sj]])
            p.op("dve", lambda e: e.tensor_scalar(out=cjunks[sj][:, LA:L], in0=sc[:, LA:L], scalar1=b_[:, 2:3], scalar2=None,
                                                  op0=ALU.is_ge, op1=ALU.add, accum_out=b_[:, 3:4]),
                 reads=[bb, score_b[s]], writes=[bb, cjunks_b[sj]])
        steps.append(counts)
        if LA > 0:
            steps.append(lambda: p.op("dve", lambda e: e.scalar_tensor_tensor(out=b_[:, 3:4], in0=bs2[s][:, 1:2], scalar=-0.5, in1=b_[:, 3:4],
                                                                              op0=ALU.mult, op1=ALU.add), reads=[bb, bs2a_b[s]], writes=[bb]))
        steps.append(lambda: p.op("dve", lambda e: e.scalar_tensor_tensor(out=b_[:, 4:5], in0=b_[:, 3:4], scalar=thr,
                                                                          in1=b_[:, 8 + n:9 + n], op0=ALU.is_ge, op1=ALU.mult),
                                  reads=[bb], writes=[bb]))
        steps.append(lambda: p.op("dve", lambda e: e.tensor_tensor(out=b_[:, 0:1], in0=b_[:, 0:1], in1=b_[:, 4:5], op=ALU.add),
                                  reads=[bb], writes=[bb]))
        return steps

    def x_final(i):
        s = i % 3
        sm = i % 2
        L = (i + 1) * 128
        b_, bb = bs[s], bs_b[s]
        m_ = mbx[sm]
        sc = score[s][:, 0:L]
        p.op("dve", lambda e: e.tensor_scalar(out=m_[:, 0:L], in0=sc, scalar1=b_[:, 0:1], scalar2=NEG,
                                              op0=ALU.is_lt, op1=ALU.mult),
             reads=[score_b[s], bb], writes=[mbx_b[sm]])
        p.dma("sp", mb_d[i, :, 0:L], m_[:, 0:L], reads=[mbx_b[sm]], writes=[mbd_b[i]])

    def x_chunk(i, ch, nch):
        s = i % 3
        sq = i % 2
        L = (i + 1) * 128
        b_, bb = bs[s], bs_b[s]
        sco, scb = score[s], score_b[s]
        k0 = ch * 512
        w = min(512, L - k0)
        rr = rh[ch % 2]
        rrb = rh_b[ch % 2]
        kb = [ki_b[t] for t in range(k0 // 128, (k0 + w) // 128)]
        for h in range(8):
            hp, j = h % 2, h // 2
            bi = h % 2
            ps = bank(bi)
            p.op("pe", lambda e, ps=ps, hp=hp, j=j: e.matmul(
                ps[:, 0:w], lhsT=qiTs[sq][hp * 64:(hp + 1) * 64, j, :], rhs=kiT2[hp * 64:(hp + 1) * 64, k0:k0 + w],
                start=True, stop=True), reads=[qiTs_b[sq]] + kb, writes=[pb[bi]])
            p.op("act", lambda e, ps=ps, h=h: e.activation(out=rr[:, h, 0:w], in_=ps[:, 0:w], func=AF.Relu),
                 reads=[pb[bi]], writes=[rrb[h]])
        b2 = 2 + ch % 2
        ps2 = bank(b2)
        for h in range(8):
            p.op("pe", lambda e, h=h: e.matmul(ps2[:, 0:w], lhsT=diag[sq][:, h, :], rhs=rr[:, h, 0:w],
                                               start=(h == 0), stop=(h == 7)),
                 reads=[diag_b[sq], rrb[h]], writes=[pb[b2]])
        last = (ch == nch - 1)
        wc = w - 128 if last else w
        if wc > 0:
            p.op("dve", lambda e: e.tensor_copy(out=sco[:, k0:k0 + wc], in_=ps2[:, 0:wc]), reads=[pb[b2]], writes=[scb])
        if last:
            p.op("dve", lambda e: e.tensor_reduce(out=b_[:, 5:6], in_=ps2[:, w - 128:w], axis=AX.X, op=ALU.min),
                 reads=[pb[b2]], writes=[bb])
            p.op("dve", lambda e: e.tensor_tensor(out=sco[:, k0 + w - 128:k0 + w], in0=ps2[:, w - 128:w],
                                                  in1=K["causal"], op=ALU.add),
                 reads=[pb[b2], c.K_b], writes=[scb])

    def x_setup(i):
        s = i % 3
        L = (i + 1) * 128
        b_, bb = bs[s], bs_b[s]
        sco, scb = score[s], score_b[s]
        sc = sco[:, 0:L]
        p.op("dve", lambda e: e.tensor_reduce(out=b_[:, 1:2], in_=sc, axis=AX.X, op=ALU.max), reads=[scb], writes=[bb])
        if L > 128:
            p.op("dve", lambda e: e.tensor_reduce(out=b_[:, 0:1], in_=sco[:, 0:L - 128], axis=AX.X, op=ALU.min),
                 reads=[scb], writes=[bb])
            p.op("dve", lambda e: e.tensor_tensor(out=b_[:, 0:1], in0=b_[:, 0:1], in1=b_[:, 5:6], op=ALU.min), reads=[bb], writes=[bb])
        else:
            p.op("dve", lambda e: e.tensor_copy(out=b_[:, 0:1], in_=b_[:, 5:6]), reads=[bb], writes=[bb])
        p.op("dve", lambda e: e.tensor_tensor(out=b_[:, 1:2], in0=b_[:, 1:2], in1=b_[:, 0:1], op=ALU.subtract), reads=[bb], writes=[bb])
        p.op("dve", lambda e: e.tensor_tensor(out=b_[:, 8:8 + NIT], in0=b_[:, 1:2].broadcast_to([128, NIT]),
                                              in1=K["pow2"][:, 0:NIT], op=ALU.mult), reads=[bb, c.K_b], writes=[bb])

    x_loads(0)
    H1 = NIT // 2
    for i in range(NT + 2):
        if i + 1 < NT:
            x_loads(i + 1)
        its = []
        ia = list(range(0, H1)) if 1 <= i <= NT else []
        ib = list(range(H1, NIT)) if 2 <= i <= NT + 1 else []
        while ia or ib:
            sa = bis_steps(i - 1, ia.pop(0)) if ia else []
            sb = bis_steps(i - 2, ib.pop(0)) if ib else []
            while sa or sb:
                if sa:
                    its.append(sa.pop(0))
                if sb:
                    its.append(sb.pop(0))
        if i < NT:
            sq = i % 2
            p.op("pool", lambda e, sq=sq, i=i: e.tensor_tensor(out=diag[sq], in0=c.ident.unsqueeze(1).broadcast_to([128, 8, 128]),
                                                               in1=wi[:, i, :].unsqueeze(2).broadcast_to([128, 8, 128]), op=ALU.mult),
                 reads=[c.ident_b, wi_b[i]], writes=[diag_b[sq]])
            nch = ((i + 1) * 128 + 511) // 512
            per = (len(its) + nch - 1) // nch if its else 0
            for ch in range(nch):
                x_chunk(i, ch, nch)
                for _ in range(per):
                    if its:
                        its.pop(0)()
        while its:
            its.pop(0)()
        if 2 <= i <= NT + 1:
            x_final(i - 2)
        if i < NT:
            x_setup(i)
    p.barrier()
    ar.pop()

    ar.push()
    mb = [ar.alloc([S], BF16) for _ in range(2)]
    mb_b = [Buf("mb%d" % i) for i in range(2)]
    pT = [ar.alloc([4, 128], BF16) for _ in range(4)]
    pT_b = [Buf("pT%d" % i) for i in range(4)]
    recip = [ar.alloc([512], F32) for _ in range(2)]
    recip_b = [Buf("recip%d" % i) for i in range(2)]
    oT = [ar.alloc([8, 128], BF16) for _ in range(2)]
    oT_b = [[Buf("oT%d_%d" % (i, hh)) for hh in range(2)] for i in range(2)]
    qTs = [ar.alloc([8, 128], BF16) for _ in range(2)]
    qTs_b = [Buf("qTs%d" % i) for i in range(2)]
    xo = [ar.alloc([D], F32) for _ in range(3)]
    xo_b = [Buf("xo%d" % i) for i in range(3)]
    scale = float(DH_A ** -0.5)
    pti = [0]

    def y_loads(i):
        s = i % 2
        L = (i + 1) * 128
        p.dma("sp", qTs[s], qT_d[i], reads=[qTd_b[i]], writes=[qTs_b[s]])
        p.dma("sp", mb[s][:, 0:L], mb_d[i, :, 0:L], reads=[mbd_b[i]], writes=[mb_b[s]])
        p.dma("sp", xo[i % 3], h_in[i * 128:(i + 1) * 128, :], writes=[xo_b[i % 3]])

    def Yhalf(i, hh):
        s = i % 2
        m_ = mb[s]
        o_ = oT[s]
        bo, bl = (5, 6) if hh == 0 else (0, 1)
        pts = {}

        def qk(kt):
            bi = 3 + kt % 2
            ab = bank(bi)
            p.op("pe", lambda e: e.matmul(ab, lhsT=kT[:, kt * 128:(kt + 1) * 128],
                                          rhs=qTs[s][:, hh * 4:(hh + 1) * 4, :], start=True, stop=False),
                 reads=[kT_b[kt], qTs_b[s]], writes=[pb[bi]])
            p.op("pe", lambda e: e.matmul(ab, lhsT=m_[:, kt * 128:(kt + 1) * 128], rhs=K["I4"], start=False, stop=True),
                 reads=[mb_b[s], c.K_b], writes=[pb[bi]])
            pt = pT[pti[0] % 4]
            ptb = pT_b[pti[0] % 4]
            pti[0] += 1
            pts[kt] = (pt, ptb)
            p.op("act", lambda e: e.activation(out=pt.rearrange("p a b -> p (a b)"), in_=ab, func=AF.Exp, scale=scale),
                 reads=[pb[bi]], writes=[ptb])

        def pv(kt):
            pt, ptb = pts.pop(kt)
            p.op("pe", lambda e: e.matmul(bank(bo), lhsT=V[:, kt, :], rhs=pt.rearrange("p a b -> p (a b)"),
                                          start=(kt == 0), stop=(kt == i)),
                 reads=[V_b[kt], ptb], writes=[pb[bo]])
            p.op("pe", lambda e: e.matmul(bank(bl), lhsT=K["ones"], rhs=pt.rearrange("p a b -> p (a b)"),
                                          start=(kt == 0), stop=(kt == i)),
                 reads=[c.K_b, ptb], writes=[pb[bl]])

        qk(0)
        for kt in range(i + 1):
            if kt + 1 <= i:
                qk(kt + 1)
            pv(kt)
        rc, rcb = recip[hh], recip_b[hh]
        p.op("act", lambda e: e.activation(out=rc, in_=bank(bl), func=AF.Ln), reads=[pb[bl]], writes=[rcb])
        p.op("act", lambda e: e.activation(out=rc, in_=rc, func=AF.Exp, scale=-1.0), reads=[rcb], writes=[rcb])
        p.op("dve", lambda e: e.tensor_tensor(out=o_[:, hh * 4:(hh + 1) * 4, :].rearrange("p a b -> p (a b)"), in0=bank(bo),
                                              in1=rc, op=ALU.mult),
             reads=[pb[bo], rcb], writes=[oT_b[s][hh]])

    def Yout(i):
        s = i % 2
        o_ = oT[s]
        for half in range(2):
            bi = 2 if half == 0 else 7
            ps = bank(bi)
            for h in range(8):
                p.op("pe", lambda e, h=h, ps=ps, half=half: e.matmul(ps, lhsT=o_[:, h, :], rhs=wout[:, h, half * 512:(half + 1) * 512],
                                                                     start=(h == 0), stop=(h == 7)),
                     reads=[oT_b[s][h // 4], wout_b], writes=[pb[bi]])
            p.op("dve", lambda e, half=half, ps=ps: e.tensor_tensor(out=xo[i % 3][:, half * 512:(half + 1) * 512], in0=ps,
                                                             in1=xo[i % 3][:, half * 512:(half + 1) * 512], op=ALU.add),
                 reads=[pb[bi], xo_b[i % 3]], writes=[xo_b[i % 3]])
        p.dma("sp", h_out[i * 128:(i + 1) * 128, :], xo[i % 3], reads=[xo_b[i % 3]], writes=[c.hres_b[i]])

    pf = []
    if c.prefetch:
        TOP = ar.nbytes - 65536
        c.pre_up0 = (ar.alloc_at(TOP, [8, DFF], BF16), Buf("wup_pre0"))
        pst = [ar.alloc([1024], F32) for _ in range(2)]
        pst_b = [Buf("pstA%d" % i) for i in range(2)]
        pf = prefetch_pieces(c, c.w_mlp_up[0], c.pre_up0[0], c.pre_up0[1], D, DFF, pst, pst_b, "sp", "dve")
    npf = (len(pf) + NT - 1) // NT if pf else 0
    y_loads(0)
    for i in range(NT):
        if i + 1 < NT:
            y_loads(i + 1)
        for _ in range(npf):
            if pf:
                pf.pop(0)()
        Yhalf(i, 0)
        Yhalf(i, 1)
        if i >= 1:
            Yout(i - 1)
    Yout(NT - 1)
    p.barrier()
    ar.pop()
    ar.pop()


def ret_phase(c, h_in, h_out):
    p, ar = c.p, c.ar
    S = c.S
    NT = S // 128
    PS = c.ps_all
    pb = c.psum_b
    K = c.K
    kf = K["f"]

    def bank(i, n=1):
        return PS[:, i * 512:(i + n) * 512]

    def bankbf(i):
        return PS[:, i * 512:(i + 1) * 512].bitcast(BF16).rearrange("p (a b) -> p a b", b=128)
    ygT_d = c.ygT_d
    rp_d = c.rp_d
    ygd_b = [Buf("ygd%d" % t) for t in range(NT)]
    rpd_b = [Buf("rpd%d" % t) for t in range(NT)]
    RC = 368
    decT = kf[:, RC:RC + 512].rearrange("p (h n) -> p h n", h=4)
    xi = kf[:, RC + 512:RC + 516]
    zeta = kf[:, RC + 516:RC + 520]
    invr = kf[:, RC + 520:RC + 648]
    lg = [math.log1p(-2.0 ** (-5.0 - h)) for h in range(4)]
    gam_c = [float(math.exp(128.0 * x)) for x in lg]
    OQ, OK_, OKZ, OV, OSG = 0, 1024, 2048, 3072, 5120

    ar.push()
    win = ar.alloc([8, B_IN], BF16)
    win_b = Buf("winB")
    gbc = ar.alloc([D], F32)
    gbc_b = Buf("gbcR")
    posi = ar.alloc([NT], I32)
    posf = ar.alloc([NT], F32)
    pos_b = Buf("posR")
    p.dma("sp", gbc, c.norm_mix_g[1:2, :].partition_broadcast(128), writes=[gbc_b])
    p.dma("sp", posi, c.pos, writes=[pos_b])
    p.op("dve", lambda e: e.tensor_copy(out=posf, in_=posi), reads=[pos_b], writes=[pos_b])
    load_weight_bf16(c, c.w_in_b, win, win_b, D, B_IN)
    xt = [ar.alloc([D], F32) for _ in range(2)]
    xt_b = [Buf("xtR%d" % i) for i in range(2)]
    hn = [ar.alloc([D], BF16) for _ in range(2)]
    hn_b = [Buf("hnR%d" % i) for i in range(2)]
    junk = ar.alloc([D], BF16)
    junk_b = Buf("junkR")
    stat = [ar.alloc([4], F32) for _ in range(2)]
    stat_b = [Buf("statR%d" % i) for i in range(2)]
    hnT = [ar.alloc([8, 128], BF16) for _ in range(2)]
    hnT_b = [Buf("hnTR%d" % i) for i in range(2)]
    tb = [ar.alloc([4, 128], F32) for _ in range(2)]
    tb_b = [Buf("tbR%d" % i) for i in range(2)]
    tts = [[ar.alloc([2, 128], F32) for _ in range(4)] for _ in range(2)]
    tts_b = [[Buf("ttR%d_%d" % (a, i)) for i in range(4)] for a in range(2)]
    rot = [ar.alloc([2, 256], BF16) for _ in range(2)]
    rot_b = [Buf("rotR%d" % i) for i in range(2)]
    pk = [ar.alloc([7168], BF16) for _ in range(2)]
    pk_b = [Buf("pkR%d" % i) for i in range(2)]
    cnt = [0]

    def stageA(t):
        s = t % 2
        p.dma("sp", xt[s], h_in[t * 128:(t + 1) * 128, :], writes=[xt_b[s]])
        rmsnorm_tile(c, xt[s], xt_b[s], gbc, gbc_b, hn[s], hn_b[s], junk, junk_b, stat[s], stat_b[s])
        transpose_to(c, hn[s], hn_b[s], 8, bankbf(0), pb[0], hnT[s], hnT_b[s])
        tb_, tbb = tb[s], tb_b[s]
        p.op("dve", lambda e: e.tensor_scalar(out=tb_[:, 0, :], in0=invr, scalar1=posf[:, t:t + 1], scalar2=None, op0=ALU.mult),
             reads=[pos_b, c.K_b], writes=[tbb])
        p.op("dve", lambda e: e.tensor_scalar(out=tb_[:, 1, :], in0=tb_[:, 0, :], scalar1=0.25, scalar2=None, op0=ALU.add),
             reads=[tbb], writes=[tbb])
        sincos_from_turns(c, tb_[:, 0:2, :].rearrange("p a b -> p (a b)"), tbb, 256, barrier=False)
        p.op("dve", lambda e: e.tensor_scalar(out=tb_[:, 2:4, :], in0=tb_[:, 0:2, :], scalar1=float(DK_R ** -0.5), scalar2=None, op0=ALU.mult),
             reads=[tbb], writes=[tbb])

    def inproj(t, c0, bi):
        s = t % 2
        ps = bank(bi)
        for k in range(8):
            p.op("pe", lambda e, k=k: e.matmul(ps, lhsT=hnT[s][:, k, :], rhs=win[:, k, c0:c0 + 512], start=(k == 0), stop=(k == 7)),
                 reads=[hnT_b[s], win_b], writes=[pb[bi]])
        return ps

    def stageB1(t):
        s = t % 2
        pk_ = pk[s]
        for qk in range(2):
            sinT = tb[s][:, 2 * qk, :].unsqueeze(1).broadcast_to([128, 2, 128])
            cosT = tb[s][:, 2 * qk + 1, :].unsqueeze(1).broadcast_to([128, 2, 128])
            for ci in range(2):
                n = cnt[0]
                cnt[0] += 1
                bi = 1 + n % 3
                ps = inproj(t, qk * 1024 + ci * 512, bi)
                px = ps.rearrange("p (h d) -> p h d", h=2)
                tt, tt_b = tts[n % 2], tts_b[n % 2]
                r_, rb = rot[n % 2], rot_b[n % 2]

                def mul(o, ob, a, b_):
                    p.op("dve", lambda e: e.tensor_tensor(out=o, in0=a, in1=b_, op=ALU.mult), reads=[pb[bi], tb_b[s]], writes=[ob])
                mul(tt[0], tt_b[0], px[:, :, 0:128], cosT)
                mul(tt[1], tt_b[1], px[:, :, 128:256], sinT)
                mul(tt[2], tt_b[2], px[:, :, 128:256], cosT)
                mul(tt[3], tt_b[3], px[:, :, 0:128], sinT)
                p.op("dve", lambda e, r_=r_, tt=tt: e.tensor_tensor(out=r_[:, :, 0:128], in0=tt[0], in1=tt[1], op=ALU.subtract),
                     reads=[tt_b[0], tt_b[1]], writes=[rb])
                p.op("dve", lambda e, r_=r_, tt=tt: e.tensor_tensor(out=r_[:, :, 128:256], in0=tt[2], in1=tt[3], op=ALU.add),
                     reads=[tt_b[2], tt_b[3]], writes=[rb])
                if qk == 1:
                    for hh in range(2):
                        h = 2 * ci + hh
                        p.op("act", lambda e, r_=r_, hh=hh, h=h: e.activation(out=pk_[:, OKZ + h * 256:OKZ + (h + 1) * 256], in_=r_[:, hh, :],
                                                                              func=AF.Identity, scale=zeta[:, h:h + 1]),
                             reads=[rb, c.K_b], writes=[pk_b[s]])
                rf = r_.rearrange("p h d -> p (h d)")
                tbi = 4 + n % 2
                for j in range(4):
                    p.op("pe", lambda e, rf=rf, j=j, tbi=tbi: e.transpose(out=bankbf(tbi)[:, j, :], in_=rf[:, j * 128:(j + 1) * 128], identity=c.ident),
                         reads=[rb, c.ident_b], writes=[pb[tbi]])
                o0 = (OQ if qk == 0 else OK_) + ci * 512
                p.op("act", lambda e, o0=o0, tbi=tbi: e.copy(out=pk_[:, o0:o0 + 512].rearrange("p (a b) -> p a b", a=4), in_=bankbf(tbi)[:, 0:4, :]),
                     reads=[pb[tbi]], writes=[pk_b[s]])

    def stageB2(t):
        s = t % 2
        pk_ = pk[s]
        for ci in range(8):
            n = cnt[0]
            cnt[0] += 1
            bi = 1 + n % 3
            ps = inproj(t, 2048 + ci * 512, bi)
            if ci < 4:
                p.op("act", lambda e, ps=ps, ci=ci: e.copy(out=pk_[:, OV + ci * 512:OV + (ci + 1) * 512], in_=ps), reads=[pb[bi]], writes=[pk_b[s]])
            else:
                p.op("act", lambda e, ps=ps, ci=ci: e.activation(out=pk_[:, OSG + (ci - 4) * 512:OSG + (ci - 3) * 512], in_=ps, func=AF.Silu),
                     reads=[pb[bi]], writes=[pk_b[s]])
        p.dma("sp", rp_d[t], pk_, reads=[pk_b[s]], writes=[rpd_b[t]])

    stageA(0)
    for t in range(NT):
        stageB1(t)
        if t + 1 < NT:
            stageA(t + 1)
        stageB2(t)
    p.barrier()
    ar.pop()
    if hasattr(c, "sc_ni"):
        del c.sc_ni

    ar.push()
    state = ar.alloc([4, 2, 512], F32)
    state_bf = ar.alloc([4, 2, 512], BF16)
    st_b = [[Buf("st%d_%d" % (h, dc)) for dc in range(2)] for h in range(4)]
    stbf_b = [[Buf("stbf%d_%d" % (h, dc)) for dc in range(2)] for h in range(4)]
    gng = ar.alloc([2048], F32)
    gng_b = Buf("gng")
    p.dma("sp", gng, c.ret_norm_g[0:1, :].partition_broadcast(128), writes=[gng_b])
    p.op("dve", lambda e: e.memset(state.rearrange("p a b c -> p (a b c)"), 0.0), writes=[b for r in st_b for b in r])
    p.op("dve", lambda e: e.memset(state_bf.rearrange("p a b c -> p (a b c)"), 0.0), writes=[b for r in stbf_b for b in r])
    rp = [ar.alloc([7168], BF16) for _ in range(2)]
    rp_b = [Buf("rp%d" % i) for i in range(2)]
    idt = [ar.alloc([128], BF16) for _ in range(4)]
    idt_b = [Buf("idtR%d" % i) for i in range(4)]
    y = [ar.alloc([512], F32) for _ in range(4)]
    y_b = [Buf("yR%d" % i) for i in range(4)]
    junk2 = ar.alloc([512], BF16)
    junk2_b = Buf("junk2R")
    G = [ar.alloc([512], F32) for _ in range(2)]
    G_b = [Buf("GR%d" % i) for i in range(2)]
    A = [ar.alloc([512], F32) for _ in range(2)]
    A_b = [Buf("AR%d" % i) for i in range(2)]
    yg = ar.alloc([4, 512], BF16)
    yg_b = [Buf("ygR%d" % i) for i in range(4)]
    ygT = [ar.alloc([16, 128], BF16) for _ in range(2)]
    ygT_b = [Buf("ygTR%d" % i) for i in range(2)]
    gs = [ar.alloc([32], F32) for _ in range(2)]
    gs_b = [Buf("gsR%d" % i) for i in range(2)]

    def r1_load(t):
        p.dma("sp", rp[t % 2], rp_d[t], reads=[rpd_b[t]], writes=[rp_b[t % 2]])

    def r1_tile(t):
        s = t % 2
        if t + 1 < NT:
            r1_load(t + 1)
        r_ = rp[s]
        rb = rp_b[s]
        qT = r_[:, OQ:OQ + 1024].rearrange("p (a b) -> p a b", a=8)
        kT = r_[:, OK_:OK_ + 1024].rearrange("p (a b) -> p a b", a=8)
        kz = r_[:, OKZ:OKZ + 1024].rearrange("p (a b) -> p a b", a=4)
        v = r_[:, OV:OV + 2048].rearrange("p (a b) -> p a b", a=4)
        sg = r_[:, OSG:OSG + 2048].rearrange("p (a b) -> p a b", a=4)
        g_, gb = gs[s], gs_b[s]
        for h in range(4):
            pin = bank(6)[:, h * 128:(h + 1) * 128]
            for dc in range(2):
                p.op("pe", lambda e, pin=pin, h=h, dc=dc: e.matmul(pin, lhsT=kT[:, 2 * h + dc, :], rhs=qT[:, 2 * h + dc, :],
                                                                   start=(dc == 0), stop=(dc == 1)),
                     reads=[rb], writes=[pb[6]])
        for h in range(4):
            pin = bank(6)[:, h * 128:(h + 1) * 128]
            p.op("dve", lambda e, pin=pin, h=h: e.tensor_tensor(out=idt[h], in0=pin, in1=decT[:, h, :], op=ALU.mult),
                 reads=[pb[6], c.K_b], writes=[idt_b[h]])
        for h in range(4):
            bo = 7 if h % 2 == 0 else 5
            po = bank(bo)
            p.op("pe", lambda e, po=po, h=h: e.matmul(po, lhsT=idt[h], rhs=v[:, h, :], start=True, stop=False),
                 reads=[idt_b[h], rb], writes=[pb[bo]])
            for dc in range(2):
                p.op("pe", lambda e, po=po, h=h, dc=dc: e.matmul(po, lhsT=qT[:, 2 * h + dc, :], rhs=state_bf[:, h, dc, :],
                                                                 start=False, stop=(dc == 1)),
                     reads=[rb, stbf_b[h][dc]], writes=[pb[bo]])
            p.op("act", lambda e, po=po, h=h: e.activation(out=y[h], in_=po, func=AF.Identity, scale=xi[:, h:h + 1],
                                                           accum_out=g_[:, h:h + 1]),
                 reads=[pb[bo], c.K_b], writes=[y_b[h], gb])
            p.op("act", lambda e, h=h: e.activation(out=junk2, in_=y[h], func=AF.Square, accum_out=g_[:, 4 + h:5 + h]),
                 reads=[y_b[h]], writes=[junk2_b, gb])
        for h in range(4):
            for dc in range(2):
                bu = 4 if dc == 0 else 3
                pu = bank(bu)
                p.op("pe", lambda e, pu=pu, h=h, dc=dc: e.matmul(pu, lhsT=kz[:, h, dc * 128:(dc + 1) * 128], rhs=v[:, h, :], start=True, stop=True),
                     reads=[rb], writes=[pb[bu]])
                p.op("dve", lambda e, pu=pu, h=h, dc=dc: e.scalar_tensor_tensor(out=state[:, h, dc, :], in0=state[:, h, dc, :], scalar=gam_c[h],
                                                                                in1=pu, op0=ALU.mult, op1=ALU.add),
                     reads=[st_b[h][dc], pb[bu]], writes=[st_b[h][dc]])
                p.op("act", lambda e, h=h, dc=dc: e.copy(out=state_bf[:, h, dc, :], in_=state[:, h, dc, :]),
                     reads=[st_b[h][dc]], writes=[stbf_b[h][dc]])
        p.op("dve", lambda e: e.tensor_scalar(out=g_[:, 8:12], in0=g_[:, 0:4], scalar1=1.0 / 512, scalar2=None, op0=ALU.mult), reads=[gb], writes=[gb])
        p.op("dve", lambda e: e.tensor_tensor(out=g_[:, 24:28], in0=g_[:, 8:12], in1=g_[:, 8:12], op=ALU.mult), reads=[gb], writes=[gb])
        p.op("dve", lambda e: e.scalar_tensor_tensor(out=g_[:, 12:16], in0=g_[:, 4:8], scalar=1.0 / 512, in1=g_[:, 24:28],
                                                     op0=ALU.mult, op1=ALU.subtract), reads=[gb], writes=[gb])
        p.op("dve", lambda e: e.tensor_scalar(out=g_[:, 12:16], in0=g_[:, 12:16], scalar1=RMS_EPS, scalar2=None, op0=ALU.add), reads=[gb], writes=[gb])
        p.op("act", lambda e: e.activation(out=g_[:, 12:16], in_=g_[:, 12:16], func=AF.Ln), reads=[gb], writes=[gb])
        p.op("act", lambda e: e.activation(out=g_[:, 16:20], in_=g_[:, 12:16], func=AF.Exp, scale=-0.5), reads=[gb], writes=[gb])
        p.op("dve", lambda e: e.scalar_tensor_tensor(out=g_[:, 20:24], in0=g_[:, 8:12], scalar=-1.0, in1=g_[:, 16:20],
                                                     op0=ALU.mult, op1=ALU.mult), reads=[gb], writes=[gb])
        for h in range(4):
            Gh, Ghb = G[h % 2], G_b[h % 2]
            Ah, Ahb = A[h % 2], A_b[h % 2]
            p.op("dve", lambda e, h=h, Gh=Gh: e.tensor_tensor(out=Gh, in0=gng[:, h * 512:(h + 1) * 512], in1=sg[:, h, :], op=ALU.mult),
                 reads=[gng_b, rb], writes=[Ghb])
            p.op("dve", lambda e, h=h, Ah=Ah: e.tensor_scalar(out=Ah, in0=y[h], scalar1=g_[:, 16 + h:17 + h], scalar2=g_[:, 20 + h:21 + h],
                                                              op0=ALU.mult, op1=ALU.add),
                 reads=[y_b[h], gb], writes=[Ahb])
            p.op("dve", lambda e, h=h, Ah=Ah, Gh=Gh: e.tensor_tensor(out=yg[:, h, :], in0=Ah, in1=Gh, op=ALU.mult), reads=[Ahb, Ghb], writes=[yg_b[h]])
        ygf = yg.rearrange("p h d -> p (h d)")
        yT = ygT[s]
        for half in range(2):
            for j in range(8):
                jj = half * 8 + j
                p.op("pe", lambda e, jj=jj, j=j, half=half: e.transpose(out=bankbf(half)[:, j, :], in_=ygf[:, jj * 128:(jj + 1) * 128], identity=c.ident),
                     reads=[yg_b[jj // 4], c.ident_b], writes=[pb[half]])
            p.op("act", lambda e, yT=yT, half=half: e.copy(out=yT[:, half * 8:(half + 1) * 8, :], in_=bankbf(half)),
                 reads=[pb[half]], writes=[ygT_b[s]])
        p.dma("sp", ygT_d[t], yT, reads=[ygT_b[s]], writes=[ygd_b[t]])

    pf = []
    if c.prefetch:
        TOP = ar.nbytes - 65536
        c.pre_up1 = (ar.alloc_at(TOP, [8, DFF], BF16), Buf("wup_pre1"))
        pst = [ar.alloc([1024], F32) for _ in range(2)]
        pst_b = [Buf("pstR%d" % i) for i in range(2)]
        pf = prefetch_pieces(c, c.w_mlp_up[1], c.pre_up1[0], c.pre_up1[1], D, DFF, pst, pst_b, "sp", "act")
    npf = (len(pf) + NT - 1) // NT if pf else 0
    r1_load(0)
    for t in range(NT):
        for _ in range(npf):
            if pf:
                pf.pop(0)()
        r1_tile(t)
    p.barrier()
    ar.pop()

    ar.push()
    wout = ar.alloc([16, D], BF16)
    wout_b = Buf("woutR")
    load_weight_bf16(c, c.w_out_b, wout, wout_b, 2048, D)
    yl = [ar.alloc([16, 128], BF16) for _ in range(2)]
    yl_b = [Buf("ylR%d" % i) for i in range(2)]
    xo = [ar.alloc([D], F32) for _ in range(2)]
    xo_b = [Buf("xoR%d" % i) for i in range(2)]

    def r2_loads(t):
        p.dma("sp", yl[t % 2], ygT_d[t], reads=[ygd_b[t]], writes=[yl_b[t % 2]])
        p.dma("sp", xo[t % 2], h_in[t * 128:(t + 1) * 128, :], writes=[xo_b[t % 2]])
    pf = []
    if c.prefetch:
        TOP1 = ar.nbytes - 131072
        c.pre_dn1 = (ar.alloc_at(TOP1, [32, D], BF16), Buf("wdn_pre1"))
        pst = [ar.alloc([1024], F32) for _ in range(2)]
        pst_b = [Buf("pstR2%d" % i) for i in range(2)]
        pf = prefetch_pieces(c, c.w_mlp_down[1], c.pre_dn1[0], c.pre_dn1[1], DFF, D, pst, pst_b, "sp", "dve")
    npf = (len(pf) + NT - 1) // NT if pf else 0
    for t in range(NT):
        s = t % 2
        if t == 0:
            r2_loads(0)
        if t + 1 < NT:
            r2_loads(t + 1)
        for _ in range(npf):
            if pf:
                pf.pop(0)()
        for half in range(2):
            ps = bank(2 * s + half)
            for j in range(16):
                p.op("pe", lambda e, ps=ps, j=j, half=half, s=s: e.matmul(ps, lhsT=yl[s][:, j, :], rhs=wout[:, j, half * 512:(half + 1) * 512],
                                                                          start=(j == 0), stop=(j == 15)),
                     reads=[yl_b[s], wout_b], writes=[pb[2 * s + half]])
            p.op("dve", lambda e, ps=ps, half=half, s=s: e.tensor_tensor(out=xo[s][:, half * 512:(half + 1) * 512], in0=ps,
                                                                         in1=xo[s][:, half * 512:(half + 1) * 512], op=ALU.add),
                 reads=[pb[2 * s + half], xo_b[s]], writes=[xo_b[s]])
        p.dma("sp", h_out[t * 128:(t + 1) * 128, :], xo[s], reads=[xo_b[s]], writes=[c.hres_b[t]])
    p.barrier()
    ar.pop()


def dump_phase(c):
    p, ar = c.p, c.ar
    ar.push()
    xt = [ar.alloc([D], F32) for _ in range(2)]
    xb = [Buf("dump%d" % i) for i in range(2)]
    for t in range(c.S // 128):
        p.dma("sp", xt[t % 2], c.hres[t * 128:(t + 1) * 128, :], writes=[xb[t % 2]])
        p.dma("sp", c.out[t * 128:(t + 1) * 128, :], xt[t % 2], reads=[xb[t % 2]], is_out=True)
    p.barrier()
    ar.pop()


NCONST = 368 + 648
FULL_PHASES = ("dsa", "mlp0h", "reth", "mlp1f")


def make_consts():
    k = np.zeros((128, NCONST), np.float32)
    k[:, 0:128] = np.eye(128, dtype=np.float32)
    q = np.arange(128)[:, None]
    kk = np.arange(128)[None, :]
    k[:, 128:256] = np.where(kk <= q, 0.0, -1.0e30).astype(np.float32)
    inv_a = (np.float32(ROPE_THETA) ** (-np.arange(16, dtype=np.float32) / np.float32(16))).astype(np.float32)
    inv_i = (np.float32(ROPE_THETA) ** (-np.arange(8, dtype=np.float32) / np.float32(8))).astype(np.float32)
    k[:, 256:304] = (np.concatenate([inv_a, inv_a, inv_i, inv_i]).astype(np.float64) / (2 * math.pi)).astype(np.float32)[None, :]
    k[:, 304:352] = np.concatenate([np.full(16, 0.0), np.full(16, 0.25), np.full(8, 0.0),
                                    np.full(8, 0.25)]).astype(np.float32)[None, :]
    k[:, 352:368] = (2.0 ** -(np.arange(16) + 1.0)).astype(np.float32)[None, :]
    RC = 368
    m = np.arange(128, dtype=np.float64)
    for h in range(4):
        lgm = math.log1p(-2.0 ** (-5.0 - h))
        dec = np.where(m[None, :] >= m[:, None], np.exp(-(m[:, None] + 1.0) * lgm), 0.0)
        k[:, RC + h * 128:RC + (h + 1) * 128] = dec.astype(np.float32)
        k[:, RC + 512 + h] = np.exp((m + 1.0) * lgm).astype(np.float32)
        k[:, RC + 516 + h] = np.exp((127.0 - m) * lgm).astype(np.float32)
    inv_r = (np.float32(RET_THETA) ** (-np.arange(128, dtype=np.float32) / np.float32(128))).astype(np.float64)
    k[:, RC + 520:RC + 648] = (inv_r / (2 * math.pi)).astype(np.float32)[None, :]
    return k


def build(S, phases=("mlp0",), debug=False):
    nc = bass.Bass("TRN2", target_bir_lowering=False)
    from contextlib import ExitStack
    es = ExitStack()
    c = Ctx()
    c.nc, c.S = nc, S
    c.prefetch = (tuple(phases) == FULL_PHASES)
    c.p = Prog()

    def din(name, shape, dt=F32):
        return nc.dram_tensor(name, shape, dt, kind="ExternalInput").ap()

    c.x = din("x", [S, D])
    c.pos = din("positions", [128, S // 128], I32)
    c.norm_mix_g = din("norm_mix_g", [2, D])
    c.norm_mlp_g = din("norm_mlp_g", [2, D])
    c.w_in_a = din("w_in_a", [D, A_IN])
    c.w_out_a = din("w_out_a", [D, D])
    c.w_in_b = din("w_in_b", [D, B_IN])
    c.ret_norm_g = din("ret_norm_g", [1, 2048])
    c.w_out_b = din("w_out_b", [2048, D])
    c.w_mlp_up = din("w_mlp_up", [2, D, DFF])
    c.w_mlp_down = din("w_mlp_down", [2, DFF, D])
    c.final_norm_g = din("final_norm_g", [1, D])
    c.out = nc.dram_tensor("out", [S, D], F32, kind="ExternalOutput").ap()
    c.hres = nc.dram_tensor("hres", [S, D], F32, kind="Internal").ap()
    c.dbg = nc.dram_tensor("dbg", [S, 32], F32, kind="ExternalOutput").ap() if debug else None
    c.dbgs = nc.dram_tensor("dbgs", [S // 128, 128, S], F32, kind="ExternalOutput").ap() if debug else None
    c.dbgt = nc.dram_tensor("dbgt", [128, (S // 128) * 49], F32, kind="ExternalOutput").ap() if debug else None

    NT = S // 128
    c.qT_d = nc.dram_tensor("qT_d", [NT, 128, 1024], BF16, kind="Internal").ap().rearrange("t p (h q) -> t p h q", h=8)
    c.qiT_d = nc.dram_tensor("qiT_d", [NT, 128, 512], BF16, kind="Internal").ap().rearrange("t p (h q) -> t p h q", h=4)
    c.hres_b = [Buf("hres%d" % t) for t in range(NT)]
    c.mb_d = nc.dram_tensor("mb_d", [NT, 128, S], BF16, kind="Internal").ap()
    c.rp_d = nc.dram_tensor("rp_d", [NT, 128, 7168], BF16, kind="Internal").ap()
    c.ygT_d = nc.dram_tensor("ygT_d", [NT, 128, 2048], BF16, kind="Internal").ap().rearrange("t p (h q) -> t p h q", h=16)
    c.consts_in = din("consts", [128, NCONST])

    c.ar = Arena(nc, es, 206 * 1024)
    c.ps_all = es.enter_context(nc.psum_tensor("ps_all", [128, 4096], F32))[:, :]
    c.psum = [c.ps_all[:, i * 512:(i + 1) * 512] for i in range(8)]
    c.psum_b = [Buf("ps%d" % i) for i in range(8)]
    c.stage = [c.ar.alloc([1024], F32) for _ in range(2)]
    c.stage_b = [Buf("stage%d" % i) for i in range(2)]
    c.stage_i = 0
    kf = c.ar.alloc([NCONST], F32)
    c.K_b = Buf("K")
    c.p.dma("sp", kf, c.consts_in, writes=[c.K_b])
    c.ident = c.ar.alloc([128], BF16)
    c.ident_b = Buf("ident")
    c.p.op("dve", lambda e: e.tensor_copy(out=c.ident, in_=kf[:, 0:128]), reads=[c.K_b], writes=[c.ident_b])
    I4 = c.ar.alloc([4, 128], BF16)
    c.p.op("dve", lambda e: e.tensor_copy(out=I4, in_=kf[:, 0:128].unsqueeze(1).broadcast_to([128, 4, 128])),
           reads=[c.K_b], writes=[c.ident_b])
    ones = c.ar.alloc([128], BF16)
    c.p.op("dve", lambda e: e.memset(ones, 1.0), writes=[c.ident_b])
    c.K = {"causal": kf[:, 128:256], "invrow": kf[:, 256:304], "offs": kf[:, 304:352], "pow2": kf[:, 352:368],
           "I4": I4.rearrange("p a b -> p (a b)"), "ones": ones, "f": kf}
    c.K_b = c.ident_b

    for ph in phases:
        if ph == "mlp0":
            mlp_phase(c, 0, c.x, c.hres)
        elif ph == "mlp0f":
            mlp_phase(c, 0, c.x, None, final_g=c.final_norm_g, out_dram=c.out)
        elif ph == "dsa":
            dsa_phase(c, c.x, c.hres)
        elif ph == "mlp0h":
            mlp_phase(c, 0, c.hres, c.hres, pre_up=getattr(c, "pre_up0", None))
        elif ph == "reth":
            ret_phase(c, c.hres, c.hres)
        elif ph == "mlp1f":
            mlp_phase(c, 1, c.hres, None, final_g=c.final_norm_g, out_dram=c.out,
                      pre_up=getattr(c, "pre_up1", None), pre_dn=getattr(c, "pre_dn1", None))
        elif ph == "ret":
            ret_phase(c, c.x, c.hres)
        elif ph == "dump":
            dump_phase(c)
    c.p.finish()
    c.p.emit(nc, es)
    es.close()
    return nc


def kernel(x, positions, norm_mix_g, norm_mlp_g, w_in_a, w_out_a, w_in_b, ret_norm_g, w_out_b,
           w_mlp_up, w_mlp_down, final_norm_g):
    f = lambda a: np.ascontiguousarray(np.asarray(a, dtype=np.float32))
    x = f(x)
    positions = np.asarray(positions).astype(np.int32)
    B, S, _ = x.shape
    nc = build(S, phases=FULL_PHASES)
    common = dict(
        norm_mix_g=f(norm_mix_g), norm_mlp_g=f(norm_mlp_g),
        w_in_a=f(np.asarray(w_in_a)[0]), w_out_a=f(np.asarray(w_out_a)[0]),
        w_in_b=f(np.asarray(w_in_b)[0]), ret_norm_g=f(np.asarray(ret_norm_g)).reshape(1, 2048),
        w_out_b=f(np.asarray(w_out_b)[0]), w_mlp_up=f(w_mlp_up), w_mlp_down=f(w_mlp_down),
        final_norm_g=f(final_norm_g).reshape(1, D), consts=make_consts())
    in_maps = []
    for b in range(B):
        m = dict(common)
        m["x"] = np.ascontiguousarray(x[b])
        m["positions"] = np.ascontiguousarray(positions[b].reshape(S // 128, 128).T)
        in_maps.append(m)
    res = run_bass_kernel_spmd(nc, in_maps, core_ids=list(range(B)))
    return np.stack([np.asarray(r["out"], dtype=np.float32) for r in res.results], axis=0)
```

```python
import math
import numpy as np
import concourse.bass as bass
import concourse.mybir as mybir
from concourse.bass_utils import run_bass_kernel_spmd

F32 = mybir.dt.float32
BF16 = mybir.dt.bfloat16
I32 = mybir.dt.int32
AF = mybir.ActivationFunctionType
ALU = mybir.AluOpType
AX = mybir.AxisListType

D = 1024
NCORES = 8
RMS_EPS = 1e-6
DFF = 4096
H_A, DH_A, ROT_A = 8, 128, 32
H_IDX, D_IDX, ROT_IDX = 8, 64, 16
A_IN = 1864
ROPE_THETA = 500000.0
H_R, DK_R, DV_R = 4, 256, 512
B_IN = 6144
RET_THETA = 10000.0
NEG = -30000.0
BIG = 3.0e38


class Buf:
    __slots__ = ("name", "w", "rs")

    def __init__(self, name):
        self.name = name
        self.w = None
        self.rs = []


class Prog:
    ENG = ("pe", "act", "dve", "pool", "sp")
    NDSEM = 8

    def __init__(self):
        self.st = {e: [] for e in self.ENG}
        self.seen = {e: {} for e in self.ENG}
        self.dq = {e: 0 for e in ("sp", "act", "pool")}
        self.dval = {}
        self.out_events = []

    def _deps(self, eng, reads, writes):
        deps = []
        for b in reads:
            if b.w is not None:
                deps.append(b.w)
        for b in writes:
            if b.w is not None:
                ev = b.w
                if not (ev[0] == "e" and ev[1] == eng):
                    deps.append(ev)
            for ev in b.rs:
                if not (ev[0] == "e" and ev[1] == eng):
                    deps.append(ev)
        return deps

    def _filter(self, eng, deps):
        waits = {}
        for ev in deps:
            if ev[0] == "e":
                if ev[1] == eng and eng == "pe":
                    continue
                key = ("e", ev[1])
            else:
                key = ("d", ev[1])
            if self.seen[eng].get(key, -1) >= ev[2]:
                continue
            if waits.get(key, -1) < ev[2]:
                waits[key] = ev[2]
        for key, v in waits.items():
            self.seen[eng][key] = v
            if key[0] == "e":
                self.st[key[1]][v]["mark"] = True
        return list(waits.items())

    def _commit(self, ev, reads, writes):
        for b in reads:
            b.rs.append(ev)
        for b in writes:
            b.w = ev
            b.rs = []

    def op(self, eng, fn, reads=(), writes=()):
        deps = self._deps(eng, reads, writes)
        waits = self._filter(eng, deps)
        idx = len(self.st[eng])
        self.st[eng].append({"fn": fn, "waits": waits, "mark": False, "dma": None})
        ev = ("e", eng, idx)
        self._commit(ev, reads, writes)
        return ev

    def dma(self, q, out, in_, reads=(), writes=(), is_out=False, **kw):
        k = self.dq[q] % self.NDSEM
        self.dq[q] += 1
        key = (q, k)
        prev = self.dval.get(key, 0)
        deps = self._deps(None, reads, writes)
        if prev > 0:
            deps.append(("d", key, prev))
        waits = self._filter(q, deps)
        val = prev + 16
        self.dval[key] = val
        idx = len(self.st[q])
        self.st[q].append({"fn": lambda e, o=out, i=in_: e.dma_start(out=o, in_=i, **kw),
                           "waits": waits, "mark": False, "dma": (key, 16)})
        ev = ("d", key, val)
        self._commit(ev, reads, writes)
        if is_out:
            self.out_events.append(ev)
        return ev

    def barrier(self):
        lasts = []
        for e in self.ENG:
            for i in range(len(self.st[e]) - 1, -1, -1):
                r = self.st[e][i]
                if r["fn"] is not None and r["dma"] is None:
                    lasts.append(("e", e, i))
                    break
        for key, v in self.dval.items():
            lasts.append(("d", key, v))
        for e in self.ENG:
            deps = [ev for ev in lasts if not (ev[0] == "e" and ev[1] == e)]
            waits = self._filter(e, deps)
            if waits:
                self.st[e].append({"fn": None, "waits": waits, "mark": False, "dma": None})

    def finish(self):
        waits = self._filter("sp", list(self.out_events))
        self.st["sp"].append({"fn": None, "waits": waits, "mark": False, "dma": None})

    def emit(self, nc, es):
        esem = {e: es.enter_context(nc.semaphore("s_" + e)) for e in self.ENG}
        dsem = {}
        for q in ("sp", "act", "pool"):
            for k in range(self.NDSEM):
                dsem[(q, k)] = es.enter_context(nc.semaphore("d_%s%d" % (q, k)))
        cum = {}
        for e in self.ENG:
            c = 0
            arr = []
            for r in self.st[e]:
                if r["mark"]:
                    c += 1
                arr.append(c)
            cum[e] = arr
        block = es.enter_context(nc.Block())

        def run(eng_name):
            def body(engine):
                for r in self.st[eng_name]:
                    for key, v in r["waits"]:
                        if key[0] == "e":
                            engine.wait_ge(esem[key[1]], cum[key[1]][v])
                        else:
                            engine.wait_ge(dsem[key[1]], v)
                    if r["fn"] is None:
                        continue
                    ins = r["fn"](engine)
                    if r["dma"] is not None:
                        ins.then_inc(dsem[r["dma"][0]], 16)
                        assert not r["mark"]
                    elif r["mark"]:
                        ins.then_inc(esem[eng_name], 1)
            return body

        block.tensor(run("pe"))
        block.scalar(run("act"))
        block.vector(run("dve"))
        block.gpsimd(run("pool"))
        block.sync(run("sp"))


class Arena:
    def __init__(self, nc, es, nbytes):
        self.t = es.enter_context(nc.sbuf_tensor("arena", [128, nbytes // 4], F32))
        self.nbytes = nbytes
        self.off = 0
        self.marks = []
        self.guard = nbytes

    def alloc_at(self, off, shape, dtype):
        esz = 2 if dtype == BF16 else 4
        n = int(np.prod(shape))
        nb = n * esz
        assert off % 64 == 0 and off + nb <= self.nbytes and self.off <= off, ("alloc_at", off, nb, self.off)
        self.guard = min(self.guard, off)
        ap = self.t[:, off // 4:(off + nb) // 4]
        if dtype != F32:
            ap = ap.bitcast(dtype)
        if len(shape) == 2:
            ap = ap.rearrange("p (a b) -> p a b", a=shape[0])
        return ap

    def release_guard(self):
        self.guard = self.nbytes

    def alloc(self, shape, dtype):
        esz = 2 if dtype == BF16 else 4
        n = int(np.prod(shape))
        nb = (n * esz + 63) // 64 * 64
        assert self.off + nb <= min(self.nbytes, self.guard), ("SBUF arena overflow", self.off, nb, self.nbytes, self.guard)
        a = self.off // 4
        ap = self.t[:, a:a + nb // 4]
        if dtype != F32:
            ap = ap.bitcast(dtype)
        ap = ap[:, 0:n]
        self.off += nb
        if len(shape) == 2:
            ap = ap.rearrange("p (a b) -> p a b", a=shape[0])
        elif len(shape) == 3:
            ap = ap.rearrange("p (a b c) -> p a b c", a=shape[0], b=shape[1])
        return ap

    def push(self):
        self.marks.append(self.off)

    def pop(self):
        self.off = self.marks.pop()


class Ctx:
    pass


def load_weight_bf16(c, w_dram, dst, dst_buf, K, C, col0=0, ncols=None, splits=2):
    ncols = C if ncols is None else ncols
    kc = K // 128
    src = w_dram.rearrange("(k p) c -> p k c", p=128)
    p = c.p
    CH = 1024
    if ncols >= CH:
        pieces = [(k, 1, c0, min(CH, ncols - c0)) for k in range(kc) for c0 in range(0, ncols, CH)]
    else:
        kk = max(1, CH // ncols)
        pieces = [(k, min(kk, kc - k), 0, ncols) for k in range(0, kc, kk)]
    engs = ("dve", "act", "dve", "act", "dve", "act", "pool")
    for (k, nk, c0, w) in pieces:
        i = c.stage_i
        c.stage_i += 1
        st = c.stage[i % len(c.stage)]
        sb = c.stage_b[i % len(c.stage)]
        stv = st[:, 0:nk * w].rearrange("p (a b) -> p a b", a=nk)
        p.dma("sp" if i % 2 == 0 else "act", stv, src[:, k:k + nk, col0 + c0:col0 + c0 + w], writes=[sb])
        eng = engs[i % len(engs)]
        if eng == "act":
            p.op("act", lambda e, stv=stv, k=k, nk=nk, c0=c0, w=w: e.copy(out=dst[:, k:k + nk, c0:c0 + w], in_=stv),
                 reads=[sb], writes=[dst_buf])
        else:
            p.op(eng, lambda e, stv=stv, k=k, nk=nk, c0=c0, w=w: e.tensor_copy(out=dst[:, k:k + nk, c0:c0 + w], in_=stv),
                 reads=[sb], writes=[dst_buf])


def prefetch_pieces(c, w_dram, dst, dst_buf, K, ncols, stage, stage_b, queue, eng):
    kc = K // 128
    src = w_dram.rearrange("(k p) c -> p k c", p=128)
    p = c.p
    CH = 1024
    pieces = [(k, c0, min(CH, ncols - c0)) for c0 in range(0, ncols, CH) for k in range(kc)]
    out = []
    for n, (k, c0, w) in enumerate(pieces):
        def piece(n=n, k=k, c0=c0, w=w):
            st, sb = stage[n % len(stage)], stage_b[n % len(stage)]
            p.dma(queue, st[:, 0:w], src[:, k, c0:c0 + w], writes=[sb])
            if eng == "act":
                p.op("act", lambda e: e.copy(out=dst[:, k, c0:c0 + w], in_=st[:, 0:w]), reads=[sb], writes=[dst_buf])
            else:
                p.op(eng, lambda e: e.tensor_copy(out=dst[:, k, c0:c0 + w], in_=st[:, 0:w]), reads=[sb], writes=[dst_buf])
        out.append(piece)
    return out


def rstd_ops(c, stat, stat_buf, scale, n=1):
    p = c.p
    p.op("dve", lambda e: e.tensor_scalar(out=stat[:, n:2 * n], in0=stat[:, 0:n], scalar1=scale, scalar2=RMS_EPS,
                                          op0=ALU.mult, op1=ALU.add),
         reads=[stat_buf], writes=[stat_buf])
    p.op("act", lambda e: e.activation(out=stat[:, n:2 * n], in_=stat[:, n:2 * n], func=AF.Ln),
         reads=[stat_buf], writes=[stat_buf])
    p.op("act", lambda e: e.activation(out=stat[:, 2 * n:3 * n], in_=stat[:, n:2 * n], func=AF.Exp, scale=-0.5),
         reads=[stat_buf], writes=[stat_buf])


def rmsnorm_tile(c, xt, xt_buf, g_bc, g_buf, hn, hn_buf, sq_junk, junk_buf, stat, stat_buf):
    p = c.p
    p.op("act", lambda e: e.activation(out=sq_junk, in_=xt, func=AF.Square, accum_out=stat[:, 0:1]),
         reads=[xt_buf], writes=[junk_buf, stat_buf])
    rstd_ops(c, stat, stat_buf, 1.0 / D)
    p.op("dve", lambda e: e.scalar_tensor_tensor(out=hn, in0=xt, scalar=stat[:, 2:3], in1=g_bc,
                                                 op0=ALU.mult, op1=ALU.mult),
         reads=[xt_buf, stat_buf, g_buf], writes=[hn_buf])


def transpose_to(c, src, src_buf, nchunks, ps, ps_buf, dst, dst_buf, evac="act"):
    p = c.p
    for k in range(nchunks):
        p.op("pe", lambda e, k=k: e.transpose(out=ps[:, k, :], in_=src[:, k * 128:(k + 1) * 128], identity=c.ident),
             reads=[src_buf, c.ident_b], writes=[ps_buf])
    if evac == "act":
        p.op("act", lambda e: e.copy(out=dst, in_=ps[:, 0:nchunks, :]), reads=[ps_buf], writes=[dst_buf])
    else:
        p.op(evac, lambda e: e.tensor_copy(out=dst, in_=ps[:, 0:nchunks, :]), reads=[ps_buf], writes=[dst_buf])


def mlp_phase(c, li, h_in, h_out, final_g=None, out_dram=None, pre_up=None, pre_dn=None):
    p, ar, nc = c.p, c.ar, c.nc
    S = c.S
    TB = 256
    NJ = TB // 128
    nblk = S // TB
    ar.push()
    if pre_up is not None:
        wup, wup_b = pre_up
    else:
        wup, wup_b = ar.alloc([8, DFF], BF16), Buf("wup")
    if pre_dn is not None:
        wdn, wdn_b = pre_dn
    else:
        wdn, wdn_b = ar.alloc([32, D], BF16), Buf("wdn")
    gbc = ar.alloc([D], F32)
    gbc_b = Buf("gbc")
    p.dma("sp", gbc, c.norm_mlp_g[li:li + 1, :].partition_broadcast(128), writes=[gbc_b])
    if pre_up is None:
        load_weight_bf16(c, c.w_mlp_up[li], wup, wup_b, D, DFF, splits=4)
    if pre_dn is None:
        load_weight_bf16(c, c.w_mlp_down[li], wdn, wdn_b, DFF, D, splits=4)
    if final_g is not None:
        fg = ar.alloc([D], F32)
        fg_b = Buf("fg")
        p.dma("sp", fg, final_g.partition_broadcast(128), writes=[fg_b])
    NX = 4
    xt = [ar.alloc([D], F32) for _ in range(NX)]
    xt_b = [Buf("xt%d" % i) for i in range(NX)]
    hn = [ar.alloc([D], BF16) for _ in range(2)]
    hn_b = [Buf("hn%d" % i) for i in range(2)]
    junk = ar.alloc([D], BF16)
    junk_b = Buf("junk")
    stat = [ar.alloc([4], F32) for _ in range(4)]
    stat_b = [Buf("stat%d" % i) for i in range(4)]
    hnT = [ar.alloc([8, TB], BF16) for _ in range(2)]
    hnT_b = [Buf("hnT%d" % i) for i in range(2)]
    actT = ar.alloc([32, TB], BF16)
    actT_b = [Buf("actT%d" % i) for i in range(32)]
    rl = [ar.alloc([TB], F32) for _ in range(2)]
    rl_b = [Buf("rl%d" % i) for i in range(2)]
    pst = [c.psum[i].bitcast(BF16) for i in (0, 1)]
    pst = [t.rearrange("p (a b) -> p a b", b=128) for t in pst]
    pst_b = [c.psum_b[0], c.psum_b[1]]
    xi = [0]

    def stage1(b):
        tiles = []
        for j in range(NJ):
            t = b * NJ + j
            s = xi[0] % NX
            xi[0] += 1
            p.dma("sp", xt[s], h_in[t * 128:(t + 1) * 128, :], writes=[xt_b[s]])
            hs = t % 2
            ss = t % 4
            rmsnorm_tile(c, xt[s], xt_b[s], gbc, gbc_b, hn[hs], hn_b[hs], junk, junk_b, stat[ss], stat_b[ss])
            transpose_to(c, hn[hs], hn_b[hs], 8, pst[t % 2], pst_b[t % 2],
                         hnT[b % 2][:, :, j * 128:(j + 1) * 128], hnT_b[b % 2])
            tiles.append(s)
        return tiles

    def stage2(b, tiles):
        hT = hnT[b % 2]
        for f in range(32):
            ps = c.psum[2 + f % 2]
            ps_b = c.psum_b[2 + f % 2]
            for k in range(8):
                p.op("pe", lambda e, ps=ps, k=k, f=f: e.matmul(ps[:, 0:TB], lhsT=wup[:, k, f * 128:(f + 1) * 128],
                                                               rhs=hT[:, k, :], start=(k == 0), stop=(k == 7)),
                     reads=[wup_b, hnT_b[b % 2]], writes=[ps_b])
            r = rl[f % 2]
            p.op("act", lambda e, ps=ps, r=r: e.activation(out=r, in_=ps[:, 0:TB], func=AF.Relu),
                 reads=[ps_b], writes=[rl_b[f % 2]])
            p.op("dve", lambda e, r=r, f=f: e.tensor_tensor(out=actT[:, f, :], in0=r, in1=r, op=ALU.mult),
                 reads=[rl_b[f % 2]], writes=[actT_b[f]])
        for j in range(NJ):
            t = b * NJ + j
            s = tiles[j]
            for half in range(2):
                ps = c.psum[4 + (2 * j + half) % 4]
                ps_b = c.psum_b[4 + (2 * j + half) % 4]
                for f in range(32):
                    p.op("pe", lambda e, ps=ps, f=f, j=j, half=half: e.matmul(
                        ps[:, 0:512], lhsT=actT[:, f, j * 128:(j + 1) * 128],
                        rhs=wdn[:, f, half * 512:(half + 1) * 512], start=(f == 0), stop=(f == 31)),
                         reads=[wdn_b, actT_b[f]], writes=[ps_b])
                p.op("dve", lambda e, ps=ps, s=s, half=half: e.tensor_tensor(
                    out=xt[s][:, half * 512:(half + 1) * 512], in0=ps[:, 0:512],
                    in1=xt[s][:, half * 512:(half + 1) * 512], op=ALU.add),
                     reads=[ps_b, xt_b[s]], writes=[xt_b[s]])
            if final_g is None:
                p.dma("sp", h_out[t * 128:(t + 1) * 128, :], xt[s], reads=[xt_b[s]])
            else:
                ss = t % 4
                p.op("act", lambda e, s=s, ss=ss: e.activation(out=junk, in_=xt[s], func=AF.Square,
                                                               accum_out=stat[ss][:, 0:1]),
                     reads=[xt_b[s]], writes=[junk_b, stat_b[ss]])
                rstd_ops(c, stat[ss], stat_b[ss], 1.0 / D)
                p.op("dve", lambda e, s=s, ss=ss: e.scalar_tensor_tensor(out=xt[s], in0=xt[s], scalar=stat[ss][:, 2:3],
                                                                         in1=fg, op0=ALU.mult, op1=ALU.mult),
                     reads=[xt_b[s], stat_b[ss], fg_b], writes=[xt_b[s]])
                p.dma("sp", out_dram[t * 128:(t + 1) * 128, :], xt[s], reads=[xt_b[s]], is_out=True)

    prev = None
    for b in range(nblk + 1):
        cur = stage1(b) if b < nblk else None
        if prev is not None:
            stage2(b - 1, prev)
        prev = cur
    p.barrier()
    ar.pop()
    ar.release_guard()


def sincos_from_turns(c, t, t_b, n, barrier=True):
    p, ar = c.p, c.ar
    if barrier:
        ar.push()
        ni = ar.alloc([n], I32)
        nf = ar.alloc([n], F32)
        tb = Buf("sc_tmp")
    else:
        if not hasattr(c, "sc_ni"):
            c.sc_ni = ar.alloc([n], I32)
            c.sc_nf = ar.alloc([n], F32)
            c.sc_tb = Buf("sc_tmpR")
        ni, nf, tb = c.sc_ni, c.sc_nf, c.sc_tb
    p.op("dve", lambda e: e.tensor_copy(out=ni, in_=t), reads=[t_b], writes=[tb])
    p.op("dve", lambda e: e.tensor_copy(out=nf, in_=ni), reads=[tb], writes=[tb])
    p.op("dve", lambda e: e.tensor_tensor(out=t, in0=t, in1=nf, op=ALU.subtract), reads=[t_b, tb], writes=[t_b])
    p.op("dve", lambda e: e.tensor_scalar(out=nf, in0=t, scalar1=0.5, scalar2=None, op0=ALU.is_gt), reads=[t_b], writes=[tb])
    p.op("dve", lambda e: e.tensor_tensor(out=t, in0=t, in1=nf, op=ALU.subtract), reads=[t_b, tb], writes=[t_b])
    p.op("act", lambda e: e.activation(out=t, in_=t, func=AF.Sin, scale=2 * math.pi * (1 - 1e-6)), reads=[t_b], writes=[t_b])
    if barrier:
        p.barrier()
        ar.pop()


def dsa_phase(c, h_in, h_out):
    p, ar = c.p, c.ar
    S = c.S
    NT = S // 128
    topk = min(256, S // 4)
    NIT = 14
    PS = c.ps_all

    def bank(i, n=1):
        return PS[:, i * 512:(i + n) * 512]

    def bankbf(i):
        return PS[:, i * 512:(i + 1) * 512].bitcast(BF16).rearrange("p (a b) -> p a b", b=128)
    pb = c.psum_b
    qT_d = c.qT_d
    qiT_d = c.qiT_d

    ar.push()
    kT = ar.alloc([S], BF16)
    V = ar.alloc([NT, 128], BF16)
    kiT2 = ar.alloc([S], BF16)
    wi = ar.alloc([NT, 8], F32)
    wout = ar.alloc([8, D], BF16)
    tab = ar.alloc([NT, 48], F32)
    posi = ar.alloc([NT], I32)
    posf = ar.alloc([NT], F32)
    gbc = ar.alloc([D], F32)
    kT_b = [Buf("kT%d" % t) for t in range(NT)]
    V_b = [Buf("V%d" % t) for t in range(NT)]
    ki_b = [Buf("ki%d" % t) for t in range(NT)]
    wi_b = [Buf("wi%d" % t) for t in range(NT)]
    qTd_b = [Buf("qTd%d" % t) for t in range(NT)]
    qiTd_b = [Buf("qiTd%d" % t) for t in range(NT)]
    wout_b, tab_b, pos_b, gbc_b = Buf("wout"), Buf("tab"), Buf("pos"), Buf("gbcA")
    K = c.K

    p.dma("sp", gbc, c.norm_mix_g[0:1, :].partition_broadcast(128), writes=[gbc_b])
    p.dma("sp", posi, c.pos, writes=[pos_b])
    p.op("dve", lambda e: e.tensor_copy(out=posf, in_=posi), reads=[pos_b], writes=[pos_b])
    p.op("dve", lambda e: e.tensor_tensor(out=tab, in0=posf.unsqueeze(2).broadcast_to([128, NT, 48]),
                                          in1=K["invrow"].unsqueeze(1).broadcast_to([128, NT, 48]), op=ALU.mult),
         reads=[pos_b, c.K_b], writes=[tab_b])
    p.op("dve", lambda e: e.tensor_tensor(out=tab, in0=tab, in1=K["offs"].unsqueeze(1).broadcast_to([128, NT, 48]),
                                          op=ALU.add), reads=[tab_b, c.K_b], writes=[tab_b])
    tabf = tab.rearrange("p a b -> p (a b)")
    if c.dbg is not None:
        p.dma("sp", c.dbgt[:, NT * 48:NT * 49], posf, reads=[pos_b], is_out=True)
    sincos_from_turns(c, tabf, tab_b, NT * 48)
    if c.dbg is not None:
        p.dma("sp", c.dbgt[:, 0:NT * 48], tabf, reads=[tab_b], is_out=True)

    ar.push()
    win = ar.alloc([8, A_IN], BF16)
    win_b = Buf("win")
    load_weight_bf16(c, c.w_in_a, win, win_b, D, A_IN)
    load_weight_bf16(c, c.w_out_a, wout, wout_b, D, D)
    xt = [ar.alloc([D], F32) for _ in range(2)]
    xt_b = [Buf("xtA%d" % i) for i in range(2)]
    hn = [ar.alloc([D], BF16) for _ in range(2)]
    hn_b = [Buf("hnA%d" % i) for i in range(2)]
    junk = ar.alloc([D], BF16)
    junk_b = Buf("junkA")
    stat = [ar.alloc([4], F32) for _ in range(2)]
    stat_b = [Buf("statA%d" % i) for i in range(2)]
    hnT = [ar.alloc([8, 128], BF16) for _ in range(2)]
    hnT_b = [Buf("hnTA%d" % i) for i in range(2)]
    proj = [ar.alloc([A_IN], F32) for _ in range(2)]
    proj_b = [[Buf("proj%d_%d" % (a, i)) for i in range(4)] for a in range(2)]
    qkb = ar.alloc([9, 128], BF16)
    qkb_b = Buf("qkb")
    ixb = ar.alloc([10, 64], BF16)
    ixb_b = Buf("ixb")
    tA = [ar.alloc([9, 16], F32) for _ in range(4)]
    tA_b = [Buf("tA%d" % i) for i in range(4)]
    tI = [ar.alloc([9, 8], F32) for _ in range(4)]
    tI_b = [Buf("tI%d" % i) for i in range(4)]
    qTt = [ar.alloc([8, 128], BF16) for _ in range(2)]
    qTt_b = [Buf("qTt%d" % i) for i in range(2)]
    qiTt = [ar.alloc([4, 128], BF16) for _ in range(2)]
    qiTt_b = [Buf("qiTt%d" % i) for i in range(2)]
    chunks = [(0, 512), (512, 512), (1024, 512), (1536, A_IN - 1536)]

    def a1_front(t):
        s = t % 2
        p.dma("sp", xt[s], h_in[t * 128:(t + 1) * 128, :], writes=[xt_b[s]])
        rmsnorm_tile(c, xt[s], xt_b[s], gbc, gbc_b, hn[s], hn_b[s], junk, junk_b, stat[s], stat_b[s])
        transpose_to(c, hn[s], hn_b[s], 8, bankbf(0), pb[0], hnT[s], hnT_b[s])
        for ci, (c0, w) in enumerate(chunks):
            ps = bank(1 + ci)
            for k in range(8):
                p.op("pe", lambda e, ps=ps, k=k, c0=c0, w=w: e.matmul(ps[:, 0:w], lhsT=hnT[s][:, k, :], rhs=win[:, k, c0:c0 + w],
                                                                      start=(k == 0), stop=(k == 7)),
                     reads=[hnT_b[s], win_b], writes=[pb[1 + ci]])
            p.op("act", lambda e, ps=ps, c0=c0, w=w: e.copy(out=proj[s][:, c0:c0 + w], in_=ps[:, 0:w]),
                 reads=[pb[1 + ci]], writes=[proj_b[s][ci]])

    def a1_back(t):
        s = t % 2
        pj = proj[s]

        def pbufs(lo, hi):
            return [proj_b[s][i] for i, (c0, w) in enumerate(chunks) if c0 < hi and c0 + w > lo]
        pa = pj[:, 0:1152].rearrange("p (h d) -> p h d", h=9)
        cosA = tab[:, t, 16:32].unsqueeze(1).broadcast_to([128, 9, 16])
        sinA = tab[:, t, 0:16].unsqueeze(1).broadcast_to([128, 9, 16])
        rA = pbufs(0, 1152)
        p.op("dve", lambda e: e.tensor_tensor(out=tA[0], in0=pa[:, :, 0:16], in1=cosA, op=ALU.mult), reads=rA + [tab_b], writes=[tA_b[0]])
        p.op("dve", lambda e: e.tensor_tensor(out=tA[1], in0=pa[:, :, 16:32], in1=sinA, op=ALU.mult), reads=rA + [tab_b], writes=[tA_b[1]])
        p.op("dve", lambda e: e.tensor_tensor(out=tA[2], in0=pa[:, :, 16:32], in1=cosA, op=ALU.mult), reads=rA + [tab_b], writes=[tA_b[2]])
        p.op("dve", lambda e: e.tensor_tensor(out=tA[3], in0=pa[:, :, 0:16], in1=sinA, op=ALU.mult), reads=rA + [tab_b], writes=[tA_b[3]])
        p.op("dve", lambda e: e.tensor_tensor(out=qkb[:, :, 0:16], in0=tA[0], in1=tA[1], op=ALU.subtract), reads=[tA_b[0], tA_b[1]], writes=[qkb_b])
        p.op("dve", lambda e: e.tensor_tensor(out=qkb[:, :, 16:32], in0=tA[2], in1=tA[3], op=ALU.add), reads=[tA_b[2], tA_b[3]], writes=[qkb_b])
        p.op("dve", lambda e: e.tensor_copy(out=qkb[:, :, 32:128], in_=pa[:, :, 32:128]), reads=rA, writes=[qkb_b])
        pi_ = pj[:, 1280:1856].rearrange("p (h d) -> p h d", h=9)
        cosI = tab[:, t, 40:48].unsqueeze(1).broadcast_to([128, 9, 8])
        sinI = tab[:, t, 32:40].unsqueeze(1).broadcast_to([128, 9, 8])
        rI = pbufs(1280, 1856)
        p.op("pool", lambda e: e.tensor_tensor(out=tI[0], in0=pi_[:, :, 0:8], in1=cosI, op=ALU.mult), reads=rI + [tab_b], writes=[tI_b[0]])
        p.op("pool", lambda e: e.tensor_tensor(out=tI[1], in0=pi_[:, :, 8:16], in1=sinI, op=ALU.mult), reads=rI + [tab_b], writes=[tI_b[1]])
        p.op("pool", lambda e: e.tensor_tensor(out=tI[2], in0=pi_[:, :, 8:16], in1=cosI, op=ALU.mult), reads=rI + [tab_b], writes=[tI_b[2]])
        p.op("pool", lambda e: e.tensor_tensor(out=tI[3], in0=pi_[:, :, 0:8], in1=sinI, op=ALU.mult), reads=rI + [tab_b], writes=[tI_b[3]])
        p.op("pool", lambda e: e.tensor_tensor(out=ixb[:, 0:9, 0:8], in0=tI[0], in1=tI[1], op=ALU.subtract), reads=[tI_b[0], tI_b[1]], writes=[ixb_b])
        p.op("pool", lambda e: e.tensor_tensor(out=ixb[:, 0:9, 8:16], in0=tI[2], in1=tI[3], op=ALU.add), reads=[tI_b[2], tI_b[3]], writes=[ixb_b])
        p.op("act", lambda e: e.copy(out=ixb[:, 0:9, 16:64], in_=pi_[:, :, 16:64]), reads=rI, writes=[ixb_b])
        p.op("act", lambda e: e.copy(out=ixb[:, 9, :], in_=ixb[:, 8, :]), reads=[ixb_b], writes=[ixb_b])
        p.op("act", lambda e: e.copy(out=V[:, t, :], in_=pj[:, 1152:1280]), reads=pbufs(1152, 1280), writes=[V_b[t]])
        p.op("act", lambda e: e.mul(out=wi[:, t, :], in_=pj[:, 1856:1864], mul=float(512 ** -0.5)), reads=pbufs(1856, 1864), writes=[wi_b[t]])
        qkf = qkb.rearrange("p h d -> p (h d)")
        ixf = ixb.rearrange("p h d -> p (h d)")
        for h in range(8):
            p.op("pe", lambda e, h=h: e.transpose(out=bankbf(5)[:, h, :], in_=qkf[:, h * 128:(h + 1) * 128], identity=c.ident),
                 reads=[qkb_b, c.ident_b], writes=[pb[5]])
        p.op("act", lambda e: e.copy(out=qTt[s], in_=bankbf(5)), reads=[pb[5]], writes=[qTt_b[s]])
        p.dma("sp", qT_d[t], qTt[s], reads=[qTt_b[s]], writes=[qTd_b[t]])
        p.op("pe", lambda e: e.transpose(out=bankbf(6)[:, 0, :], in_=qkf[:, 1024:1152], identity=c.ident),
             reads=[qkb_b, c.ident_b], writes=[pb[6]])
        for g in range(5):
            p.op("pe", lambda e, g=g: e.transpose(out=bankbf(6)[:, 1 + g, :], in_=ixf[:, g * 128:(g + 1) * 128], identity=c.ident),
                 reads=[ixb_b, c.ident_b], writes=[pb[6]])
        p.op("act", lambda e: e.copy(out=kT[:, t * 128:(t + 1) * 128], in_=bankbf(6)[:, 0, :]), reads=[pb[6]], writes=[kT_b[t]])
        p.op("act", lambda e: e.copy(out=qiTt[s], in_=bankbf(6)[:, 1:5, :]), reads=[pb[6]], writes=[qiTt_b[s]])
        p.op("act", lambda e: e.copy(out=kiT2[:, t * 128:(t + 1) * 128], in_=bankbf(6)[:, 5, :]), reads=[pb[6]], writes=[ki_b[t]])
        p.dma("sp", qiT_d[t], qiTt[s], reads=[qiTt_b[s]], writes=[qiTd_b[t]])

    a1_front(0)
    for t in range(NT):
        if t + 1 < NT:
            a1_front(t + 1)
        a1_back(t)
    p.barrier()
    ar.pop()

    mb_d = c.mb_d
    mbd_b = [Buf("mbd%d" % t) for t in range(NT)]
    ar.push()
    rh = [ar.alloc([8, 512], BF16) for _ in range(2)]
    rh_b = [[Buf("rh%d_%d" % (i, h)) for h in range(8)] for i in range(2)]
    diag = [ar.alloc([8, 128], BF16) for _ in range(2)]
    diag_b = [Buf("diag%d" % i) for i in range(2)]
    score = [ar.alloc([S], F32) for _ in range(3)]
    score_b = [Buf("score%d" % i) for i in range(3)]
    cjunk = ar.alloc([S], BF16)
    cjunk_b = Buf("cjunk")
    mbx = [ar.alloc([S], BF16) for _ in range(2)]
    mbx_b = [Buf("mbx%d" % i) for i in range(2)]
    qiTs = [ar.alloc([4, 128], BF16) for _ in range(2)]
    qiTs_b = [Buf("qiTs%d" % i) for i in range(2)]
    bs = [ar.alloc([8 + NIT], F32) for _ in range(3)]
    bs_b = [Buf("bs%d" % i) for i in range(3)]
    bs2 = [ar.alloc([4], F32) for _ in range(3)]
    bs2_b = [Buf("bs2_%d" % i) for i in range(3)]
    bs2a_b = [Buf("bs2a_%d" % i) for i in range(3)]
    ajunk = ar.alloc([S], BF16)
    ajunk_b = Buf("ajunk")
    bmx = [ar.alloc([16], F32) for _ in range(3)]
    bmx_b = [Buf("bmx%d" % i) for i in range(3)]
    cjunks = [cjunk, ar.alloc([S], BF16)]
    cjunks_b = [cjunk_b, Buf("cjunk1")]
    ajunks = [ajunk, ar.alloc([S], BF16)]
    ajunks_b = [ajunk_b, Buf("ajunk1")]

    def x_loads(i):
        p.dma("sp", qiTs[i % 2], qiT_d[i], reads=[qiTd_b[i]], writes=[qiTs_b[i % 2]])

    def bis_steps(i, n):
        s = i % 3
        sj = i % 2
        L = (i + 1) * 128
        b_, bb = bs[s], bs_b[s]
        LA = (L // 384) * 128
        sc = score[s]
        thr = float(topk) - 0.5 - 0.5 * LA
        steps = []
        steps.append(lambda: p.op("dve", lambda e: e.tensor_tensor(out=b_[:, 2:3], in0=b_[:, 0:1], in1=b_[:, 8 + n:9 + n], op=ALU.add),
                                  reads=[bb], writes=[bb]))

        def counts():
            if LA > 0:
                p.op("act", lambda e: e.activation(out=ajunks[sj][:, 0:LA], in_=sc[:, 0:LA], func=AF.Sign, bias=b_[:, 2:3], scale=-1.0,
                                                   accum_out=bs2[s][:, 1:2]), reads=[bb, score_b[s]], writes=[bs2a_b[s], ajunks_b[sj]])
            p.op("dve", lambda e: e.tensor_scalar(out=cjunks[sj][:, LA:L], in0=sc[:, LA:L], scalar1=b_[:, 2:3], scalar2=None,
                                                  op0=ALU.is_ge, op1=ALU.add, accum_out=b_[:, 3:4]),
                 reads=[bb, score_b[s]], writes=[bb, cjunks_b[sj]])
        steps.append(counts)
        if LA > 0:
            steps.append(lambda: p.op("dve", lambda e: e.scalar_tensor_tensor(out=b_[:, 3:4], in0=bs2[s][:, 1:2], scalar=-0.5, in1=b_[:, 3:4],
                                                                              op0=ALU.mult, op1=ALU.add), reads=[bb, bs2a_b[s]], writes=[bb]))
        steps.append(lambda: p.op("dve", lambda e: e.scalar_tensor_tensor(out=b_[:, 4:5], in0=b_[:, 3:4], scalar=thr,
                                                                          in1=b_[:, 8 + n:9 + n], op0=ALU.is_ge, op1=ALU.mult),
                                  reads=[bb], writes=[bb]))
        steps.append(lambda: p.op("dve", lambda e: e.tensor_tensor(out=b_[:, 0:1], in0=b_[:, 0:1], in1=b_[:, 4:5], op=ALU.add),
                                  reads=[bb], writes=[bb]))
        return steps

    def x_final(i):
        s = i % 3
        sm = i % 2
        L = (i + 1) * 128
        b_, bb = bs[s], bs_b[s]
        m_ = mbx[sm]
        sc = score[s][:, 0:L]
        p.op("dve", lambda e: e.tensor_scalar(out=m_[:, 0:L], in0=sc, scalar1=b_[:, 0:1], scalar2=NEG,
                                              op0=ALU.is_lt, op1=ALU.mult),
             reads=[score_b[s], bb], writes=[mbx_b[sm]])
        p.dma("sp", mb_d[i, :, 0:L], m_[:, 0:L], reads=[mbx_b[sm]], writes=[mbd_b[i]])

    def x_chunk(i, ch, nch):
        s = i % 3
        sq = i % 2
        L = (i + 1) * 128
        b_, bb = bs[s], bs_b[s]
        sco, scb = score[s], score_b[s]
        k0 = ch * 512
        w = min(512, L - k0)
        rr = rh[ch % 2]
        rrb = rh_b[ch % 2]
        kb = [ki_b[t] for t in range(k0 // 128, (k0 + w) // 128)]
        for h in range(8):
            hp, j = h % 2, h // 2
            bi = h % 2
            ps = bank(bi)
            p.op("pe", lambda e, ps=ps, hp=hp, j=j: e.matmul(
                ps[:, 0:w], lhsT=qiTs[sq][hp * 64:(hp + 1) * 64, j, :], rhs=kiT2[hp * 64:(hp + 1) * 64, k0:k0 + w],
                start=True, stop=True), reads=[qiTs_b[sq]] + kb, writes=[pb[bi]])
            p.op("act", lambda e, ps=ps, h=h: e.activation(out=rr[:, h, 0:w], in_=ps[:, 0:w], func=AF.Relu),
                 reads=[pb[bi]], writes=[rrb[h]])
        b2 = 2 + ch % 2
        ps2 = bank(b2)
        for h in range(8):
            p.op("pe", lambda e, h=h: e.matmul(ps2[:, 0:w], lhsT=diag[sq][:, h, :], rhs=rr[:, h, 0:w],
                                               start=(h == 0), stop=(h == 7)),
                 reads=[diag_b[sq], rrb[h]], writes=[pb[b2]])
        last = (ch == nch - 1)
        wc = w - 128 if last else w
        mx, mxb = bmx[s], bmx_b[s]
        if wc > 0:
            p.op("dve", lambda e: e.tensor_scalar(out=sco[:, k0:k0 + wc], in0=ps2[:, 0:wc], scalar1=1.0, scalar2=None,
                                                  op0=ALU.mult, op1=ALU.max, accum_out=mx[:, ch:ch + 1]),
                 reads=[pb[b2]], writes=[scb, mxb])
        if last:
            p.op("dve", lambda e: e.tensor_reduce(out=mx[:, 8:9], in_=ps2[:, w - 128:w], axis=AX.X, op=ALU.max),
                 reads=[pb[b2]], writes=[mxb])
            p.op("dve", lambda e: e.tensor_reduce(out=b_[:, 5:6], in_=ps2[:, w - 128:w], axis=AX.X, op=ALU.min),
                 reads=[pb[b2]], writes=[bb])
            p.op("dve", lambda e: e.tensor_tensor(out=sco[:, k0 + w - 128:k0 + w], in0=ps2[:, w - 128:w],
                                                  in1=K["causal"], op=ALU.add),
                 reads=[pb[b2], c.K_b], writes=[scb])

    def x_setup(i):
        s = i % 3
        L = (i + 1) * 128
        b_, bb = bs[s], bs_b[s]
        sco, scb = score[s], score_b[s]
        sc = sco[:, 0:L]
        nchx = (L + 511) // 512
        lastw = L - (nchx - 1) * 512
        c0x = 0 if lastw > 128 else nchx - 1
        mx, mxb = bmx[s], bmx_b[s]
        if lastw == 128:
            cols = list(range(0, nchx - 1)) + [8]
        else:
            cols = list(range(0, nchx)) + [8]
        p.op("dve", lambda e: e.tensor_copy(out=b_[:, 1:2], in_=mx[:, 8:9]), reads=[mxb], writes=[bb])
        if len(cols) > 1:
            lo_c, hi_c = cols[0], cols[-2] + 1
            p.op("dve", lambda e: e.tensor_reduce(out=b_[:, 6:7], in_=mx[:, lo_c:hi_c], axis=AX.X, op=ALU.max), reads=[mxb], writes=[bb])
            p.op("dve", lambda e: e.tensor_tensor(out=b_[:, 1:2], in0=b_[:, 1:2], in1=b_[:, 6:7], op=ALU.max), reads=[bb], writes=[bb])
        if L > 128:
            p.op("dve", lambda e: e.tensor_reduce(out=b_[:, 0:1], in_=sco[:, 0:L - 128], axis=AX.X, op=ALU.min),
                 reads=[scb], writes=[bb])
            p.op("dve", lambda e: e.tensor_tensor(out=b_[:, 0:1], in0=b_[:, 0:1], in1=b_[:, 5:6], op=ALU.min), reads=[bb], writes=[bb])
        else:
            p.op("dve", lambda e: e.tensor_copy(out=b_[:, 0:1], in_=b_[:, 5:6]), reads=[bb], writes=[bb])
        p.op("dve", lambda e: e.tensor_tensor(out=b_[:, 1:2], in0=b_[:, 1:2], in1=b_[:, 0:1], op=ALU.subtract), reads=[bb], writes=[bb])
        p.op("dve", lambda e: e.tensor_tensor(out=b_[:, 8:8 + NIT], in0=b_[:, 1:2].broadcast_to([128, NIT]),
                                              in1=K["pow2"][:, 0:NIT], op=ALU.mult), reads=[bb, c.K_b], writes=[bb])

    x_loads(0)
    H1 = NIT // 2
    for i in range(NT + 2):
        if i + 1 < NT:
            x_loads(i + 1)
        its = []
        ia = list(range(0, H1)) if 1 <= i <= NT else []
        ib = list(range(H1, NIT)) if 2 <= i <= NT + 1 else []
        while ia or ib:
            sa = bis_steps(i - 1, ia.pop(0)) if ia else []
            sb = bis_steps(i - 2, ib.pop(0)) if ib else []
            while sa or sb:
                if sa:
                    its.append(sa.pop(0))
                if sb:
                    its.append(sb.pop(0))
        if i < NT:
            sq = i % 2
            p.op("pool", lambda e, sq=sq, i=i: e.tensor_tensor(out=diag[sq], in0=c.ident.unsqueeze(1).broadcast_to([128, 8, 128]),
                                                               in1=wi[:, i, :].unsqueeze(2).broadcast_to([128, 8, 128]), op=ALU.mult),
                 reads=[c.ident_b, wi_b[i]], writes=[diag_b[sq]])
            nch = ((i + 1) * 128 + 511) // 512
            per = (len(its) + nch - 1) // nch if its else 0
            for ch in range(nch):
                x_chunk(i, ch, nch)
                for _ in range(per):
                    if its:
                        its.pop(0)()
        while its:
            its.pop(0)()
        if 2 <= i <= NT + 1:
            x_final(i - 2)
        if i < NT:
            x_setup(i)
    p.barrier()
    ar.pop()

    ar.push()
    mb = [ar.alloc([S], BF16) for _ in range(2)]
    mb_b = [Buf("mb%d" % i) for i in range(2)]
    pT = [ar.alloc([4, 128], BF16) for _ in range(4)]
    pT_b = [Buf("pT%d" % i) for i in range(4)]
    recip = [ar.alloc([512], F32) for _ in range(2)]
    recip_b = [Buf("recip%d" % i) for i in range(2)]
    oT = [ar.alloc([8, 128], BF16) for _ in range(2)]
    oT_b = [[Buf("oT%d_%d" % (i, hh)) for hh in range(2)] for i in range(2)]
    qTs = [ar.alloc([8, 128], BF16) for _ in range(2)]
    qTs_b = [Buf("qTs%d" % i) for i in range(2)]
    xo = [ar.alloc([D], F32) for _ in range(3)]
    xo_b = [Buf("xo%d" % i) for i in range(3)]
    scale = float(DH_A ** -0.5)
    pti = [0]

    def y_loads(i):
        s = i % 2
        L = (i + 1) * 128
        p.dma("sp", qTs[s], qT_d[i], reads=[qTd_b[i]], writes=[qTs_b[s]])
        p.dma("sp", mb[s][:, 0:L], mb_d[i, :, 0:L], reads=[mbd_b[i]], writes=[mb_b[s]])
        p.dma("sp", xo[i % 3], h_in[i * 128:(i + 1) * 128, :], writes=[xo_b[i % 3]])

    def Yhalf(i, hh):
        s = i % 2
        m_ = mb[s]
        o_ = oT[s]
        bo, bl = (5, 6) if hh == 0 else (0, 1)
        pts = {}

        def qk(kt):
            bi = 3 + kt % 2
            ab = bank(bi)
            p.op("pe", lambda e: e.matmul(ab, lhsT=kT[:, kt * 128:(kt + 1) * 128],
                                          rhs=qTs[s][:, hh * 4:(hh + 1) * 4, :], start=True, stop=False),
                 reads=[kT_b[kt], qTs_b[s]], writes=[pb[bi]])
            p.op("pe", lambda e: e.matmul(ab, lhsT=m_[:, kt * 128:(kt + 1) * 128], rhs=K["I4"], start=False, stop=True),
                 reads=[mb_b[s], c.K_b], writes=[pb[bi]])
            pt = pT[pti[0] % 4]
            ptb = pT_b[pti[0] % 4]
            pti[0] += 1
            pts[kt] = (pt, ptb)
            p.op("act", lambda e: e.activation(out=pt.rearrange("p a b -> p (a b)"), in_=ab, func=AF.Exp, scale=scale),
                 reads=[pb[bi]], writes=[ptb])

        def pv(kt):
            pt, ptb = pts.pop(kt)
            p.op("pe", lambda e: e.matmul(bank(bo), lhsT=V[:, kt, :], rhs=pt.rearrange("p a b -> p (a b)"),
                                          start=(kt == 0), stop=(kt == i)),
                 reads=[V_b[kt], ptb], writes=[pb[bo]])
            p.op("pe", lambda e: e.matmul(bank(bl), lhsT=K["ones"], rhs=pt.rearrange("p a b -> p (a b)"),
                                          start=(kt == 0), stop=(kt == i)),
                 reads=[c.K_b, ptb], writes=[pb[bl]])

        qk(0)
        for kt in range(i + 1):
            if kt + 1 <= i:
                qk(kt + 1)
            pv(kt)
        rc, rcb = recip[hh], recip_b[hh]
        p.op("act", lambda e: e.activation(out=rc, in_=bank(bl), func=AF.Ln), reads=[pb[bl]], writes=[rcb])
        p.op("act", lambda e: e.activation(out=rc, in_=rc, func=AF.Exp, scale=-1.0), reads=[rcb], writes=[rcb])
        p.op("dve", lambda e: e.tensor_tensor(out=o_[:, hh * 4:(hh + 1) * 4, :].rearrange("p a b -> p (a b)"), in0=bank(bo),
                                              in1=rc, op=ALU.mult),
             reads=[pb[bo], rcb], writes=[oT_b[s][hh]])

    def Yout(i):
        s = i % 2
        o_ = oT[s]
        for half in range(2):
            bi = 2 if half == 0 else 7
            ps = bank(bi)
            for h in range(8):
                p.op("pe", lambda e, h=h, ps=ps, half=half: e.matmul(ps, lhsT=o_[:, h, :], rhs=wout[:, h, half * 512:(half + 1) * 512],
                                                                     start=(h == 0), stop=(h == 7)),
                     reads=[oT_b[s][h // 4], wout_b], writes=[pb[bi]])
            p.op("dve", lambda e, half=half, ps=ps: e.tensor_tensor(out=xo[i % 3][:, half * 512:(half + 1) * 512], in0=ps,
                                                             in1=xo[i % 3][:, half * 512:(half + 1) * 512], op=ALU.add),
                 reads=[pb[bi], xo_b[i % 3]], writes=[xo_b[i % 3]])
        p.dma("sp", h_out[i * 128:(i + 1) * 128, :], xo[i % 3], reads=[xo_b[i % 3]], writes=[c.hres_b[i]])

    pf = []
    if c.prefetch:
        TOP = ar.nbytes - 65536
        c.pre_up0 = (ar.alloc_at(TOP, [8, DFF], BF16), Buf("wup_pre0"))
        pst = [ar.alloc([1024], F32) for _ in range(2)]
        pst_b = [Buf("pstA%d" % i) for i in range(2)]
        pf = prefetch_pieces(c, c.w_mlp_up[0], c.pre_up0[0], c.pre_up0[1], D, DFF, pst, pst_b, "sp", "dve")
    npf = (len(pf) + NT - 1) // NT if pf else 0
    y_loads(0)
    for i in range(NT):
        if i + 1 < NT:
            y_loads(i + 1)
        for _ in range(npf):
            if pf:
                pf.pop(0)()
        Yhalf(i, 0)
        Yhalf(i, 1)
        if i >= 1:
            Yout(i - 1)
    Yout(NT - 1)
    p.barrier()
    ar.pop()
    ar.pop()


def ret_phase(c, h_in, h_out):
    p, ar = c.p, c.ar
    S = c.S
    NT = S // 128
    PS = c.ps_all
    pb = c.psum_b
    K = c.K
    kf = K["f"]

    def bank(i, n=1):
        return PS[:, i * 512:(i + n) * 512]

    def bankbf(i):
        return PS[:, i * 512:(i + 1) * 512].bitcast(BF16).rearrange("p (a b) -> p a b", b=128)
    ygT_d = c.ygT_d
    rp_d = c.rp_d
    ygd_b = [Buf("ygd%d" % t) for t in range(NT)]
    rpd_b = [Buf("rpd%d" % t) for t in range(NT)]
    RC = 368
    decT = kf[:, RC:RC + 512].rearrange("p (h n) -> p h n", h=4)
    xi = kf[:, RC + 512:RC + 516]
    zeta = kf[:, RC + 516:RC + 520]
    invr = kf[:, RC + 520:RC + 648]
    lg = [math.log1p(-2.0 ** (-5.0 - h)) for h in range(4)]
    gam_c = [float(math.exp(128.0 * x)) for x in lg]
    OQ, OK_, OKZ, OV, OSG = 0, 1024, 2048, 3072, 5120

    ar.push()
    win = ar.alloc([8, B_IN], BF16)
    win_b = Buf("winB")
    gbc = ar.alloc([D], F32)
    gbc_b = Buf("gbcR")
    posi = ar.alloc([NT], I32)
    posf = ar.alloc([NT], F32)
    pos_b = Buf("posR")
    p.dma("sp", gbc, c.norm_mix_g[1:2, :].partition_broadcast(128), writes=[gbc_b])
    p.dma("sp", posi, c.pos, writes=[pos_b])
    p.op("dve", lambda e: e.tensor_copy(out=posf, in_=posi), reads=[pos_b], writes=[pos_b])
    load_weight_bf16(c, c.w_in_b, win, win_b, D, B_IN)
    xt = [ar.alloc([D], F32) for _ in range(2)]
    xt_b = [Buf("xtR%d" % i) for i in range(2)]
    hn = [ar.alloc([D], BF16) for _ in range(2)]
    hn_b = [Buf("hnR%d" % i) for i in range(2)]
    junk = ar.alloc([D], BF16)
    junk_b = Buf("junkR")
    stat = [ar.alloc([4], F32) for _ in range(2)]
    stat_b = [Buf("statR%d" % i) for i in range(2)]
    hnT = [ar.alloc([8, 128], BF16) for _ in range(2)]
    hnT_b = [Buf("hnTR%d" % i) for i in range(2)]
    tb = [ar.alloc([4, 128], F32) for _ in range(2)]
    tb_b = [Buf("tbR%d" % i) for i in range(2)]
    tts = [[ar.alloc([2, 128], F32) for _ in range(4)] for _ in range(2)]
    tts_b = [[Buf("ttR%d_%d" % (a, i)) for i in range(4)] for a in range(2)]
    rot = [ar.alloc([2, 256], BF16) for _ in range(2)]
    rot_b = [Buf("rotR%d" % i) for i in range(2)]
    pk = [ar.alloc([7168], BF16) for _ in range(2)]
    pk_b = [Buf("pkR%d" % i) for i in range(2)]
    cnt = [0]

    def stageA(t):
        s = t % 2
        p.dma("sp", xt[s], h_in[t * 128:(t + 1) * 128, :], writes=[xt_b[s]])
        rmsnorm_tile(c, xt[s], xt_b[s], gbc, gbc_b, hn[s], hn_b[s], junk, junk_b, stat[s], stat_b[s])
        transpose_to(c, hn[s], hn_b[s], 8, bankbf(0), pb[0], hnT[s], hnT_b[s])
        tb_, tbb = tb[s], tb_b[s]
        p.op("dve", lambda e: e.tensor_scalar(out=tb_[:, 0, :], in0=invr, scalar1=posf[:, t:t + 1], scalar2=None, op0=ALU.mult),
             reads=[pos_b, c.K_b], writes=[tbb])
        p.op("dve", lambda e: e.tensor_scalar(out=tb_[:, 1, :], in0=tb_[:, 0, :], scalar1=0.25, scalar2=None, op0=ALU.add),
             reads=[tbb], writes=[tbb])
        sincos_from_turns(c, tb_[:, 0:2, :].rearrange("p a b -> p (a b)"), tbb, 256, barrier=False)
        p.op("dve", lambda e: e.tensor_scalar(out=tb_[:, 2:4, :], in0=tb_[:, 0:2, :], scalar1=float(DK_R ** -0.5), scalar2=None, op0=ALU.mult),
             reads=[tbb], writes=[tbb])

    def inproj(t, c0, bi):
        s = t % 2
        ps = bank(bi)
        for k in range(8):
            p.op("pe", lambda e, k=k: e.matmul(ps, lhsT=hnT[s][:, k, :], rhs=win[:, k, c0:c0 + 512], start=(k == 0), stop=(k == 7)),
                 reads=[hnT_b[s], win_b], writes=[pb[bi]])
        return ps

    def stageB1(t):
        s = t % 2
        pk_ = pk[s]
        for qk in range(2):
            sinT = tb[s][:, 2 * qk, :].unsqueeze(1).broadcast_to([128, 2, 128])
            cosT = tb[s][:, 2 * qk + 1, :].unsqueeze(1).broadcast_to([128, 2, 128])
            for ci in range(2):
                n = cnt[0]
                cnt[0] += 1
                bi = 1 + n % 3
                ps = inproj(t, qk * 1024 + ci * 512, bi)
                px = ps.rearrange("p (h d) -> p h d", h=2)
                tt, tt_b = tts[n % 2], tts_b[n % 2]
                r_, rb = rot[n % 2], rot_b[n % 2]

                def mul(o, ob, a, b_):
                    p.op("dve", lambda e: e.tensor_tensor(out=o, in0=a, in1=b_, op=ALU.mult), reads=[pb[bi], tb_b[s]], writes=[ob])
                mul(tt[0], tt_b[0], px[:, :, 0:128], cosT)
                mul(tt[1], tt_b[1], px[:, :, 128:256], sinT)
                mul(tt[2], tt_b[2], px[:, :, 128:256], cosT)
                mul(tt[3], tt_b[3], px[:, :, 0:128], sinT)
                p.op("dve", lambda e, r_=r_, tt=tt: e.tensor_tensor(out=r_[:, :, 0:128], in0=tt[0], in1=tt[1], op=ALU.subtract),
                     reads=[tt_b[0], tt_b[1]], writes=[rb])
                p.op("dve", lambda e, r_=r_, tt=tt: e.tensor_tensor(out=r_[:, :, 128:256], in0=tt[2], in1=tt[3], op=ALU.add),
                     reads=[tt_b[2], tt_b[3]], writes=[rb])
                if qk == 1:
                    for hh in range(2):
                        h = 2 * ci + hh
                        p.op("act", lambda e, r_=r_, hh=hh, h=h: e.activation(out=pk_[:, OKZ + h * 256:OKZ + (h + 1) * 256], in_=r_[:, hh, :],
                                                                              func=AF.Identity, scale=zeta[:, h:h + 1]),
                             reads=[rb, c.K_b], writes=[pk_b[s]])
                rf = r_.rearrange("p h d -> p (h d)")
                tbi = 4 + n % 2
                for j in range(4):
                    p.op("pe", lambda e, rf=rf, j=j, tbi=tbi: e.transpose(out=bankbf(tbi)[:, j, :], in_=rf[:, j * 128:(j + 1) * 128], identity=c.ident),
                         reads=[rb, c.ident_b], writes=[pb[tbi]])
                o0 = (OQ if qk == 0 else OK_) + ci * 512
                p.op("act", lambda e, o0=o0, tbi=tbi: e.copy(out=pk_[:, o0:o0 + 512].rearrange("p (a b) -> p a b", a=4), in_=bankbf(tbi)[:, 0:4, :]),
                     reads=[pb[tbi]], writes=[pk_b[s]])

    def stageB2(t):
        s = t % 2
        pk_ = pk[s]
        for ci in range(8):
            n = cnt[0]
            cnt[0] += 1
            bi = 1 + n % 3
            ps = inproj(t, 2048 + ci * 512, bi)
            if ci < 4:
                p.op("act", lambda e, ps=ps, ci=ci: e.copy(out=pk_[:, OV + ci * 512:OV + (ci + 1) * 512], in_=ps), reads=[pb[bi]], writes=[pk_b[s]])
            else:
                p.op("act", lambda e, ps=ps, ci=ci: e.activation(out=pk_[:, OSG + (ci - 4) * 512:OSG + (ci - 3) * 512], in_=ps, func=AF.Silu),
                     reads=[pb[bi]], writes=[pk_b[s]])
        p.dma("sp", rp_d[t], pk_, reads=[pk_b[s]], writes=[rpd_b[t]])

    stageA(0)
    for t in range(NT):
        stageB1(t)
        if t + 1 < NT:
            stageA(t + 1)
        stageB2(t)
    p.barrier()
    ar.pop()
    if hasattr(c, "sc_ni"):
        del c.sc_ni

    ar.push()
    state = ar.alloc([4, 2, 512], F32)
    state_bf = ar.alloc([4, 2, 512], BF16)
    st_b = [[Buf("st%d_%d" % (h, dc)) for dc in range(2)] for h in range(4)]
    stbf_b = [[Buf("stbf%d_%d" % (h, dc)) for dc in range(2)] for h in range(4)]
    gng = ar.alloc([2048], F32)
    gng_b = Buf("gng")
    p.dma("sp", gng, c.ret_norm_g[0:1, :].partition_broadcast(128), writes=[gng_b])
    p.op("dve", lambda e: e.memset(state.rearrange("p a b c -> p (a b c)"), 0.0), writes=[b for r in st_b for b in r])
    p.op("dve", lambda e: e.memset(state_bf.rearrange("p a b c -> p (a b c)"), 0.0), writes=[b for r in stbf_b for b in r])
    rp = [ar.alloc([7168], BF16) for _ in range(2)]
    rp_b = [Buf("rp%d" % i) for i in range(2)]
    idt = [ar.alloc([128], BF16) for _ in range(4)]
    idt_b = [Buf("idtR%d" % i) for i in range(4)]
    y = [ar.alloc([512], F32) for _ in range(4)]
    y_b = [Buf("yR%d" % i) for i in range(4)]
    junk2 = ar.alloc([512], BF16)
    junk2_b = Buf("junk2R")
    G = [ar.alloc([512], F32) for _ in range(2)]
    G_b = [Buf("GR%d" % i) for i in range(2)]
    A = [ar.alloc([512], F32) for _ in range(2)]
    A_b = [Buf("AR%d" % i) for i in range(2)]
    yg = ar.alloc([4, 512], BF16)
    yg_b = [Buf("ygR%d" % i) for i in range(4)]
    ygT = [ar.alloc([16, 128], BF16) for _ in range(2)]
    ygT_b = [Buf("ygTR%d" % i) for i in range(2)]
    gs = [ar.alloc([32], F32) for _ in range(2)]
    gs_b = [Buf("gsR%d" % i) for i in range(2)]

    def r1_load(t):
        p.dma("sp", rp[t % 2], rp_d[t], reads=[rpd_b[t]], writes=[rp_b[t % 2]])

    def r1_tile(t):
        s = t % 2
        if t + 1 < NT:
            r1_load(t + 1)
        r_ = rp[s]
        rb = rp_b[s]
        qT = r_[:, OQ:OQ + 1024].rearrange("p (a b) -> p a b", a=8)
        kT = r_[:, OK_:OK_ + 1024].rearrange("p (a b) -> p a b", a=8)
        kz = r_[:, OKZ:OKZ + 1024].rearrange("p (a b) -> p a b", a=4)
        v = r_[:, OV:OV + 2048].rearrange("p (a b) -> p a b", a=4)
        sg = r_[:, OSG:OSG + 2048].rearrange("p (a b) -> p a b", a=4)
        g_, gb = gs[s], gs_b[s]
        for h in range(4):
            pin = bank(6)[:, h * 128:(h + 1) * 128]
            for dc in range(2):
                p.op("pe", lambda e, pin=pin, h=h, dc=dc: e.matmul(pin, lhsT=kT[:, 2 * h + dc, :], rhs=qT[:, 2 * h + dc, :],
                                                                   start=(dc == 0), stop=(dc == 1)),
                     reads=[rb], writes=[pb[6]])
        for h in range(4):
            pin = bank(6)[:, h * 128:(h + 1) * 128]
            p.op("dve", lambda e, pin=pin, h=h: e.tensor_tensor(out=idt[h], in0=pin, in1=decT[:, h, :], op=ALU.mult),
                 reads=[pb[6], c.K_b], writes=[idt_b[h]])
        for h in range(4):
            bo = 7 if h % 2 == 0 else 5
            po = bank(bo)
            p.op("pe", lambda e, po=po, h=h: e.matmul(po, lhsT=idt[h], rhs=v[:, h, :], start=True, stop=False),
                 reads=[idt_b[h], rb], writes=[pb[bo]])
            for dc in range(2):
                p.op("pe", lambda e, po=po, h=h, dc=dc: e.matmul(po, lhsT=qT[:, 2 * h + dc, :], rhs=state_bf[:, h, dc, :],
                                                                 start=False, stop=(dc == 1)),
                     reads=[rb, stbf_b[h][dc]], writes=[pb[bo]])
            p.op("act", lambda e, po=po, h=h: e.activation(out=y[h], in_=po, func=AF.Identity, scale=xi[:, h:h + 1],
                                                           accum_out=g_[:, h:h + 1]),
                 reads=[pb[bo], c.K_b], writes=[y_b[h], gb])
            p.op("act", lambda e, h=h: e.activation(out=junk2, in_=y[h], func=AF.Square, accum_out=g_[:, 4 + h:5 + h]),
                 reads=[y_b[h]], writes=[junk2_b, gb])
        for h in range(4):
            for dc in range(2):
                bu = 4 if dc == 0 else 3
                pu = bank(bu)
                p.op("pe", lambda e, pu=pu, h=h, dc=dc: e.matmul(pu, lhsT=kz[:, h, dc * 128:(dc + 1) * 128], rhs=v[:, h, :], start=True, stop=True),
                     reads=[rb], writes=[pb[bu]])
                p.op("dve", lambda e, pu=pu, h=h, dc=dc: e.scalar_tensor_tensor(out=state[:, h, dc, :], in0=state[:, h, dc, :], scalar=gam_c[h],
                                                                                in1=pu, op0=ALU.mult, op1=ALU.add),
                     reads=[st_b[h][dc], pb[bu]], writes=[st_b[h][dc]])
                p.op("act", lambda e, h=h, dc=dc: e.copy(out=state_bf[:, h, dc, :], in_=state[:, h, dc, :]),
                     reads=[st_b[h][dc]], writes=[stbf_b[h][dc]])
        p.op("dve", lambda e: e.tensor_scalar(out=g_[:, 8:12], in0=g_[:, 0:4], scalar1=1.0 / 512, scalar2=None, op0=ALU.mult), reads=[gb], writes=[gb])
        p.op("dve", lambda e: e.tensor_tensor(out=g_[:, 24:28], in0=g_[:, 8:12], in1=g_[:, 8:12], op=ALU.mult), reads=[gb], writes=[gb])
        p.op("dve", lambda e: e.scalar_tensor_tensor(out=g_[:, 12:16], in0=g_[:, 4:8], scalar=1.0 / 512, in1=g_[:, 24:28],
                                                     op0=ALU.mult, op1=ALU.subtract), reads=[gb], writes=[gb])
        p.op("dve", lambda e: e.tensor_scalar(out=g_[:, 12:16], in0=g_[:, 12:16], scalar1=RMS_EPS, scalar2=None, op0=ALU.add), reads=[gb], writes=[gb])
        p.op("act", lambda e: e.activation(out=g_[:, 12:16], in_=g_[:, 12:16], func=AF.Ln), reads=[gb], writes=[gb])
        p.op("act", lambda e: e.activation(out=g_[:, 16:20], in_=g_[:, 12:16], func=AF.Exp, scale=-0.5), reads=[gb], writes=[gb])
        p.op("dve", lambda e: e.scalar_tensor_tensor(out=g_[:, 20:24], in0=g_[:, 8:12], scalar=-1.0, in1=g_[:, 16:20],
                                                     op0=ALU.mult, op1=ALU.mult), reads=[gb], writes=[gb])
        for h in range(4):
            Gh, Ghb = G[h % 2], G_b[h % 2]
            Ah, Ahb = A[h % 2], A_b[h % 2]
            p.op("dve", lambda e, h=h, Gh=Gh: e.tensor_tensor(out=Gh, in0=gng[:, h * 512:(h + 1) * 512], in1=sg[:, h, :], op=ALU.mult),
                 reads=[gng_b, rb], writes=[Ghb])
            p.op("dve", lambda e, h=h, Ah=Ah: e.tensor_scalar(out=Ah, in0=y[h], scalar1=g_[:, 16 + h:17 + h], scalar2=g_[:, 20 + h:21 + h],
                                                              op0=ALU.mult, op1=ALU.add),
                 reads=[y_b[h], gb], writes=[Ahb])
            p.op("dve", lambda e, h=h, Ah=Ah, Gh=Gh: e.tensor_tensor(out=yg[:, h, :], in0=Ah, in1=Gh, op=ALU.mult), reads=[Ahb, Ghb], writes=[yg_b[h]])
        ygf = yg.rearrange("p h d -> p (h d)")
        yT = ygT[s]
        for half in range(2):
            for j in range(8):
                jj = half * 8 + j
                p.op("pe", lambda e, jj=jj, j=j, half=half: e.transpose(out=bankbf(half)[:, j, :], in_=ygf[:, jj * 128:(jj + 1) * 128], identity=c.ident),
                     reads=[yg_b[jj // 4], c.ident_b], writes=[pb[half]])
            p.op("act", lambda e, yT=yT, half=half: e.copy(out=yT[:, half * 8:(half + 1) * 8, :], in_=bankbf(half)),
                 reads=[pb[half]], writes=[ygT_b[s]])
        p.dma("sp", ygT_d[t], yT, reads=[ygT_b[s]], writes=[ygd_b[t]])

    pf = []
    if c.prefetch:
        TOP = ar.nbytes - 65536
        c.pre_up1 = (ar.alloc_at(TOP, [8, DFF], BF16), Buf("wup_pre1"))
        pst = [ar.alloc([1024], F32) for _ in range(2)]
        pst_b = [Buf("pstR%d" % i) for i in range(2)]
        pf = prefetch_pieces(c, c.w_mlp_up[1], c.pre_up1[0], c.pre_up1[1], D, DFF, pst, pst_b, "sp", "act")
    npf = (len(pf) + NT - 1) // NT if pf else 0
    r1_load(0)
    for t in range(NT):
        for _ in range(npf):
            if pf:
                pf.pop(0)()
        r1_tile(t)
    p.barrier()
    ar.pop()

    ar.push()
    wout = ar.alloc([16, D], BF16)
    wout_b = Buf("woutR")
    load_weight_bf16(c, c.w_out_b, wout, wout_b, 2048, D)
    yl = [ar.alloc([16, 128], BF16) for _ in range(2)]
    yl_b = [Buf("ylR%d" % i) for i in range(2)]
    xo = [ar.alloc([D], F32) for _ in range(2)]
    xo_b = [Buf("xoR%d" % i) for i in range(2)]

    def r2_loads(t):
        p.dma("sp", yl[t % 2], ygT_d[t], reads=[ygd_b[t]], writes=[yl_b[t % 2]])
        p.dma("sp", xo[t % 2], h_in[t * 128:(t + 1) * 128, :], writes=[xo_b[t % 2]])
    pf = []
    if c.prefetch:
        TOP1 = ar.nbytes - 131072
        c.pre_dn1 = (ar.alloc_at(TOP1, [32, D], BF16), Buf("wdn_pre1"))
        pst = [ar.alloc([1024], F32) for _ in range(2)]
        pst_b = [Buf("pstR2%d" % i) for i in range(2)]
        pf = prefetch_pieces(c, c.w_mlp_down[1], c.pre_dn1[0], c.pre_dn1[1], DFF, D, pst, pst_b, "sp", "dve")
    npf = (len(pf) + NT - 1) // NT if pf else 0
    for t in range(NT):
        s = t % 2
        if t == 0:
            r2_loads(0)
        if t + 1 < NT:
            r2_loads(t + 1)
        for _ in range(npf):
            if pf:
                pf.pop(0)()
        for half in range(2):
            ps = bank(2 * s + half)
            for j in range(16):
                p.op("pe", lambda e, ps=ps, j=j, half=half, s=s: e.matmul(ps, lhsT=yl[s][:, j, :], rhs=wout[:, j, half * 512:(half + 1) * 512],
                                                                          start=(j == 0), stop=(j == 15)),
                     reads=[yl_b[s], wout_b], writes=[pb[2 * s + half]])
            p.op("dve", lambda e, ps=ps, half=half, s=s: e.tensor_tensor(out=xo[s][:, half * 512:(half + 1) * 512], in0=ps,
                                                                         in1=xo[s][:, half * 512:(half + 1) * 512], op=ALU.add),
                 reads=[pb[2 * s + half], xo_b[s]], writes=[xo_b[s]])
        p.dma("sp", h_out[t * 128:(t + 1) * 128, :], xo[s], reads=[xo_b[s]], writes=[c.hres_b[t]])
    p.barrier()
    ar.pop()


def dump_phase(c):
    p, ar = c.p, c.ar
    ar.push()
    xt = [ar.alloc([D], F32) for _ in range(2)]
    xb = [Buf("dump%d" % i) for i in range(2)]
    for t in range(c.S // 128):
        p.dma("sp", xt[t % 2], c.hres[t * 128:(t + 1) * 128, :], writes=[xb[t % 2]])
        p.dma("sp", c.out[t * 128:(t + 1) * 128, :], xt[t % 2], reads=[xb[t % 2]], is_out=True)
    p.barrier()
    ar.pop()


NCONST = 368 + 648
FULL_PHASES = ("dsa", "mlp0h", "reth", "mlp1f")


def make_consts():
    k = np.zeros((128, NCONST), np.float32)
    k[:, 0:128] = np.eye(128, dtype=np.float32)
    q = np.arange(128)[:, None]
    kk = np.arange(128)[None, :]
    k[:, 128:256] = np.where(kk <= q, 0.0, -1.0e30).astype(np.float32)
    inv_a = (np.float32(ROPE_THETA) ** (-np.arange(16, dtype=np.float32) / np.float32(16))).astype(np.float32)
    inv_i = (np.float32(ROPE_THETA) ** (-np.arange(8, dtype=np.float32) / np.float32(8))).astype(np.float32)
    k[:, 256:304] = (np.concatenate([inv_a, inv_a, inv_i, inv_i]).astype(np.float64) / (2 * math.pi)).astype(np.float32)[None, :]
    k[:, 304:352] = np.concatenate([np.full(16, 0.0), np.full(16, 0.25), np.full(8, 0.0),
                                    np.full(8, 0.25)]).astype(np.float32)[None, :]
    k[:, 352:368] = (2.0 ** -(np.arange(16) + 1.0)).astype(np.float32)[None, :]
    RC = 368
    m = np.arange(128, dtype=np.float64)
    for h in range(4):
        lgm = math.log1p(-2.0 ** (-5.0 - h))
        dec = np.where(m[None, :] >= m[:, None], np.exp(-(m[:, None] + 1.0) * lgm), 0.0)
        k[:, RC + h * 128:RC + (h + 1) * 128] = dec.astype(np.float32)
        k[:, RC + 512 + h] = np.exp((m + 1.0) * lgm).astype(np.float32)
        k[:, RC + 516 + h] = np.exp((127.0 - m) * lgm).astype(np.float32)
    inv_r = (np.float32(RET_THETA) ** (-np.arange(128, dtype=np.float32) / np.float32(128))).astype(np.float64)
    k[:, RC + 520:RC + 648] = (inv_r / (2 * math.pi)).astype(np.float32)[None, :]
    return k


def build(S, phases=("mlp0",), debug=False):
    nc = bass.Bass("TRN2", target_bir_lowering=False)
    from contextlib import ExitStack
    es = ExitStack()
    c = Ctx()
    c.nc, c.S = nc, S
    c.prefetch = (tuple(phases) == FULL_PHASES)
    c.p = Prog()

    def din(name, shape, dt=F32):
        return nc.dram_tensor(name, shape, dt, kind="ExternalInput").ap()

    c.x = din("x", [S, D])
    c.pos = din("positions", [128, S // 128], I32)
    c.norm_mix_g = din("norm_mix_g", [2, D])
    c.norm_mlp_g = din("norm_mlp_g", [2, D])
    c.w_in_a = din("w_in_a", [D, A_IN])
    c.w_out_a = din("w_out_a", [D, D])
    c.w_in_b = din("w_in_b", [D, B_IN])
    c.ret_norm_g = din("ret_norm_g", [1, 2048])
    c.w_out_b = din("w_out_b", [2048, D])
    c.w_mlp_up = din("w_mlp_up", [2, D, DFF])
    c.w_mlp_down = din("w_mlp_down", [2, DFF, D])
    c.final_norm_g = din("final_norm_g", [1, D])
    c.out = nc.dram_tensor("out", [S, D], F32, kind="ExternalOutput").ap()
    c.hres = nc.dram_tensor("hres", [S, D], F32, kind="Internal").ap()
    c.dbg = nc.dram_tensor("dbg", [S, 32], F32, kind="ExternalOutput").ap() if debug else None
    c.dbgs = nc.dram_tensor("dbgs", [S // 128, 128, S], F32, kind="ExternalOutput").ap() if debug else None
    c.dbgt = nc.dram_tensor("dbgt", [128, (S // 128) * 49], F32, kind="ExternalOutput").ap() if debug else None

    NT = S // 128
    c.qT_d = nc.dram_tensor("qT_d", [NT, 128, 1024], BF16, kind="Internal").ap().rearrange("t p (h q) -> t p h q", h=8)
    c.qiT_d = nc.dram_tensor("qiT_d", [NT, 128, 512], BF16, kind="Internal").ap().rearrange("t p (h q) -> t p h q", h=4)
    c.hres_b = [Buf("hres%d" % t) for t in range(NT)]
    c.mb_d = nc.dram_tensor("mb_d", [NT, 128, S], BF16, kind="Internal").ap()
    c.rp_d = nc.dram_tensor("rp_d", [NT, 128, 7168], BF16, kind="Internal").ap()
    c.ygT_d = nc.dram_tensor("ygT_d", [NT, 128, 2048], BF16, kind="Internal").ap().rearrange("t p (h q) -> t p h q", h=16)
    c.consts_in = din("consts", [128, NCONST])

    c.ar = Arena(nc, es, 206 * 1024)
    c.ps_all = es.enter_context(nc.psum_tensor("ps_all", [128, 4096], F32))[:, :]
    c.psum = [c.ps_all[:, i * 512:(i + 1) * 512] for i in range(8)]
    c.psum_b = [Buf("ps%d" % i) for i in range(8)]
    c.stage = [c.ar.alloc([1024], F32) for _ in range(2)]
    c.stage_b = [Buf("stage%d" % i) for i in range(2)]
    c.stage_i = 0
    kf = c.ar.alloc([NCONST], F32)
    c.K_b = Buf("K")
    c.p.dma("sp", kf, c.consts_in, writes=[c.K_b])
    c.ident = c.ar.alloc([128], BF16)
    c.ident_b = Buf("ident")
    c.p.op("dve", lambda e: e.tensor_copy(out=c.ident, in_=kf[:, 0:128]), reads=[c.K_b], writes=[c.ident_b])
    I4 = c.ar.alloc([4, 128], BF16)
    c.p.op("dve", lambda e: e.tensor_copy(out=I4, in_=kf[:, 0:128].unsqueeze(1).broadcast_to([128, 4, 128])),
           reads=[c.K_b], writes=[c.ident_b])
    ones = c.ar.alloc([128], BF16)
    c.p.op("dve", lambda e: e.memset(ones, 1.0), writes=[c.ident_b])
    c.K = {"causal": kf[:, 128:256], "invrow": kf[:, 256:304], "offs": kf[:, 304:352], "pow2": kf[:, 352:368],
           "I4": I4.rearrange("p a b -> p (a b)"), "ones": ones, "f": kf}
    c.K_b = c.ident_b

    for ph in phases:
        if ph == "mlp0":
            mlp_phase(c, 0, c.x, c.hres)
        elif ph == "mlp0f":
            mlp_phase(c, 0, c.x, None, final_g=c.final_norm_g, out_dram=c.out)
        elif ph == "dsa":
            dsa_phase(c, c.x, c.hres)
        elif ph == "mlp0h":
            mlp_phase(c, 0, c.hres, c.hres, pre_up=getattr(c, "pre_up0", None))
        elif ph == "reth":
            ret_phase(c, c.hres, c.hres)
        elif ph == "mlp1f":
            mlp_phase(c, 1, c.hres, None, final_g=c.final_norm_g, out_dram=c.out,
                      pre_up=getattr(c, "pre_up1", None), pre_dn=getattr(c, "pre_dn1", None))
        elif ph == "ret":
            ret_phase(c, c.x, c.hres)
        elif ph == "dump":
            dump_phase(c)
    c.p.finish()
    c.p.emit(nc, es)
    es.close()
    return nc


def kernel(x, positions, norm_mix_g, norm_mlp_g, w_in_a, w_out_a, w_in_b, ret_norm_g, w_out_b,
           w_mlp_up, w_mlp_down, final_norm_g):
    f = lambda a: np.ascontiguousarray(np.asarray(a, dtype=np.float32))
    x = f(x)
    positions = np.asarray(positions).astype(np.int32)
    B, S, _ = x.shape
    nc = build(S, phases=FULL_PHASES)
    common = dict(
        norm_mix_g=f(norm_mix_g), norm_mlp_g=f(norm_mlp_g),
        w_in_a=f(np.asarray(w_in_a)[0]), w_out_a=f(np.asarray(w_out_a)[0]),
        w_in_b=f(np.asarray(w_in_b)[0]), ret_norm_g=f(np.asarray(ret_norm_g)).reshape(1, 2048),
        w_out_b=f(np.asarray(w_out_b)[0]), w_mlp_up=f(w_mlp_up), w_mlp_down=f(w_mlp_down),
        final_norm_g=f(final_norm_g).reshape(1, D), consts=make_consts())
    in_maps = []
    for b in range(B):
        m = dict(common)
        m["x"] = np.ascontiguousarray(x[b])
        m["positions"] = np.ascontiguousarray(positions[b].reshape(S // 128, 128).T)
        in_maps.append(m)
    res = run_bass_kernel_spmd(nc, in_maps, core_ids=list(range(B)))
    return np.stack([np.asarray(r["out"], dtype=np.float32) for r in res.results], axis=0)
```
